# Optimizing a Trainium2 kernel written in Bass

```python
import jax, jax.numpy as jnp
from jax import lax
import numpy as np

D_MODEL = 1024
BATCH = 8
SEQ = 4096
DEPTH = 4

BLOCK = 128
EPS = 1e-6
ROPE_BASE = 10000.0

MLA_HEADS = 8
MLA_NOPE = 64
MLA_ROPE = 32
MLA_V = 64
MLA_Q_RANK = 384
MLA_KV_RANK = 256
SB_HEADS = 8
SB_DIM = 64
RET_HEADS = 8
RET_DK = 64
RET_DV = 64

BRANCH_WIDTH = 512
N_BRANCH = 3
D_FF = 4 * D_MODEL

IN_SIZES = (MLA_Q_RANK, MLA_KV_RANK, MLA_ROPE,
            SB_HEADS * SB_DIM, SB_HEADS * SB_DIM, SB_HEADS * SB_DIM,
            RET_HEADS * RET_DK, RET_HEADS * RET_DK, RET_HEADS * RET_DV, RET_HEADS * RET_DV,
            N_BRANCH * D_MODEL)
D_IN = sum(IN_SIZES)

kernel_name = "hybrid_mla_stickbreak_retention_gated"


def rmsnorm(x, g):
    xf = x.astype(jnp.float32)
    y = xf * lax.rsqrt(jnp.mean(xf * xf, axis=-1, keepdims=True) + EPS)
    return (y * g.astype(jnp.float32)).astype(x.dtype)


def head_group_norm(y, g):
    yf = y.astype(jnp.float32)
    mu = jnp.mean(yf, axis=-1, keepdims=True)
    var = jnp.mean(jnp.square(yf - mu), axis=-1, keepdims=True)
    yn = ((yf - mu) * lax.rsqrt(var + EPS)).reshape(y.shape[0], y.shape[1], -1)
    return (yn * g.astype(jnp.float32)).astype(y.dtype)


def rope_tables(positions, dim):
    inv_freq = ROPE_BASE ** (-jnp.arange(0, dim, 2, dtype=jnp.float32) / dim)
    ang = positions.astype(jnp.float32)[..., None] * inv_freq
    return jnp.cos(ang)[:, :, None, :], jnp.sin(ang)[:, :, None, :]


def apply_rope(x, cos, sin):
    half = x.shape[-1] // 2
    xf = x.astype(jnp.float32)
    x1, x2 = xf[..., :half], xf[..., half:]
    return jnp.concatenate([x1 * cos - x2 * sin, x2 * cos + x1 * sin], axis=-1).astype(x.dtype)


def causal_softmax_attention(q, k, v, scale):
    S = q.shape[1]
    outs = []
    for start in range(0, S, BLOCK):
        end = start + BLOCK
        s = jnp.einsum('bqhd,bkhd->bhqk', q[:, start:end], k[:, :end]).astype(jnp.float32) * scale
        t_idx = start + jnp.arange(BLOCK)[:, None]
        s_idx = jnp.arange(end)[None, :]
        s = jnp.where(s_idx <= t_idx, s, -jnp.inf)
        p = jax.nn.softmax(s, axis=-1)
        outs.append(jnp.einsum('bhqk,bkhd->bqhd', p.astype(v.dtype), v[:, :end]))
    return jnp.concatenate(outs, axis=1)


def stick_breaking_attention(q, k, v):
    S = q.shape[1]
    scale = SB_DIM ** -0.5
    outs = []
    for start in range(0, S, BLOCK):
        end = start + BLOCK
        z = jnp.einsum('bqhd,bkhd->bhqk', q[:, start:end], k[:, :end]).astype(jnp.float32) * scale
        t_idx = start + jnp.arange(BLOCK)[:, None]
        s_idx = jnp.arange(end)[None, :]
        before = s_idx < t_idx
        log_not = jnp.where(before, jax.nn.log_sigmoid(-z), 0.0)
        between = lax.cumsum(log_not, axis=3, reverse=True) - log_not
        a = jnp.where(before, jnp.exp(jax.nn.log_sigmoid(z) + between), 0.0)
        outs.append(jnp.einsum('bhqk,bkhd->bqhd', a.astype(v.dtype), v[:, :end]))
    return jnp.concatenate(outs, axis=1)


def retention(q, k, v, log_gamma):
    B, S, H, dk = q.shape
    dv = v.shape[-1]
    C = BLOCK
    N = S // C
    qc = q.astype(jnp.float32).reshape(B, N, C, H, dk)
    kc = (k.astype(jnp.float32) * dk ** -0.5).reshape(B, N, C, H, dk)
    vc = v.astype(jnp.float32).reshape(B, N, C, H, dv)
    idx = jnp.arange(C, dtype=jnp.float32)
    diff = idx[:, None] - idx[None, :]
    intra_decay = jnp.where(diff[None] >= 0,
                            jnp.exp(jnp.maximum(diff, 0.0)[None] * log_gamma[:, None, None]), 0.0)
    scores = jnp.einsum('bnchd,bnmhd->bnhcm', qc, kc) * intra_decay
    y_intra = jnp.einsum('bnhcm,bnmhe->bnche', scores, vc)
    zeta = jnp.exp((C - 1 - idx)[:, None] * log_gamma[None, :])
    kv = jnp.einsum('bnchd,bnche->nbhde', kc * zeta[None, None, :, :, None], vc)
    chunk_decay = jnp.exp(C * log_gamma)[None, :, None, None]

    def step(state, kv_n):
        return state * chunk_decay + kv_n, state

    _, prev = lax.scan(step, jnp.zeros((B, H, dk, dv), jnp.float32), kv)
    xi = jnp.exp((idx + 1.0)[:, None] * log_gamma[None, :])
    y_cross = jnp.einsum('bnchd,nbhde->bnche', qc, prev) * xi[None, None, :, :, None]
    return (y_intra + y_cross).reshape(B, S, H, dv).astype(v.dtype)


def setup_inputs(seed: int = 0) -> dict:
    key = jax.random.key(seed)
    ks = jax.random.split(key, 16)
    f32 = jnp.float32

    def dense(k, shape, fan_in):
        return jax.random.normal(k, shape, f32) * fan_in ** -0.5

    def gain(k, shape):
        return 1.0 + 0.02 * jax.random.normal(k, shape, f32)

    x = jax.random.normal(ks[0], (BATCH, SEQ, D_MODEL), f32)
    offset = jax.random.randint(ks[1], (BATCH, 1), 0, 1024, dtype=jnp.int32)
    positions = offset + jnp.arange(SEQ, dtype=jnp.int32)[None, :]
    return {
        "x": x,
        "positions": positions,
        "norm_mix_g": gain(ks[2], (DEPTH, D_MODEL)),
        "w_in": dense(ks[3], (DEPTH, D_MODEL, D_IN), D_MODEL),
        "mla_q_norm_g": gain(ks[4], (DEPTH, MLA_Q_RANK)),
        "mla_w_uq": dense(ks[5], (DEPTH, MLA_Q_RANK, MLA_HEADS * (MLA_NOPE + MLA_ROPE)), MLA_Q_RANK),
        "mla_kv_norm_g": gain(ks[6], (DEPTH, MLA_KV_RANK)),
        "mla_w_ukv": dense(ks[7], (DEPTH, MLA_KV_RANK, MLA_HEADS * (MLA_NOPE + MLA_V)), MLA_KV_RANK),
        "ret_norm_g": gain(ks[8], (DEPTH, RET_HEADS * RET_DV)),
        "w_branch": dense(ks[9], (DEPTH, N_BRANCH, BRANCH_WIDTH, D_MODEL), BRANCH_WIDTH),
        "w_out": dense(ks[10], (DEPTH, D_MODEL, D_MODEL), D_MODEL),
        "norm_mlp_g": gain(ks[11], (DEPTH, D_MODEL)),
        "w_up": dense(ks[12], (DEPTH, D_MODEL, D_FF), D_MODEL),
        "w_down": dense(ks[13], (DEPTH, D_FF, D_MODEL), D_FF),
        "final_norm_g": gain(ks[14], (D_MODEL,)),
    }


def reference(x, positions, norm_mix_g, w_in, mla_q_norm_g, mla_w_uq, mla_kv_norm_g, mla_w_ukv,
              ret_norm_g, w_branch, w_out, norm_mlp_g, w_up, w_down, final_norm_g):
    B, S, _ = x.shape
    cos_m, sin_m = rope_tables(positions, MLA_ROPE)
    cos_r, sin_r = rope_tables(positions, RET_DK)
    log_gamma = jnp.log1p(-jnp.exp2(-5.0 - jnp.arange(RET_HEADS, dtype=jnp.float32)))
    splits = [int(v) for v in np.cumsum(IN_SIZES)[:-1]]

    for l in range(DEPTH):
        h = rmsnorm(x, norm_mix_g[l])
        proj = h @ w_in[l]
        (c_q, c_kv, k_pe, sb_q, sb_k, sb_v, r_q, r_k, r_v, r_g, gate_logits) = jnp.split(proj, splits, axis=-1)

        q = (rmsnorm(c_q, mla_q_norm_g[l]) @ mla_w_uq[l]).reshape(B, S, MLA_HEADS, MLA_NOPE + MLA_ROPE)
        kv = (rmsnorm(c_kv, mla_kv_norm_g[l]) @ mla_w_ukv[l]).reshape(B, S, MLA_HEADS, MLA_NOPE + MLA_V)
        k_rot = jnp.broadcast_to(apply_rope(k_pe[:, :, None, :], cos_m, sin_m), (B, S, MLA_HEADS, MLA_ROPE))
        q_a = jnp.concatenate([q[..., :MLA_NOPE], apply_rope(q[..., MLA_NOPE:], cos_m, sin_m)], axis=-1)
        k_a = jnp.concatenate([kv[..., :MLA_NOPE], k_rot], axis=-1)
        y_a = causal_softmax_attention(q_a, k_a, kv[..., MLA_NOPE:], (MLA_NOPE + MLA_ROPE) ** -0.5).reshape(B, S, -1)

        y_b = stick_breaking_attention(sb_q.reshape(B, S, SB_HEADS, SB_DIM),
                                       sb_k.reshape(B, S, SB_HEADS, SB_DIM),
                                       sb_v.reshape(B, S, SB_HEADS, SB_DIM)).reshape(B, S, -1)

        y_c = retention(apply_rope(r_q.reshape(B, S, RET_HEADS, RET_DK), cos_r, sin_r),
                        apply_rope(r_k.reshape(B, S, RET_HEADS, RET_DK), cos_r, sin_r),
                        r_v.reshape(B, S, RET_HEADS, RET_DV), log_gamma)
        y_c = jax.nn.silu(r_g) * head_group_norm(y_c, ret_norm_g[l])

        branches = jnp.stack([y_a, y_b, y_c], axis=2)
        up = jnp.einsum('bsnw,nwd->bsnd', branches, w_branch[l])
        gates = jax.nn.sigmoid(gate_logits.reshape(B, S, N_BRANCH, D_MODEL))
        x = x + jnp.einsum('bsnd,bsnd->bsd', gates, up) @ w_out[l]

        h = rmsnorm(x, norm_mlp_g[l])
        x = x + jnp.square(jax.nn.relu(h @ w_up[l])) @ w_down[l]

    return rmsnorm(x, final_norm_g)
```

```python
import math
import numpy as np
import concourse.bass as bass
import concourse.mybir as mybir
from concourse.bass_utils import run_bass_kernel_spmd

F32 = mybir.dt.float32
BF16 = mybir.dt.bfloat16
I32 = mybir.dt.int32
U8 = mybir.dt.uint8
ALU = mybir.AluOpType
AF = mybir.ActivationFunctionType

D = 1024
EPS = 1e-6
NCORES = 8
DEPTH = 4
SEQ = 4096
QS = 96 ** -0.5


class Buf:
    __slots__ = ("name", "last_w", "readers")

    def __init__(self, name, reg=None):
        self.name = name
        self.last_w = None
        self.readers = []
        if reg is not None:
            reg.append(self)


class Prog:
    ENGS = ("tensor", "vector", "scalar", "gpsimd", "sync")

    def __init__(self, nc, n_dma_sems=32):
        self.nc = nc
        self.ops = []
        self.n_dma_sems = n_dma_sems
        self.allbufs = []
        self.last_barrier = None

    def buf(self, name="b"):
        b = Buf(name, self.allbufs)
        b.last_w = self.last_barrier
        return b

    def op(self, eng, fn, reads=(), writes=(), dma=False):
        idx = len(self.ops)
        deps = set()
        for b in reads:
            if b.last_w is not None:
                deps.add(b.last_w)
        for b in writes:
            if b.last_w is not None:
                deps.add(b.last_w)
            deps.update(b.readers)
        deps.discard(idx)
        self.ops.append(dict(eng=eng, fn=fn, deps=deps, dma=dma, signal=False))
        for b in reads:
            b.readers.append(idx)
        for b in writes:
            b.last_w = idx
            b.readers = []
        return idx

    def dma(self, eng, out, in_, reads=(), writes=()):
        return self.op(eng, lambda e: e.dma_start(out=out, in_=in_), reads, writes, dma=True)

    def barrier(self, scratch_ap):
        bs = list(self.allbufs)
        i = self.op("vector", lambda e: e.memset(scratch_ap, 0.0), bs, bs)
        self.last_barrier = i
        return i

    def emit(self, final_wait_ops=()):
        nc = self.nc
        ops = self.ops
        for i, o in enumerate(ops):
            nd = set()
            for d in o["deps"]:
                p = ops[d]
                if (not p["dma"]) and (not o["dma"]) and p["eng"] == o["eng"] and o["eng"] == "tensor":
                    continue
                nd.add(d)
            o["deps"] = nd
            for d in nd:
                ops[d]["signal"] = True
        for d in final_wait_ops:
            ops[d]["signal"] = True
        eng_sem = {e: nc.alloc_semaphore(name=f"s_{e}") for e in self.ENGS}
        dma_sems = [nc.alloc_semaphore(name=f"d_{i}") for i in range(self.n_dma_sems)]
        cnt = {e: 0 for e in self.ENGS}
        dma_cnt = [0] * self.n_dma_sems
        dma_rr = 0
        for o in ops:
            if o["dma"]:
                s = dma_rr % self.n_dma_sems
                dma_rr += 1
                o["prev_val"] = dma_cnt[s]
                dma_cnt[s] += 16
                o["sem"] = ("d", s)
                o["val"] = dma_cnt[s]
            elif o["signal"]:
                cnt[o["eng"]] += 1
                o["sem"] = ("e", o["eng"])
                o["val"] = cnt[o["eng"]]
        per_eng = {e: [] for e in self.ENGS}
        for i, o in enumerate(ops):
            per_eng[o["eng"]].append(i)

        def semof(key):
            return dma_sems[key[1]] if key[0] == "d" else eng_sem[key[1]]

        def run_engine(ename, eng, final=False):
            waited = {}
            for i in per_eng[ename]:
                o = ops[i]
                need = {}
                for d in o["deps"]:
                    p = ops[d]
                    k = p["sem"]
                    if p["val"] > need.get(k, 0):
                        need[k] = p["val"]
                if o["dma"] and o["prev_val"] > 0:
                    k = o["sem"]
                    need[k] = max(need.get(k, 0), o["prev_val"])
                for k, v in need.items():
                    if waited.get(k, 0) < v:
                        eng.wait_ge(semof(k), v)
                        waited[k] = v
                ins = o["fn"](eng)
                if o["dma"]:
                    ins.then_inc(semof(o["sem"]), 16)
                elif o["signal"]:
                    ins.then_inc(semof(o["sem"]), 1)
            if final:
                need = {}
                for d in final_wait_ops:
                    p = ops[d]
                    need[p["sem"]] = max(need.get(p["sem"], 0), p["val"])
                for k, v in need.items():
                    if waited.get(k, 0) < v:
                        eng.wait_ge(semof(k), v)

        with nc.Block() as block:
            @block.tensor
            def _(e):
                run_engine("tensor", e)

            @block.vector
            def _(e):
                run_engine("vector", e)

            @block.scalar
            def _(e):
                run_engine("scalar", e)

            @block.gpsimd
            def _(e):
                run_engine("gpsimd", e)

            @block.sync
            def _(e):
                run_engine("sync", e, final=True)
        return {e: len(per_eng[e]) for e in self.ENGS}


class Arena:
    def __init__(self, ap_u8, P):
        self.ap = ap_u8
        self.size = ap_u8.shape[1]
        self.off = 0
        self.P = P

    def reset(self):
        self.off = 0

    def tile(self, free_shape, dtype, name="t"):
        esz = 2 if dtype == BF16 else 4
        n = 1
        for s in free_shape:
            n *= s
        nbytes = (n * esz + 31) // 32 * 32
        assert self.off + nbytes <= self.size, (name, self.off, nbytes, self.size)
        v = self.ap[:, self.off:self.off + nbytes]
        self.off += nbytes
        v = v[:, 0:n * esz].bitcast(dtype)
        if len(free_shape) == 2:
            v = v.rearrange("p (a b) -> p a b", b=free_shape[1])
        elif len(free_shape) == 3:
            v = v.rearrange("p (a b c) -> p a b c", b=free_shape[1], c=free_shape[2])
        return v, self.P.buf(name)


class Rot:
    def __init__(self, arena, n, free_shape, dtype, name="r"):
        self.items = [arena.tile(free_shape, dtype, f"{name}{i}") for i in range(n)]
        self.i = 0

    def next(self):
        it = self.items[self.i % len(self.items)]
        self.i += 1
        return it


CONST_LAYOUT = {}


def build_consts():
    cols = []

    def add(name, arr):
        arr = np.asarray(arr, np.float64).reshape(128, -1)
        off = sum(c.shape[1] for c in cols)
        CONST_LAYOUT[name] = (off, arr.shape[1])
        cols.append(arr)

    bd = np.zeros((128, 128))
    bd[0:64, 0:64] = 1.0 / 64
    bd[64:128, 64:128] = 1.0 / 64
    add("bd", bd)
    h = np.arange(8)
    lg = np.log1p(-np.exp2(-5.0 - h).astype(np.float32)).astype(np.float32).astype(np.float64)
    m = np.arange(128)[:, None]
    c = np.arange(128)[None, :]
    dt = np.stack([np.where(c >= m, np.exp(np.maximum(c - m, 0) * lg[hh]), 0.0) for hh in range(8)], 1)
    add("dt", dt)
    z = np.zeros((128, 4, 128))
    for pr in range(4):
        for col in range(128):
            hh = 2 * pr + col // 64
            z[:, pr, col] = np.exp((127 - np.arange(128)) * lg[hh])
    add("zeta", z)
    xi = np.zeros((128, 8, 128))
    for hh in range(8):
        xi[:, hh, :] = np.exp((np.arange(128) + 1.0) * lg[hh])[None, :]
    add("xi", xi)
    r = np.arange(128)
    invf_r = (10000.0 ** (-(2.0 * (r % 32)) / 64)).astype(np.float32)
    invf_m = (10000.0 ** (-(2.0 * (r % 16)) / 32)).astype(np.float32)
    add("invf", np.stack([invf_r, invf_m], 1))
    sg_r = np.where((r % 64) < 32, -1.0, 1.0)
    sg_m = np.where((r % 32) < 16, -1.0, 1.0)
    add("sgn", np.stack([sg_r, sg_m], 1))
    cd = np.exp(128.0 * lg)
    add("cdecay", np.tile(cd[None, :], (128, 1)))
    return np.concatenate(cols, 1).astype(np.float32)


IN_OFFS = dict(cq=(0, 384), ckv=(384, 640), kpe=(640, 672), sbq=(672, 1184), sbk=(1184, 1696), sbv=(1696, 2208),
               rq=(2208, 2720), rk=(2720, 3232), rv=(3232, 3744), rg=(3744, 4256), gate=(4256, 7328))
WA_COLS = 3840
WA_OFF = dict(cq=0, ckv=384, sbq=640, sbk=1152, rq=1664, rqs=2176, rk=2688, rks=3200, kpe=3712, kpes=3744)


def build_masks():
    p = np.arange(128)[:, None]
    j = np.arange(512)[None, :]
    mA = np.stack([(j >= p + 128 * d) for d in range(4)], 1).astype(np.float32).reshape(128, -1)
    mS = np.stack([(j > p + 128 * d) for d in range(4)], 1).astype(np.float32).reshape(128, -1)
    jj = np.arange(128)[:, None]
    ss = np.arange(128)[None, :]
    tri = -(jj >= ss).astype(np.float32)
    return np.concatenate([mA, mS, tri, np.eye(128, dtype=np.float32)], 1)


def host_layout(inputs):
    w_in = np.asarray(inputs["w_in"])
    depth = w_in.shape[0]

    def sl(name):
        a, b = IN_OFFS[name]
        return w_in[:, :, a:b]

    def swap_heads(w, hd):
        L, K, N = w.shape
        w4 = w.reshape(L, K, N // hd, hd)
        return np.concatenate([w4[..., hd // 2:], w4[..., :hd // 2]], -1).reshape(L, K, N)

    WA = np.concatenate([sl("cq"), sl("ckv"), sl("sbq"), sl("sbk"), sl("rq"), swap_heads(sl("rq"), 64),
                         sl("rk"), swap_heads(sl("rk"), 64), sl("kpe"), swap_heads(sl("kpe"), 32)], -1)
    WV = np.concatenate([sl("sbv"), sl("rv")], -1)
    WG = np.concatenate([sl("rg"), sl("gate")], -1)
    wuq = np.asarray(inputs["mla_w_uq"]).reshape(depth, 384, 8, 96)
    qn = wuq[..., :64].reshape(depth, 384, 512)
    qr = wuq[..., 64:].reshape(depth, 384, 256)
    WQ = np.concatenate([qn, qr, swap_heads(qr, 32)], -1)
    wukv = np.asarray(inputs["mla_w_ukv"]).reshape(depth, 256, 8, 128)
    WKV = np.concatenate([wukv[..., :64].reshape(depth, 256, 512), wukv[..., 64:].reshape(depth, 256, 512)], -1)
    WB = np.asarray(inputs["w_branch"]).reshape(depth, 1536, 1024)

    def g128(v):
        L, N = v.shape
        return v.reshape(L, N // 128, 128).transpose(0, 2, 1)

    gains = np.concatenate([g128(np.asarray(inputs["norm_mix_g"])), g128(np.asarray(inputs["mla_q_norm_g"])),
                            g128(np.asarray(inputs["mla_kv_norm_g"])), g128(np.asarray(inputs["norm_mlp_g"])),
                            g128(np.asarray(inputs["ret_norm_g"])),
                            np.broadcast_to(g128(np.asarray(inputs["final_norm_g"])[None]), (depth, 128, 8))], -1)
    com = dict(WA=WA, WV=WV, WG=WG, WQ=WQ, WKV=WKV, WB=WB, WO=np.asarray(inputs["w_out"]),
               WU=np.asarray(inputs["w_up"]), WD=np.asarray(inputs["w_down"]), gains=gains)
    return {k: np.ascontiguousarray(v, dtype=np.float32) for k, v in com.items()}


WSHAPES = dict(WA=(1024, 3776), WV=(1024, 1024), WG=(1024, 3584), WQ=(384, 1024), WKV=(256, 1024),
               WB=(1536, 1024), WO=(1024, 1024), WU=(1024, 4096), WD=(4096, 1024))
WGAIN = dict(WA=0, WV=0, WG=0, WQ=8, WKV=11, WU=13)


def build(S=SEQ, depth=DEPTH, debug=()):
    NG = S // 512
    NB = S // 128
    nc = bass.Bass("TRN2", target_bir_lowering=False)
    P = Prog(nc)
    consts_np = build_consts()
    NCONST = consts_np.shape[1]

    def dram(name, shape, dt, kind=None):
        if kind is None:
            kind = "ExternalOutput" if name in debug else "Internal"
        return nc.dram_tensor(name, list(shape), dt, kind=kind).ap()

    xin = dram("xin", [D, S], F32, "ExternalInput")
    pos = dram("pos", [1, S], I32, "ExternalInput")
    cin = dram("consts", [128, NCONST], F32, "ExternalInput")
    cmin = dram("cmask", [128, 4352], F32, "ExternalInput")
    gin = dram("gains", [depth, 128, 33], F32, "ExternalInput")
    Win = {k: dram(k, [depth] + list(s), F32, "ExternalInput") for k, s in WSHAPES.items()}
    outT = dram("outT", [D, S], F32, "ExternalOutput")
    Wb = {k: dram("b" + k, [depth] + list(s), BF16) for k, s in WSHAPES.items()}
    xres = dram("xres", [D, S], F32)
    tabR = dram("tabR", [128, 2, S], F32)
    tabM = dram("tabM", [128, 2, S], F32)
    d_sq = dram("d_sq", [512, S], BF16)
    d_sk = dram("d_sk", [512, S], BF16)
    d_sv = dram("d_sv", [S, 512], BF16)
    d_rq = dram("d_rq", [512, S], BF16)
    d_rk = dram("d_rk", [512, S], BF16)
    d_rv = dram("d_rv", [S, 512], BF16)
    d_krot = dram("d_krot", [32, S], BF16)
    d_qn = dram("d_qn", [512, S], BF16)
    d_qr = dram("d_qr", [256, S], BF16)
    d_kn = dram("d_kn", [512, S], BF16)
    d_va = dram("d_va", [S, 8, 65], BF16)
    d_ya = dram("d_ya", [512, S], BF16)
    d_yb = dram("d_yb", [512, S], BF16)
    d_yc = dram("d_yc", [512, S], F32)
    Bx = [P.buf(f"x{g}") for g in range(NG)]
    Bw = P.buf("wb16")
    Btab = P.buf("tab")
    Bd = {n: P.buf(n) for n in "sq sk sv rq rk rv krot qn qr kn va ya yb yc".split()}

    cst = nc.alloc_sbuf_tensor("cst", [128, NCONST], F32)
    Bc = P.buf("cst")
    gsb = nc.alloc_sbuf_tensor("gsb", [128, depth, 33], F32)
    Bg = P.buf("gsb")
    PERS_BF = 4 * 512 * 2 + 128 * 4
    cb = nc.alloc_sbuf_tensor("cb", [128, 4 * 512 * 2 + 128 * 3], BF16)
    Bcb = P.buf("cb")
    ones32 = nc.alloc_sbuf_tensor("ones32", [128, 128], F32)
    scr = nc.alloc_sbuf_tensor("scr", [128, 8], F32)
    ARENA_BYTES = 186 * 1024
    arena_t = nc.alloc_sbuf_tensor("arena", [128, ARENA_BYTES], U8)
    A = Arena(arena_t[:, :], P)
    psb = [nc.alloc_psum_tensor(f"ps{i}", [128, 512], F32)[:, :] for i in range(8)]
    PB = [P.buf(f"ps{i}") for i in range(8)]

    class PsRot:
        def __init__(self, idxs):
            self.idxs = idxs
            self.i = 0

        def next(self):
            k = self.idxs[self.i % len(self.idxs)]
            self.i += 1
            return psb[k], PB[k]

    def C(name):
        o, n = CONST_LAYOUT[name]
        return cst[:, o:o + n]

    maskA = cb[:, 0:2048].rearrange("p (d j) -> p d j", j=512)
    maskS = cb[:, 2048:4096].rearrange("p (d j) -> p d j", j=512)
    trineg = cb[:, 4096:4224]
    identb = cb[:, 4224:4352]
    onesb = cb[:, 4352:4480]

    P.dma("sync", cst[:], cin, writes=[Bc])
    P.dma("sync", gsb[:], gin.rearrange("l p c -> p l c"), writes=[Bg])
    A.reset()
    cm_t, cm_b = A.tile([4352], F32, "cmask")
    P.dma("sync", cm_t, cmin, writes=[cm_b])
    P.op("vector", lambda e: e.tensor_copy(out=cb[:, 0:4352], in_=cm_t), [cm_b], [Bcb])
    P.barrier(scr[:, 0:1])
    P.op("vector", lambda e: e.memset(onesb, 1.0), [], [Bcb])
    P.op("vector", lambda e: e.memset(ones32[:], 1.0), [], [Bcb])
    P.dma("sync", xres, xin, writes=Bx)

    A.reset()
    TW = min(S, 2048)
    pi_t, b_pi = A.tile([TW], I32, "pi")
    pf_t, b_pf = A.tile([TW], F32, "pf")
    ang_t, _ = A.tile([TW], F32, "ang")
    t4_t, _ = A.tile([TW], F32, "t4")
    ki_t, _ = A.tile([TW], I32, "ki")
    kf_t, _ = A.tile([TW], F32, "kf")
    mk_t, _ = A.tile([TW], F32, "mk")
    tb_t, b_tb = A.tile([2, TW], F32, "tabst")
    C1 = 6.28125
    C2 = 2 * math.pi - C1
    bt = b_pf

    def reduce_and_sin(shift, out_ap, post_sign_col):
        V = lambda f: P.op("vector", f, [bt, Bc], [bt])
        V(lambda e: e.tensor_scalar(out=ang_t, in0=ang_t, scalar1=float(shift), scalar2=None, op0=ALU.add))
        V(lambda e: e.tensor_scalar(out=t4_t, in0=ang_t, scalar1=1.0 / (2 * math.pi), scalar2=0.5, op0=ALU.mult, op1=ALU.add))
        V(lambda e: e.tensor_copy(out=ki_t, in_=t4_t))
        V(lambda e: e.tensor_copy(out=kf_t, in_=ki_t))
        V(lambda e: e.scalar_tensor_tensor(out=t4_t, in0=kf_t, scalar=-C1, in1=ang_t, op0=ALU.mult, op1=ALU.add))
        V(lambda e: e.scalar_tensor_tensor(out=t4_t, in0=kf_t, scalar=-C2, in1=t4_t, op0=ALU.mult, op1=ALU.add))
        V(lambda e: e.tensor_scalar(out=mk_t, in0=t4_t, scalar1=-math.pi, scalar2=2 * math.pi, op0=ALU.is_lt, op1=ALU.mult))
        V(lambda e: e.tensor_tensor(out=t4_t, in0=t4_t, in1=mk_t, op=ALU.add))
        V(lambda e: e.tensor_scalar(out=mk_t, in0=t4_t, scalar1=math.pi, scalar2=-2 * math.pi, op0=ALU.is_gt, op1=ALU.mult))
        V(lambda e: e.tensor_tensor(out=t4_t, in0=t4_t, in1=mk_t, op=ALU.add))
        V(lambda e: e.tensor_scalar(out=t4_t, in0=t4_t, scalar1=-3.1415925, scalar2=3.1415925, op0=ALU.max, op1=ALU.min))
        P.op("scalar", lambda e: e.activation(out=out_ap, in_=t4_t, func=AF.Sin), [bt], [b_tb])
        if post_sign_col is not None:
            P.op("vector", lambda e: e.tensor_scalar(out=out_ap, in0=out_ap, scalar1=post_sign_col, scalar2=None, op0=ALU.mult), [b_tb, Bc], [b_tb])

    for which, tab in ((0, tabR), (1, tabM)):
        for t0 in range(0, S, TW):
            P.dma("sync", pi_t, pos[:, t0:t0 + TW].partition_broadcast(128), writes=[b_pi])
            P.op("vector", lambda e: e.tensor_copy(out=pf_t, in_=pi_t), [b_pi], [bt])
            invc = C("invf")[:, which:which + 1]
            sgc = C("sgn")[:, which:which + 1]
            P.op("vector", lambda e, invc=invc: e.tensor_scalar(out=ang_t, in0=pf_t, scalar1=invc, scalar2=None, op0=ALU.mult), [bt, Bc], [bt])
            reduce_and_sin(math.pi / 2, tb_t[:, 0, :], None)
            P.op("vector", lambda e, invc=invc: e.tensor_scalar(out=ang_t, in0=pf_t, scalar1=invc, scalar2=None, op0=ALU.mult), [bt, Bc], [bt])
            reduce_and_sin(0.0, tb_t[:, 1, :], sgc)
            P.dma("gpsimd", tab[:, :, t0:t0 + TW], tb_t, reads=[b_tb], writes=[Btab])

    P.barrier(scr[:, 0:1])
    A.reset()
    PW = 2048
    ldr = Rot(A, 3, [PW], F32, "wld")
    cvr = Rot(A, 3, [PW], BF16, "wcv")
    cnt = 0
    for l in range(depth):
        for k, (K, N) in WSHAPES.items():
            goff = WGAIN.get(k)
            for kc in range(K // 128):
                for c0 in range(0, N, PW):
                    n = min(PW, N - c0)
                    lt, lb = ldr.next()
                    ct, cbf = cvr.next()
                    P.dma("sync", lt[:, 0:n], Win[k][l, kc * 128:(kc + 1) * 128, c0:c0 + n], writes=[lb])
                    eng = ("vector", "gpsimd")[cnt % 2]
                    cnt += 1
                    if goff is None:
                        P.op(eng, lambda e, ct=ct, lt=lt, n=n: e.tensor_copy(out=ct[:, 0:n], in_=lt[:, 0:n]), [lb], [cbf])
                    else:
                        gcol = gsb[:, l, goff + kc:goff + kc + 1]
                        P.op(eng, lambda e, ct=ct, lt=lt, n=n, gcol=gcol: e.tensor_scalar(out=ct[:, 0:n], in0=lt[:, 0:n], scalar1=gcol, scalar2=None, op0=ALU.mult), [lb, Bg], [cbf])
                    P.dma("gpsimd", Wb[k][l, kc * 128:(kc + 1) * 128, c0:c0 + n], ct[:, 0:n], reads=[cbf], writes=[Bw])

    def wview(k, l):
        return Wb[k][l].rearrange("(k p) n -> p k n", p=128)

    def ACT(fn, r, w):
        return P.op("scalar", fn, r, w)

    def DVE(fn, r, w):
        return P.op("vector", fn, r, w)

    def POOL(fn, r, w):
        return P.op("gpsimd", fn, r, w)

    def MM(ps, lhsT, rhs, start, stop, r, w):
        return P.op("tensor", lambda e: e.matmul(ps, lhsT=lhsT, rhs=rhs, start=start, stop=stop), r, [w])

    def rstd_from_ps(ps, pb, n, out_ap, out_b, tmp, tmpb):
        ACT(lambda e: e.activation(out=tmp, in_=ps, func=AF.Ln, bias=EPS, scale=1.0 / n), [pb], [tmpb])
        ACT(lambda e: e.activation(out=out_ap, in_=tmp, func=AF.Exp, scale=-0.5), [tmpb], [out_b])

    xv = xres.rearrange("(k p) s -> p k s", p=128)

    for l in range(depth):
        P.barrier(scr[:, 0:1])
        A.reset()
        cqT, b_cq = A.tile([3, S], BF16, "cqT")
        ckvT, b_ckv = A.tile([2, S], BF16, "ckvT")
        hT, b_h = A.tile([8, S], BF16, "hT")
        Bh = [P.buf(f"h{g}") for g in range(NG)]
        xg_r = Rot(A, 1, [8, 512], F32, "xg")
        sq_r = Rot(A, 3, [512], BF16, "sq")
        f32_r = Rot(A, 6, [512], F32, "f32")
        w_r = Rot(A, 2, [8, 256], BF16, "wblk")
        wv_t, b_wv = A.tile([8, 512], BF16, "wv")
        st_r = Rot(A, 2, [S], BF16, "stage")
        tab_r = Rot(A, 2, [2, 512], F32, "tab")
        vst_r = Rot(A, 2, [4, 512], BF16, "vst")
        pr = PsRot([0, 1, 2, 3, 4, 5, 6, 7])

        def sl(g):
            return slice(g * 512, (g + 1) * 512)

        for g in range(NG):
            xg, bxg = xg_r.next()
            P.dma("sync", xg, xv[:, :, sl(g)], reads=[Bx[g]], writes=[bxg])
            ps, pb = pr.next()
            for k in range(8):
                sq, bsq = sq_r.next()
                ACT(lambda e, sq=sq, xg=xg, k=k: e.activation(out=sq, in_=xg[:, k, :], func=AF.Square), [bxg], [bsq])
                MM(ps, onesb, sq, k == 0, k == 7, [bsq, Bcb], pb)
            tmp, tmpb = f32_r.next()
            rs, rsb = f32_r.next()
            rstd_from_ps(ps, pb, 1024.0, rs, rsb, tmp, tmpb)
            for k in range(8):
                DVE(lambda e, k=k, xg=xg, rs=rs, g=g: e.tensor_tensor(out=hT[:, k, sl(g)], in0=xg[:, k, :], in1=rs, op=ALU.mult), [bxg, rsb], [Bh[g]])

        wa = wview("WA", l)

        def fm_block(c0, M, n_mm=1, c1=None):
            wt, wtb = w_r.next()
            P.dma("sync", wt[:, :, 0:M], wa[:, :, c0:c0 + M], reads=[Bw], writes=[wtb])
            if c1 is not None:
                P.dma("sync", wt[:, :, 128:128 + M], wa[:, :, c1:c1 + M], reads=[Bw], writes=[wtb])
            return wt, wtb

        def fm_mm(wt, wtb, off, M, g):
            ps, pb = pr.next()
            for k in range(8):
                MM(ps[0:M, :], wt[:, k, off:off + M], hT[:, k, sl(g)], k == 0, k == 7, [wtb, Bh[g]], pb)
            return ps, pb

        for name, dst, dstb, nblk in (("cq", cqT, b_cq, 3), ("ckv", ckvT, b_ckv, 2)):
            for c in range(nblk):
                wt, wtb = fm_block(WA_OFF[name] + c * 128, 128)
                for g in range(NG):
                    ps, pb = fm_mm(wt, wtb, 0, 128, g)
                    ACT(lambda e, ps=ps, dst=dst, c=c, g=g: e.copy(out=dst[:, c, sl(g)], in_=ps), [pb], [dstb])
        for name, dd, dbuf, scale in (("sbq", d_sq, Bd["sq"], 0.125), ("sbk", d_sk, Bd["sk"], 1.0)):
            for c in range(4):
                wt, wtb = fm_block(WA_OFF[name] + c * 128, 128)
                st, stb = st_r.next()
                for g in range(NG):
                    ps, pb = fm_mm(wt, wtb, 0, 128, g)
                    ACT(lambda e, ps=ps, st=st, g=g, scale=scale: e.mul(out=st[:, sl(g)], in_=ps, mul=scale), [pb], [stb])
                P.dma("gpsimd", dd[c * 128:(c + 1) * 128, :], st, reads=[stb], writes=[dbuf])
        for name, sname, dd, dbuf, scale, M, tab, nblk in (
                ("rq", "rqs", d_rq, Bd["rq"], 1.0, 128, tabR, 4), ("rk", "rks", d_rk, Bd["rk"], 0.125, 128, tabR, 4),
                ("kpe", "kpes", d_krot, Bd["krot"], 1.0, 32, tabM, 1)):
            for c in range(nblk):
                wt, wtb = fm_block(WA_OFF[name] + c * 128, M, c1=WA_OFF[sname] + c * 128)
                st, stb = st_r.next()
                for g in range(NG):
                    tb, tbb = tab_r.next()
                    P.dma("sync", tb[0:M], tab[0:M, :, sl(g)], reads=[Btab], writes=[tbb])
                    ps, pb = fm_mm(wt, wtb, 0, M, g)
                    ps2, pb2 = fm_mm(wt, wtb, 128, M, g)
                    t1, t1b = f32_r.next()
                    t2, t2b = f32_r.next()
                    DVE(lambda e, ps=ps, tb=tb, t1=t1, M=M, scale=scale: e.scalar_tensor_tensor(out=t1[0:M], in0=ps[0:M, :], scalar=scale, in1=tb[0:M, 0, :], op0=ALU.mult, op1=ALU.mult), [pb, tbb], [t1b])
                    DVE(lambda e, ps2=ps2, tb=tb, t2=t2, M=M, scale=scale: e.scalar_tensor_tensor(out=t2[0:M], in0=ps2[0:M, :], scalar=scale, in1=tb[0:M, 1, :], op0=ALU.mult, op1=ALU.mult), [pb2, tbb], [t2b])
                    POOL(lambda e, st=st, t1=t1, t2=t2, g=g, M=M: e.tensor_tensor(out=st[0:M, sl(g)], in0=t1[0:M], in1=t2[0:M], op=ALU.add), [t1b, t2b], [stb])
                P.dma("gpsimd", dd[c * 128:c * 128 + M, :], st[0:M], reads=[stb], writes=[dbuf])
        wvv = wview("WV", l)
        for vi, (dd, dbuf) in enumerate(((d_sv, Bd["sv"]), (d_rv, Bd["rv"]))):
            P.dma("sync", wv_t, wvv[:, :, vi * 512:(vi + 1) * 512], reads=[Bw], writes=[b_wv])
            ddv = dd.rearrange("(b p) c -> p b c", p=128)
            for g in range(NG):
                vs, vsb = vst_r.next()
                for j in range(4):
                    tbi = g * 4 + j
                    ps, pb = pr.next()
                    for k in range(8):
                        MM(ps, hT[:, k, tbi * 128:(tbi + 1) * 128], wv_t[:, k, :], k == 0, k == 7, [Bh[g], b_wv], pb)
                    if j % 2 == 0:
                        ACT(lambda e, ps=ps, vs=vs, j=j: e.copy(out=vs[:, j, :], in_=ps), [pb], [vsb])
                    else:
                        DVE(lambda e, ps=ps, vs=vs, j=j: e.tensor_copy(out=vs[:, j, :], in_=ps), [pb], [vsb])
                P.dma("gpsimd", ddv[:, g * 4:(g + 1) * 4, :], vs, reads=[vsb], writes=[dbuf])

        P.barrier(scr[:, 0:1])
        A.reset()
        cqT, b_cq = A.tile([3, S], BF16, "cqT")
        ckvT, b_ckv = A.tile([2, S], BF16, "ckvT")
        sq_r = Rot(A, 4, [512], BF16, "sq")
        f32_r = Rot(A, 8, [512], F32, "f32")
        tab_r = Rot(A, 2, [2, 512], F32, "tab")
        wq_t, b_wq = A.tile([3, 1024], BF16, "wq")
        wkv_t, b_wkv = A.tile([2, 1024], BF16, "wkv")
        ost_r = Rot(A, 4, [512], BF16, "ost")
        vaug_r = Rot(A, 2, [4, 8, 65], BF16, "vaug")
        rtok_r = Rot(A, 2, [4], F32, "rtok")
        P.dma("sync", wq_t, wview("WQ", l), reads=[Bw], writes=[b_wq])
        P.dma("sync", wkv_t, wview("WKV", l), reads=[Bw], writes=[b_wkv])
        for it in vaug_r.items:
            DVE(lambda e, t=it[0]: e.memset(t[:, :, :, 64:65], 1.0), [], [it[1]])
        vav = d_va.rearrange("(b p) h e -> p b (h e)", p=128)
        for g in range(NG):
            tbm, tbmb = tab_r.next()
            P.dma("sync", tbm, tabM[:, :, sl(g)], reads=[Btab], writes=[tbmb])
            ps, pb = pr.next()
            for k in range(3):
                sq, bsq = sq_r.next()
                POOL(lambda e, sq=sq, k=k, g=g: e.tensor_tensor(out=sq, in0=cqT[:, k, sl(g)], in1=cqT[:, k, sl(g)], op=ALU.mult), [b_cq], [bsq])
                MM(ps, onesb, sq, k == 0, k == 2, [bsq, Bcb], pb)
            tmp, tmpb = f32_r.next()
            rq_, rqb = f32_r.next()
            rstd_from_ps(ps, pb, 384.0, rq_, rqb, tmp, tmpb)
            ps, pb = pr.next()
            ps_t, pb_t = pr.next()
            sqs = []
            for k in range(2):
                sq, bsq = sq_r.next()
                POOL(lambda e, sq=sq, k=k, g=g: e.tensor_tensor(out=sq, in0=ckvT[:, k, sl(g)], in1=ckvT[:, k, sl(g)], op=ALU.mult), [b_ckv], [bsq])
                MM(ps, onesb, sq, k == 0, k == 1, [bsq, Bcb], pb)
                sqs.append((sq, bsq))
            for j in range(4):
                for k in range(2):
                    MM(ps_t[:, j:j + 1], sqs[k][0][:, j * 128:(j + 1) * 128], onesb[:, 0:1], k == 0, k == 1, [sqs[k][1], Bcb], pb_t)
            tmp2, tmp2b = f32_r.next()
            rkv, rkvb = f32_r.next()
            rstd_from_ps(ps, pb, 256.0, rkv, rkvb, tmp2, tmp2b)
            rt, rtb = rtok_r.next()
            ACT(lambda e, rt=rt, ps_t=ps_t: e.activation(out=rt, in_=ps_t[:, 0:4], func=AF.Ln, bias=EPS, scale=1.0 / 256.0), [pb_t], [rtb])
            ACT(lambda e, rt=rt: e.activation(out=rt, in_=rt, func=AF.Exp, scale=-0.5), [rtb], [rtb])
            for c in range(4):
                ps, pb = pr.next()
                for k in range(3):
                    MM(ps, wq_t[:, k, c * 128:(c + 1) * 128], cqT[:, k, sl(g)], k == 0, k == 2, [b_wq, b_cq], pb)
                o, ob = ost_r.next()
                DVE(lambda e, o=o, ps=ps, rq_=rq_: e.scalar_tensor_tensor(out=o, in0=ps, scalar=QS, in1=rq_, op0=ALU.mult, op1=ALU.mult), [pb, rqb], [ob])
                P.dma("gpsimd", d_qn[c * 128:(c + 1) * 128, sl(g)], o, reads=[ob], writes=[Bd["qn"]])
            for c in range(2):
                ps, pb = pr.next()
                ps2, pb2 = pr.next()
                for k in range(3):
                    MM(ps, wq_t[:, k, 512 + c * 128:512 + (c + 1) * 128], cqT[:, k, sl(g)], k == 0, k == 2, [b_wq, b_cq], pb)
                for k in range(3):
                    MM(ps2, wq_t[:, k, 768 + c * 128:768 + (c + 1) * 128], cqT[:, k, sl(g)], k == 0, k == 2, [b_wq, b_cq], pb2)
                t1, t1b = f32_r.next()
                t2, t2b = f32_r.next()
                DVE(lambda e, ps=ps, tbm=tbm, t1=t1: e.scalar_tensor_tensor(out=t1, in0=ps, scalar=QS, in1=tbm[:, 0, :], op0=ALU.mult, op1=ALU.mult), [pb, tbmb], [t1b])
                DVE(lambda e, ps2=ps2, tbm=tbm, t2=t2: e.scalar_tensor_tensor(out=t2, in0=ps2, scalar=QS, in1=tbm[:, 1, :], op0=ALU.mult, op1=ALU.mult), [pb2, tbmb], [t2b])
                POOL(lambda e, t1=t1, t2=t2: e.tensor_tensor(out=t1, in0=t1, in1=t2, op=ALU.add), [t1b, t2b], [t1b])
                o, ob = ost_r.next()
                POOL(lambda e, o=o, t1=t1, rq_=rq_: e.tensor_tensor(out=o, in0=t1, in1=rq_, op=ALU.mult), [t1b, rqb], [ob])
                P.dma("gpsimd", d_qr[c * 128:(c + 1) * 128, sl(g)], o, reads=[ob], writes=[Bd["qr"]])
            for c in range(4):
                ps, pb = pr.next()
                for k in range(2):
                    MM(ps, wkv_t[:, k, c * 128:(c + 1) * 128], ckvT[:, k, sl(g)], k == 0, k == 1, [b_wkv, b_ckv], pb)
                o, ob = ost_r.next()
                DVE(lambda e, o=o, ps=ps, rkv=rkv: e.tensor_tensor(out=o, in0=ps, in1=rkv, op=ALU.mult), [pb, rkvb], [ob])
                P.dma("gpsimd", d_kn[c * 128:(c + 1) * 128, sl(g)], o, reads=[ob], writes=[Bd["kn"]])
            va, vab = vaug_r.next()
            for j in range(4):
                tbi = g * 4 + j
                ps, pb = pr.next()
                for k in range(2):
                    MM(ps, ckvT[:, k, tbi * 128:(tbi + 1) * 128], wkv_t[:, k, 512:1024], k == 0, k == 1, [b_ckv, b_wkv], pb)
                DVE(lambda e, va=va, ps=ps, rt=rt, j=j: e.tensor_scalar(out=va[:, j, :, 0:64], in0=ps.rearrange("p (h e) -> p h e", e=64), scalar1=rt[:, j:j + 1], scalar2=None, op0=ALU.mult), [pb, rtb], [vab])
            P.dma("gpsimd", vav[:, g * 4:(g + 1) * 4, :], va.rearrange("p j h e -> p j (h e)"), reads=[vab], writes=[Bd["va"]])

        P.barrier(scr[:, 0:1])
        A.reset()
        q_r = Rot(A, 2, [S], BF16, "q")
        k_r = Rot(A, 2, [S], BF16, "k")
        v_r = Rot(A, 2, [NB, 128], BF16, "v")
        p_r = Rot(A, 4, [512], BF16, "p")
        e_r = Rot(A, 3, [512], F32, "e")
        t_r = Rot(A, 3, [512], F32, "t")
        lp_r = Rot(A, 3, [512], BF16, "lp")
        csb, csbb = A.tile([512], F32, "csb")
        ys_r = Rot(A, 2, [S], BF16, "ys")
        rden, rdenb = A.tile([512], F32, "rden")
        bcs, bcsb = A.tile([512], F32, "bcs")
        ycs_r = Rot(A, 4, [512], F32, "ycs")
        st_t, st_b = A.tile([64], F32, "state")
        prevb_t, prevb_b = A.tile([64], BF16, "prevb")
        ktok_r = Rot(A, 2, [128], BF16, "ktok")
        sT_r = Rot(A, 3, [128], BF16, "sT")
        tmpc_r = Rot(A, 3, [128], F32, "tmpc")
        ps_s = PsRot([0, 1, 2])
        ps_y = PsRot([3, 4])
        ps_c = PsRot([5, 6])
        vav4 = d_va.rearrange("(b p) h e -> p b h e", p=128)
        for h in range(8):
            qt, qb = q_r.next()
            kt, kb_ = k_r.next()
            vt, vb = v_r.next()
            P.dma("sync", qt[0:64], d_qn[h * 64:(h + 1) * 64, :], reads=[Bd["qn"]], writes=[qb])
            P.dma("sync", qt[64:96], d_qr[h * 32:(h + 1) * 32, :], reads=[Bd["qr"]], writes=[qb])
            P.dma("sync", kt[0:64], d_kn[h * 64:(h + 1) * 64, :], reads=[Bd["kn"]], writes=[kb_])
            P.dma("sync", kt[64:96], d_krot, reads=[Bd["krot"]], writes=[kb_])
            P.dma("sync", vt[:, :, 0:65], vav4[:, :, h, :], reads=[Bd["va"]], writes=[vb])
            ys, ysb = ys_r.next()
            for g in range(NG):
                nkb = 4 * (g + 1)
                yps, ypb = ps_y.next()
                for kb in range(nkb):
                    sps, spb = ps_s.next()
                    MM(sps, kt[0:96, kb * 128:(kb + 1) * 128], qt[0:96, sl(g)], True, True, [kb_, qb], spb)
                    pt, ptb = p_r.next()
                    ACT(lambda e, pt=pt, sps=sps: e.activation(out=pt, in_=sps, func=AF.Exp), [spb], [ptb])
                    d = kb - 4 * g
                    if d >= 0:
                        DVE(lambda e, pt=pt, d=d: e.tensor_tensor(out=pt, in0=pt, in1=maskA[:, d, :], op=ALU.mult), [ptb, Bcb], [ptb])
                    MM(yps[0:65, :], vt[:, kb, 0:65], pt, kb == 0, kb == nkb - 1, [vb, ptb], ypb)
                DVE(lambda e, yps=yps: e.reciprocal(out=rden[64:65, :], in_=yps[64:65, :]), [ypb], [rdenb])
                bps, bpb = ps_c.next()
                MM(bps[0:64, :], ones32[64:65, 0:64], rden[64:65, :], True, True, [rdenb, Bcb], bpb)
                ACT(lambda e, bps=bps: e.copy(out=bcs[0:64, :], in_=bps[0:64, :]), [bpb], [bcsb])
                DVE(lambda e, ys=ys, yps=yps, g=g: e.tensor_tensor(out=ys[0:64, sl(g)], in0=yps[0:64, :], in1=bcs[0:64, :], op=ALU.mult), [ypb, bcsb], [ysb])
            P.dma("gpsimd", d_ya[h * 64:(h + 1) * 64, :], ys[0:64], reads=[ysb], writes=[Bd["ya"]])

        svv = d_sv.rearrange("(b p) c -> p b c", p=128)
        for h in range(8):
            qt, qb = q_r.next()
            kt, kb_ = k_r.next()
            vt, vb = v_r.next()
            P.dma("sync", qt[0:64], d_sq[h * 64:(h + 1) * 64, :], reads=[Bd["sq"]], writes=[qb])
            P.dma("sync", kt[0:64], d_sk[h * 64:(h + 1) * 64, :], reads=[Bd["sk"]], writes=[kb_])
            P.dma("sync", vt[:, :, 0:64], svv[:, :, h * 64:(h + 1) * 64], reads=[Bd["sv"]], writes=[vb])
            ys, ysb = ys_r.next()
            for g in range(NG):
                nkb = 4 * (g + 1)
                yps, ypb = ps_y.next()
                POOL(lambda e: e.memset(csb, 0.0), [], [csbb])
                for kb in range(nkb - 1, -1, -1):
                    d = kb - 4 * g
                    aps, apb = ps_s.next()
                    MM(aps, kt[0:64, kb * 128:(kb + 1) * 128], qt[0:64, sl(g)], True, False, [kb_, qb], apb)
                    et, etb = e_r.next()
                    ACT(lambda e, et=et, aps=aps: e.activation(out=et, in_=aps, func=AF.Exp), [apb], [etb])
                    lp, lpb = lp_r.next()
                    ACT(lambda e, et=et, lp=lp: e.activation(out=lp, in_=et, func=AF.Ln, bias=1.0), [etb], [lpb])
                    if d >= 0:
                        DVE(lambda e, lp=lp, d=d: e.tensor_tensor(out=lp, in0=lp, in1=maskS[:, d, :], op=ALU.mult), [lpb, Bcb], [lpb])
                    MM(aps, trineg, lp, False, True, [Bcb, lpb], apb)
                    tt, ttb = t_r.next()
                    DVE(lambda e, tt=tt, aps=aps: e.tensor_tensor(out=tt, in0=aps, in1=csb, op=ALU.subtract), [apb, csbb], [ttb])
                    if kb > 0:
                        cps, cpb = ps_c.next()
                        MM(cps, onesb, lp, True, True, [Bcb, lpb], cpb)
                        DVE(lambda e, cps=cps: e.tensor_tensor(out=csb, in0=cps, in1=csb, op=ALU.add), [cpb, csbb], [csbb])
                    pt, ptb = p_r.next()
                    ACT(lambda e, pt=pt, tt=tt: e.activation(out=pt, in_=tt, func=AF.Exp), [ttb], [ptb])
                    if d >= 0:
                        DVE(lambda e, pt=pt, d=d: e.tensor_tensor(out=pt, in0=pt, in1=maskS[:, d, :], op=ALU.mult), [ptb, Bcb], [ptb])
                    MM(yps[0:64, :], vt[:, kb, 0:64], pt, kb == nkb - 1, kb == 0, [vb, ptb], ypb)
                ACT(lambda e, ys=ys, yps=yps, g=g: e.copy(out=ys[0:64, sl(g)], in_=yps[0:64, :]), [ypb], [ysb])
            P.dma("gpsimd", d_yb[h * 64:(h + 1) * 64, :], ys[0:64], reads=[ysb], writes=[Bd["yb"]])

        rvv = d_rv.rearrange("(b p) c -> p b c", p=128)
        dtv = C("dt").rearrange("p (h c) -> p h c", c=128)
        ztv = C("zeta").rearrange("p (a c) -> p a c", c=128)
        xiv = C("xi").rearrange("p (h c) -> p h c", c=128)
        ps_a = PsRot([0, 1, 2])
        ps_b = PsRot([3, 4])
        ps_k = PsRot([5, 6])
        ps_t = PsRot([7])
        for pr_i in range(4):
            qt, qb = q_r.next()
            kt, kb_ = k_r.next()
            vt, vb = v_r.next()
            P.dma("sync", qt, d_rq[pr_i * 128:(pr_i + 1) * 128, :], reads=[Bd["rq"]], writes=[qb])
            P.dma("sync", kt, d_rk[pr_i * 128:(pr_i + 1) * 128, :], reads=[Bd["rk"]], writes=[kb_])
            P.dma("sync", vt, rvv[:, :, pr_i * 128:(pr_i + 1) * 128], reads=[Bd["rv"]], writes=[vb])
            POOL(lambda e: e.memset(st_t, 0.0), [], [st_b])
            POOL(lambda e: e.memset(prevb_t, 0.0), [], [prevb_b])
            ycs_cur = [None, None]
            for n in range(NB):
                cs = slice(n * 128, (n + 1) * 128)
                tps, tpb = ps_t.next()
                tpv = tps[:, :].bitcast(BF16)
                P.op("tensor", lambda e, tpv=tpv, kt=kt, cs=cs: e.transpose(tpv[:, 0:128], kt[:, cs], identb), [kb_, Bcb], [tpb])
                ktk, ktkb = ktok_r.next()
                DVE(lambda e, ktk=ktk, tpv=tpv, pr_i=pr_i: e.tensor_tensor(out=ktk, in0=tpv[:, 0:128], in1=ztv[:, pr_i, :], op=ALU.mult), [tpb, Bc], [ktkb])
                for j in range(2):
                    hh = 2 * pr_i + j
                    rs_ = slice(64 * j, 64 * j + 64)
                    if n % 4 == 0:
                        ycs_cur[j] = ycs_r.next()
                    yc_t, yc_b = ycs_cur[j]
                    aps, apb = ps_a.next()
                    MM(aps[:, 0:128], kt[rs_, cs], qt[rs_, cs], True, True, [kb_, qb], apb)
                    sT, sTb = sT_r.next()
                    DVE(lambda e, sT=sT, aps=aps, hh=hh: e.tensor_tensor(out=sT, in0=aps[:, 0:128], in1=dtv[:, hh, :], op=ALU.mult), [apb, Bc], [sTb])
                    yps, ypb = ps_b.next()
                    MM(yps[0:64, 0:128], vt[:, n, rs_], sT, True, True, [vb, sTb], ypb)
                    xps, xpb = ps_b.next()
                    MM(xps[0:64, 0:128], prevb_t[rs_, :], qt[rs_, cs], True, True, [prevb_b, qb], xpb)
                    tc_, tcb = tmpc_r.next()
                    DVE(lambda e, tc_=tc_, xps=xps, hh=hh: e.tensor_tensor(out=tc_[0:64], in0=xps[0:64, 0:128], in1=xiv[0:64, hh, :], op=ALU.mult), [xpb, Bc], [tcb])
                    off = (n % 4) * 128
                    DVE(lambda e, yc_t=yc_t, yps=yps, tc_=tc_, off=off: e.tensor_tensor(out=yc_t[0:64, off:off + 128], in0=yps[0:64, 0:128], in1=tc_[0:64], op=ALU.add), [ypb, tcb], [yc_b])
                    if n % 4 == 3:
                        P.dma("gpsimd", d_yc[hh * 64:(hh + 1) * 64, (n - 3) * 128:(n + 1) * 128], yc_t[0:64], reads=[yc_b], writes=[Bd["yc"]])
                kps, kpb = ps_k.next()
                MM(kps[:, 0:128], ktk, vt[:, n, :], True, True, [ktkb, vb], kpb)
                for j in range(2):
                    hh = 2 * pr_i + j
                    rs_ = slice(64 * j, 64 * j + 64)
                    cdc = C("cdecay")[rs_, hh:hh + 1]
                    DVE(lambda e, kps=kps, rs_=rs_, cdc=cdc: e.scalar_tensor_tensor(out=st_t[rs_, :], in0=st_t[rs_, :], scalar=cdc, in1=kps[rs_, rs_], op0=ALU.mult, op1=ALU.add), [kpb, st_b, Bc], [st_b])
                POOL(lambda e: e.tensor_copy(out=prevb_t, in_=st_t), [st_b], [prevb_b])

        P.barrier(scr[:, 0:1])
        A.reset()
        xg_r = Rot(A, 2, [8, 512], F32, "xg")
        hg, hgb = A.tile([8, 512], BF16, "hg")
        sq_r = Rot(A, 3, [512], BF16, "sq")
        f32_r = Rot(A, 6, [512], F32, "f32")
        ya_r = Rot(A, 2, [4, 512], BF16, "ya")
        yb_r = Rot(A, 2, [4, 512], BF16, "yb")
        yc_r = Rot(A, 1, [4, 512], F32, "yc")
        ycg, ycgb = A.tile([4, 512], BF16, "ycg")
        mT, mTb = A.tile([8, 512], BF16, "mT")
        h2, h2b = A.tile([8, 512], BF16, "h2")
        act, actb = A.tile([32, 512], BF16, "act")
        wg_r = Rot(A, 3, [8, 128], BF16, "wg")
        wb_r = Rot(A, 3, [4, 128], BF16, "wbr")
        wo_r = Rot(A, 2, [8, 128], BF16, "wo")
        wu_r = Rot(A, 2, [8, 512], BF16, "wu")
        wd_r = Rot(A, 2, [32, 128], BF16, "wd")
        macc, maccb = A.tile([512], F32, "macc")
        pr = PsRot([0, 1, 2, 3, 4, 5, 6, 7])
        wgv = wview("WG", l)
        wbv = Wb["WB"][l].rearrange("(n k p) d -> p n k d", p=128, k=4)
        wov = wview("WO", l)
        wuv = wview("WU", l)
        wdv = wview("WD", l)
        yav = d_ya.rearrange("(k p) s -> p k s", p=128)
        ybv = d_yb.rearrange("(k p) s -> p k s", p=128)
        ycv = d_yc.rearrange("(k p) s -> p k s", p=128)
        bdm = C("bd")
        last = (l == depth - 1)
        for g in range(NG):
            xg, bxg = xg_r.next()
            P.dma("sync", xg, xv[:, :, sl(g)], reads=[Bx[g]], writes=[bxg])
            yat, yab = ya_r.next()
            ybt, ybb = yb_r.next()
            yct, ycb = yc_r.next()
            P.dma("sync", yat, yav[:, :, sl(g)], reads=[Bd["ya"]], writes=[yab])
            P.dma("sync", ybt, ybv[:, :, sl(g)], reads=[Bd["yb"]], writes=[ybb])
            P.dma("sync", yct, ycv[:, :, sl(g)], reads=[Bd["yc"]], writes=[ycb])
            ps, pb = pr.next()
            for k in range(8):
                sq, bsq = sq_r.next()
                ACT(lambda e, sq=sq, xg=xg, k=k: e.activation(out=sq, in_=xg[:, k, :], func=AF.Square), [bxg], [bsq])
                MM(ps, onesb, sq, k == 0, k == 7, [bsq, Bcb], pb)
            tmp, tmpb = f32_r.next()
            rs, rsb = f32_r.next()
            rstd_from_ps(ps, pb, 1024.0, rs, rsb, tmp, tmpb)
            for k in range(8):
                DVE(lambda e, k=k, xg=xg, rs=rs: e.tensor_tensor(out=hg[:, k, :], in0=xg[:, k, :], in1=rs, op=ALU.mult), [bxg, rsb], [hgb])
            for c in range(4):
                wt, wtb = wg_r.next()
                P.dma("sync", wt, wgv[:, :, c * 128:(c + 1) * 128], reads=[Bw], writes=[wtb])
                gps, gpb = pr.next()
                for k in range(8):
                    MM(gps, wt[:, k, :], hg[:, k, :], k == 0, k == 7, [wtb, hgb], gpb)
                sil, silb = f32_r.next()
                ACT(lambda e, sil=sil, gps=gps: e.activation(out=sil, in_=gps, func=AF.Silu), [gpb], [silb])
                mps, mpb = pr.next()
                MM(mps, bdm, yct[:, c, :], True, True, [Bc, ycb], mpb)
                cen, cenb = f32_r.next()
                DVE(lambda e, cen=cen, mps=mps, yct=yct, c=c: e.scalar_tensor_tensor(out=cen, in0=mps, scalar=-1.0, in1=yct[:, c, :], op0=ALU.mult, op1=ALU.add), [mpb, ycb], [cenb])
                sq32, sq32b = f32_r.next()
                POOL(lambda e, sq32=sq32, cen=cen: e.tensor_tensor(out=sq32, in0=cen, in1=cen, op=ALU.mult), [cenb], [sq32b])
                vps, vpb = pr.next()
                MM(vps, bdm, sq32, True, True, [Bc, sq32b], vpb)
                tmp, tmpb = f32_r.next()
                rv_, rvb = f32_r.next()
                rstd_from_ps(vps, vpb, 1.0, rv_, rvb, tmp, tmpb)
                gcol = gsb[:, l, 21 + c:22 + c]
                DVE(lambda e, cen=cen, rv_=rv_, gcol=gcol: e.scalar_tensor_tensor(out=cen, in0=cen, scalar=gcol, in1=rv_, op0=ALU.mult, op1=ALU.mult), [cenb, rvb, Bg], [cenb])
                POOL(lambda e, cen=cen, sil=sil, c=c: e.tensor_tensor(out=ycg[:, c, :], in0=cen, in1=sil, op=ALU.mult), [cenb, silb], [ycgb])
            for db in range(8):
                for n in range(3):
                    ysrc, ysb_ = ((yat, yab), (ybt, ybb), (ycg, ycgb))[n]
                    wb_t, wb_b = wb_r.next()
                    P.dma("sync", wb_t, wbv[:, n, :, db * 128:(db + 1) * 128], reads=[Bw], writes=[wb_b])
                    wt, wtb = wg_r.next()
                    P.dma("sync", wt, wgv[:, :, 512 + n * 1024 + db * 128:512 + n * 1024 + (db + 1) * 128], reads=[Bw], writes=[wtb])
                    ups, upb = pr.next()
                    for k in range(4):
                        MM(ups, wb_t[:, k, :], ysrc[:, k, :], k == 0, k == 3, [wb_b, ysb_], upb)
                    gps, gpb = pr.next()
                    for k in range(8):
                        MM(gps, wt[:, k, :], hg[:, k, :], k == 0, k == 7, [wtb, hgb], gpb)
                    sg, sgb = f32_r.next()
                    ACT(lambda e, sg=sg, gps=gps: e.activation(out=sg, in_=gps, func=AF.Sigmoid), [gpb], [sgb])
                    if n == 0:
                        DVE(lambda e, ups=ups, sg=sg: e.tensor_tensor(out=macc, in0=ups, in1=sg, op=ALU.mult), [upb, sgb], [maccb])
                    else:
                        DVE(lambda e, ups=ups, sg=sg: e.tensor_tensor(out=sg, in0=ups, in1=sg, op=ALU.mult), [upb, sgb], [sgb])
                        if n == 1:
                            POOL(lambda e, sg=sg: e.tensor_tensor(out=macc, in0=macc, in1=sg, op=ALU.add), [maccb, sgb], [maccb])
                        else:
                            POOL(lambda e, sg=sg, db=db: e.tensor_tensor(out=mT[:, db, :], in0=macc, in1=sg, op=ALU.add), [maccb, sgb], [mTb])
            for ob_ in range(8):
                wt, wtb = wo_r.next()
                P.dma("sync", wt, wov[:, :, ob_ * 128:(ob_ + 1) * 128], reads=[Bw], writes=[wtb])
                ops_, opb = pr.next()
                for k in range(8):
                    MM(ops_, wt[:, k, :], mT[:, k, :], k == 0, k == 7, [wtb, mTb], opb)
                DVE(lambda e, xg=xg, ops_=ops_, ob_=ob_: e.tensor_tensor(out=xg[:, ob_, :], in0=ops_, in1=xg[:, ob_, :], op=ALU.add), [opb, bxg], [bxg])
            ps, pb = pr.next()
            for k in range(8):
                sq, bsq = sq_r.next()
                ACT(lambda e, sq=sq, xg=xg, k=k: e.activation(out=sq, in_=xg[:, k, :], func=AF.Square), [bxg], [bsq])
                MM(ps, onesb, sq, k == 0, k == 7, [bsq, Bcb], pb)
            tmp, tmpb = f32_r.next()
            rs, rsb = f32_r.next()
            rstd_from_ps(ps, pb, 1024.0, rs, rsb, tmp, tmpb)
            for k in range(8):
                DVE(lambda e, k=k, xg=xg, rs=rs: e.tensor_tensor(out=h2[:, k, :], in0=xg[:, k, :], in1=rs, op=ALU.mult), [bxg, rsb], [h2b])
            for fq in range(8):
                wt, wtb = wu_r.next()
                P.dma("sync", wt, wuv[:, :, fq * 512:(fq + 1) * 512], reads=[Bw], writes=[wtb])
                for fi in range(4):
                    f = fq * 4 + fi
                    ups, upb = pr.next()
                    for k in range(8):
                        MM(ups, wt[:, k, fi * 128:(fi + 1) * 128], h2[:, k, :], k == 0, k == 7, [wtb, h2b], upb)
                    rl, rlb = f32_r.next()
                    ACT(lambda e, rl=rl, ups=ups: e.activation(out=rl, in_=ups, func=AF.Relu), [upb], [rlb])
                    eng = DVE if f % 2 == 0 else POOL
                    eng(lambda e, rl=rl, f=f: e.tensor_tensor(out=act[:, f, :], in0=rl, in1=rl, op=ALU.mult), [rlb], [actb])
            for ob_ in range(8):
                wt, wtb = wd_r.next()
                P.dma("sync", wt, wdv[:, :, ob_ * 128:(ob_ + 1) * 128], reads=[Bw], writes=[wtb])
                ops_, opb = pr.next()
                for f in range(32):
                    MM(ops_, wt[:, f, :], act[:, f, :], f == 0, f == 31, [wtb, actb], opb)
                DVE(lambda e, xg=xg, ops_=ops_, ob_=ob_: e.tensor_tensor(out=xg[:, ob_, :], in0=ops_, in1=xg[:, ob_, :], op=ALU.add), [opb, bxg], [bxg])
            if not last:
                P.dma("gpsimd", xv[:, :, sl(g)], xg, reads=[bxg], writes=[Bx[g]])
            else:
                ps, pb = pr.next()
                for k in range(8):
                    sq, bsq = sq_r.next()
                    ACT(lambda e, sq=sq, xg=xg, k=k: e.activation(out=sq, in_=xg[:, k, :], func=AF.Square), [bxg], [bsq])
                    MM(ps, onesb, sq, k == 0, k == 7, [bsq, Bcb], pb)
                tmp, tmpb = f32_r.next()
                rs, rsb = f32_r.next()
                rstd_from_ps(ps, pb, 1024.0, rs, rsb, tmp, tmpb)
                for k in range(8):
                    gcol = gsb[:, 0, 25 + k:26 + k]
                    DVE(lambda e, k=k, xg=xg, rs=rs, gcol=gcol: e.scalar_tensor_tensor(out=xg[:, k, :], in0=xg[:, k, :], scalar=gcol, in1=rs, op0=ALU.mult, op1=ALU.mult), [bxg, rsb, Bg], [bxg])
                fin.append(P.dma("gpsimd", outT.rearrange("(k p) s -> p k s", p=128)[:, :, sl(g)], xg, reads=[bxg], writes=[Bout]))

    return nc, P


fin = []
Bout = Buf("out")


def build_and_emit(S=SEQ, depth=DEPTH, debug=()):
    global fin, Bout
    fin = []
    Bout = Buf("out")
    nc, P = build(S, depth, debug)
    if not fin:
        raise RuntimeError("no output")
    stats = P.emit(final_wait_ops=fin)
    return nc, stats


def kernel(**inputs):
    x = np.asarray(inputs["x"], np.float32)
    B, S, _ = x.shape
    positions = np.asarray(inputs["positions"]).astype(np.int32)
    com = host_layout(inputs)
    consts = build_consts()
    masks = build_masks()
    depth = com["WA"].shape[0]
    nc, _ = build_and_emit(S, depth)
    in_maps = []
    for b in range(B):
        m = dict(com)
        m["xin"] = np.ascontiguousarray(x[b].T)
        m["pos"] = np.ascontiguousarray(positions[b][None, :])
        m["consts"] = consts
        m["cmask"] = masks
        in_maps.append(m)
    res = run_bass_kernel_spmd(nc, in_maps, core_ids=list(range(B)))
    out = np.stack([np.ascontiguousarray(res.results[b]["outT"].T) for b in range(B)], 0)
    return out.astype(np.float32)
```

```python
import math
import os
import numpy as np
import concourse.bass as bass
import concourse.mybir as mybir
from concourse.bass_utils import run_bass_kernel_spmd

F32 = mybir.dt.float32
BF16 = mybir.dt.bfloat16
I32 = mybir.dt.int32
U8 = mybir.dt.uint8
ALU = mybir.AluOpType
AF = mybir.ActivationFunctionType

D = 1024
EPS = 1e-6
NCORES = 8
DEPTH = 4
SEQ = 4096
QS = 96 ** -0.5
OPT_P0ACT = int(os.environ.get('KV_P0ACT', '1'))
OPT_MLA = int(os.environ.get('KV_MLA', '1'))
OPT_SB = int(os.environ.get('KV_SB', '1'))
OPT_RET = int(os.environ.get('KV_RET', '1'))


class Buf:
    __slots__ = ("name", "last_w", "readers")

    def __init__(self, name, reg=None):
        self.name = name
        self.last_w = None
        self.readers = []
        if reg is not None:
            reg.append(self)


class Prog:
    ENGS = ("tensor", "vector", "scalar", "gpsimd", "sync")

    def __init__(self, nc, n_dma_sems=32):
        self.nc = nc
        self.ops = []
        self.n_dma_sems = n_dma_sems
        self.allbufs = []
        self.last_barrier = None

    def buf(self, name="b"):
        b = Buf(name, self.allbufs)
        b.last_w = self.last_barrier
        return b

    def op(self, eng, fn, reads=(), writes=(), dma=False):
        idx = len(self.ops)
        deps = set()
        for b in reads:
            if b.last_w is not None:
                deps.add(b.last_w)
        for b in writes:
            if b.last_w is not None:
                deps.add(b.last_w)
            deps.update(b.readers)
        deps.discard(idx)
        self.ops.append(dict(eng=eng, fn=fn, deps=deps, dma=dma, signal=False))
        for b in reads:
            b.readers.append(idx)
        for b in writes:
            b.last_w = idx
            b.readers = []
        return idx

    def dma(self, eng, out, in_, reads=(), writes=()):
        return self.op(eng, lambda e: e.dma_start(out=out, in_=in_), reads, writes, dma=True)

    def barrier(self, scratch_ap):
        bs = list(self.allbufs)
        i = self.op("vector", lambda e: e.memset(scratch_ap, 0.0), bs, bs)
        self.last_barrier = i
        return i

    def emit(self, final_wait_ops=()):
        nc = self.nc
        ops = self.ops
        for i, o in enumerate(ops):
            nd = set()
            for d in o["deps"]:
                p = ops[d]
                if (not p["dma"]) and (not o["dma"]) and p["eng"] == o["eng"] and o["eng"] == "tensor":
                    continue
                nd.add(d)
            o["deps"] = nd
            for d in nd:
                ops[d]["signal"] = True
        for d in final_wait_ops:
            ops[d]["signal"] = True
        eng_sem = {e: nc.alloc_semaphore(name=f"s_{e}") for e in self.ENGS}
        dma_sems = [nc.alloc_semaphore(name=f"d_{i}") for i in range(self.n_dma_sems)]
        cnt = {e: 0 for e in self.ENGS}
        dma_cnt = [0] * self.n_dma_sems
        dma_rr = 0
        for o in ops:
            if o["dma"]:
                s = dma_rr % self.n_dma_sems
                dma_rr += 1
                o["prev_val"] = dma_cnt[s]
                dma_cnt[s] += 16
                o["sem"] = ("d", s)
                o["val"] = dma_cnt[s]
            elif o["signal"]:
                cnt[o["eng"]] += 1
                o["sem"] = ("e", o["eng"])
                o["val"] = cnt[o["eng"]]
        per_eng = {e: [] for e in self.ENGS}
        for i, o in enumerate(ops):
            per_eng[o["eng"]].append(i)

        def semof(key):
            return dma_sems[key[1]] if key[0] == "d" else eng_sem[key[1]]

        def run_engine(ename, eng, final=False):
            waited = {}
            for i in per_eng[ename]:
                o = ops[i]
                need = {}
                for d in o["deps"]:
                    p = ops[d]
                    k = p["sem"]
                    if p["val"] > need.get(k, 0):
                        need[k] = p["val"]
                if o["dma"] and o["prev_val"] > 0:
                    k = o["sem"]
                    need[k] = max(need.get(k, 0), o["prev_val"])
                for k, v in need.items():
                    if waited.get(k, 0) < v:
                        eng.wait_ge(semof(k), v)
                        waited[k] = v
                ins = o["fn"](eng)
                if o["dma"]:
                    ins.then_inc(semof(o["sem"]), 16)
                elif o["signal"]:
                    ins.then_inc(semof(o["sem"]), 1)
            if final:
                need = {}
                for d in final_wait_ops:
                    p = ops[d]
                    need[p["sem"]] = max(need.get(p["sem"], 0), p["val"])
                for k, v in need.items():
                    if waited.get(k, 0) < v:
                        eng.wait_ge(semof(k), v)

        with nc.Block() as block:
            @block.tensor
            def _(e):
                run_engine("tensor", e)

            @block.vector
            def _(e):
                run_engine("vector", e)

            @block.scalar
            def _(e):
                run_engine("scalar", e)

            @block.gpsimd
            def _(e):
                run_engine("gpsimd", e)

            @block.sync
            def _(e):
                run_engine("sync", e, final=True)
        return {e: len(per_eng[e]) for e in self.ENGS}


class Arena:
    def __init__(self, ap_u8, P):
        self.ap = ap_u8
        self.size = ap_u8.shape[1]
        self.off = 0
        self.P = P

    def reset(self):
        self.off = 0

    def tile(self, free_shape, dtype, name="t"):
        esz = 2 if dtype == BF16 else 4
        n = 1
        for s in free_shape:
            n *= s
        nbytes = (n * esz + 31) // 32 * 32
        assert self.off + nbytes <= self.size, (name, self.off, nbytes, self.size)
        v = self.ap[:, self.off:self.off + nbytes]
        self.off += nbytes
        v = v[:, 0:n * esz].bitcast(dtype)
        if len(free_shape) == 2:
            v = v.rearrange("p (a b) -> p a b", b=free_shape[1])
        elif len(free_shape) == 3:
            v = v.rearrange("p (a b c) -> p a b c", b=free_shape[1], c=free_shape[2])
        return v, self.P.buf(name)


class Rot:
    def __init__(self, arena, n, free_shape, dtype, name="r"):
        self.items = [arena.tile(free_shape, dtype, f"{name}{i}") for i in range(n)]
        self.i = 0

    def next(self):
        it = self.items[self.i % len(self.items)]
        self.i += 1
        return it


CONST_LAYOUT = {}


def build_consts():
    cols = []

    def add(name, arr):
        arr = np.asarray(arr, np.float64).reshape(128, -1)
        off = sum(c.shape[1] for c in cols)
        CONST_LAYOUT[name] = (off, arr.shape[1])
        cols.append(arr)

    bd = np.zeros((128, 128))
    bd[0:64, 0:64] = 1.0 / 64
    bd[64:128, 64:128] = 1.0 / 64
    add("bd", bd)
    h = np.arange(8)
    lg = np.log1p(-np.exp2(-5.0 - h).astype(np.float32)).astype(np.float32).astype(np.float64)
    m = np.arange(128)[:, None]
    c = np.arange(128)[None, :]
    dt = np.stack([np.where(c >= m, np.exp(np.maximum(c - m, 0) * lg[hh]), 0.0) for hh in range(8)], 1)
    add("dt", dt)
    z = np.zeros((128, 4, 128))
    for pr in range(4):
        for col in range(128):
            hh = 2 * pr + col // 64
            z[:, pr, col] = np.exp((127 - np.arange(128)) * lg[hh])
    add("zeta", z)
    xi = np.zeros((128, 8, 128))
    for hh in range(8):
        xi[:, hh, :] = np.exp((np.arange(128) + 1.0) * lg[hh])[None, :]
    add("xi", xi)
    r = np.arange(128)
    invf_r = (10000.0 ** (-(2.0 * (r % 32)) / 64)).astype(np.float32)
    invf_m = (10000.0 ** (-(2.0 * (r % 16)) / 32)).astype(np.float32)
    add("invf", np.stack([invf_r, invf_m], 1))
    sg_r = np.where((r % 64) < 32, -1.0, 1.0)
    sg_m = np.where((r % 32) < 16, -1.0, 1.0)
    add("sgn", np.stack([sg_r, sg_m], 1))
    cd = np.exp(128.0 * lg)
    add("cdecay", np.tile(cd[None, :], (128, 1)))
    return np.concatenate(cols, 1).astype(np.float32)


IN_OFFS = dict(cq=(0, 384), ckv=(384, 640), kpe=(640, 672), sbq=(672, 1184), sbk=(1184, 1696), sbv=(1696, 2208),
               rq=(2208, 2720), rk=(2720, 3232), rv=(3232, 3744), rg=(3744, 4256), gate=(4256, 7328))
WA_COLS = 3840
WA_OFF = dict(cq=0, ckv=384, sbq=640, sbk=1152, rq=1664, rqs=2176, rk=2688, rks=3200, kpe=3712, kpes=3744)


def build_masks():
    p = np.arange(128)[:, None]
    j = np.arange(512)[None, :]
    mA = np.stack([(j >= p + 128 * d) for d in range(4)], 1).astype(np.float32).reshape(128, -1)
    mS = np.stack([(j > p + 128 * d) for d in range(4)], 1).astype(np.float32).reshape(128, -1)
    jj = np.arange(128)[:, None]
    ss = np.arange(128)[None, :]
    tri = -(jj >= ss).astype(np.float32)
    return np.concatenate([mA, mS, tri, np.eye(128, dtype=np.float32)], 1)


def host_layout(inputs):
    w_in = np.asarray(inputs["w_in"])
    depth = w_in.shape[0]

    def sl(name):
        a, b = IN_OFFS[name]
        return w_in[:, :, a:b]

    def swap_heads(w, hd):
        L, K, N = w.shape
        w4 = w.reshape(L, K, N // hd, hd)
        return np.concatenate([w4[..., hd // 2:], w4[..., :hd // 2]], -1).reshape(L, K, N)

    WA = np.concatenate([sl("cq"), sl("ckv"), sl("sbq"), sl("sbk"), sl("rq"), swap_heads(sl("rq"), 64),
                         sl("rk"), swap_heads(sl("rk"), 64), sl("kpe"), swap_heads(sl("kpe"), 32)], -1)
    WV = np.concatenate([sl("sbv"), sl("rv")], -1)
    WG = np.concatenate([sl("rg"), sl("gate")], -1)
    wuq = np.asarray(inputs["mla_w_uq"]).reshape(depth, 384, 8, 96)
    qn = wuq[..., :64].reshape(depth, 384, 512)
    qr = wuq[..., 64:].reshape(depth, 384, 256)
    WQ = np.concatenate([qn, qr, swap_heads(qr, 32)], -1)
    wukv = np.asarray(inputs["mla_w_ukv"]).reshape(depth, 256, 8, 128)
    WKV = np.concatenate([wukv[..., :64].reshape(depth, 256, 512), wukv[..., 64:].reshape(depth, 256, 512)], -1)
    WB = np.asarray(inputs["w_branch"]).reshape(depth, 1536, 1024)

    def g128(v):
        L, N = v.shape
        return v.reshape(L, N // 128, 128).transpose(0, 2, 1)

    gains = np.concatenate([g128(np.asarray(inputs["norm_mix_g"])), g128(np.asarray(inputs["mla_q_norm_g"])),
                            g128(np.asarray(inputs["mla_kv_norm_g"])), g128(np.asarray(inputs["norm_mlp_g"])),
                            g128(np.asarray(inputs["ret_norm_g"])),
                            np.broadcast_to(g128(np.asarray(inputs["final_norm_g"])[None]), (depth, 128, 8))], -1)
    com = dict(WA=WA, WV=WV, WG=WG, WQ=WQ, WKV=WKV, WB=WB, WO=np.asarray(inputs["w_out"]),
               WU=np.asarray(inputs["w_up"]), WD=np.asarray(inputs["w_down"]), gains=gains)
    return {k: np.ascontiguousarray(v, dtype=np.float32) for k, v in com.items()}


WSHAPES = dict(WA=(1024, 3776), WV=(1024, 1024), WG=(1024, 3584), WQ=(384, 1024), WKV=(256, 1024),
               WB=(1536, 1024), WO=(1024, 1024), WU=(1024, 4096), WD=(4096, 1024))
WGAIN = dict(WA=0, WV=0, WG=0, WQ=8, WKV=11, WU=13)


def build(S=SEQ, depth=DEPTH, debug=()):
    NG = S // 512
    NB = S // 128
    nc = bass.Bass("TRN2", target_bir_lowering=False)
    P = Prog(nc)
    consts_np = build_consts()
    NCONST = consts_np.shape[1]

    def dram(name, shape, dt, kind=None):
        if kind is None:
            kind = "ExternalOutput" if name in debug else "Internal"
        return nc.dram_tensor(name, list(shape), dt, kind=kind).ap()

    xin = dram("xin", [D, S], F32, "ExternalInput")
    pos = dram("pos", [1, S], I32, "ExternalInput")
    cin = dram("consts", [128, NCONST], F32, "ExternalInput")
    cmin = dram("cmask", [128, 4352], F32, "ExternalInput")
    gin = dram("gains", [depth, 128, 33], F32, "ExternalInput")
    Win = {k: dram(k, [depth] + list(s), F32, "ExternalInput") for k, s in WSHAPES.items()}
    outT = dram("outT", [D, S], F32, "ExternalOutput")
    Wb = {k: dram("b" + k, [depth] + list(s), BF16) for k, s in WSHAPES.items()}
    xres = dram("xres", [D, S], F32)
    tabR = dram("tabR", [128, 2, S], F32)
    tabM = dram("tabM", [128, 2, S], F32)
    d_sq = dram("d_sq", [512, S], BF16)
    d_sk = dram("d_sk", [512, S], BF16)
    d_sv = dram("d_sv", [S, 512], BF16)
    d_rq = dram("d_rq", [512, S], BF16)
    d_rk = dram("d_rk", [512, S], BF16)
    d_rv = dram("d_rv", [S, 512], BF16)
    d_krot = dram("d_krot", [32, S], BF16)
    d_qn = dram("d_qn", [512, S], BF16)
    d_qr = dram("d_qr", [256, S], BF16)
    d_kn = dram("d_kn", [512, S], BF16)
    d_va = dram("d_va", [S, 8, 65], BF16)
    d_ya = dram("d_ya", [512, S], BF16)
    d_yb = dram("d_yb", [512, S], BF16)
    d_yc = dram("d_yc", [512, S], F32)
    Bx = [P.buf(f"x{g}") for g in range(NG)]
    Bw = P.buf("wb16")
    Btab = P.buf("tab")
    Bd = {n: P.buf(n) for n in "sq sk sv rq rk rv krot qn qr kn va ya yb yc".split()}

    cst = nc.alloc_sbuf_tensor("cst", [128, NCONST], F32)
    Bc = P.buf("cst")
    gsb = nc.alloc_sbuf_tensor("gsb", [128, depth, 33], F32)
    Bg = P.buf("gsb")
    PERS_BF = 4 * 512 * 2 + 128 * 4
    cb = nc.alloc_sbuf_tensor("cb", [128, 4 * 512 * 2 + 128 * 3], BF16)
    Bcb = P.buf("cb")
    ones32 = nc.alloc_sbuf_tensor("ones32", [128, 128], F32)
    scr = nc.alloc_sbuf_tensor("scr", [128, 8], F32)
    ARENA_BYTES = 186 * 1024
    arena_t = nc.alloc_sbuf_tensor("arena", [128, ARENA_BYTES], U8)
    A = Arena(arena_t[:, :], P)
    psb = [nc.alloc_psum_tensor(f"ps{i}", [128, 512], F32)[:, :] for i in range(8)]
    PB = [P.buf(f"ps{i}") for i in range(8)]

    class PsRot:
        def __init__(self, idxs):
            self.idxs = idxs
            self.i = 0

        def next(self):
            k = self.idxs[self.i % len(self.idxs)]
            self.i += 1
            return psb[k], PB[k]

    def C(name):
        o, n = CONST_LAYOUT[name]
        return cst[:, o:o + n]

    maskA = cb[:, 0:2048].rearrange("p (d j) -> p d j", j=512)
    maskS = cb[:, 2048:4096].rearrange("p (d j) -> p d j", j=512)
    trineg = cb[:, 4096:4224]
    identb = cb[:, 4224:4352]
    onesb = cb[:, 4352:4480]

    P.dma("sync", cst[:], cin, writes=[Bc])
    P.dma("sync", gsb[:], gin.rearrange("l p c -> p l c"), writes=[Bg])
    A.reset()
    cm_t, cm_b = A.tile([4352], F32, "cmask")
    P.dma("sync", cm_t, cmin, writes=[cm_b])
    P.op("vector", lambda e: e.tensor_copy(out=cb[:, 0:4352], in_=cm_t), [cm_b], [Bcb])
    P.barrier(scr[:, 0:1])
    P.op("vector", lambda e: e.memset(onesb, 1.0), [], [Bcb])
    P.op("vector", lambda e: e.memset(ones32[:], 1.0), [], [Bcb])
    P.dma("sync", xres, xin, writes=Bx)

    A.reset()
    TW = min(S, 2048)
    pi_t, b_pi = A.tile([TW], I32, "pi")
    pf_t, b_pf = A.tile([TW], F32, "pf")
    ang_t, _ = A.tile([TW], F32, "ang")
    t4_t, _ = A.tile([TW], F32, "t4")
    ki_t, _ = A.tile([TW], I32, "ki")
    kf_t, _ = A.tile([TW], F32, "kf")
    mk_t, _ = A.tile([TW], F32, "mk")
    tb_t, b_tb = A.tile([2, TW], F32, "tabst")
    C1 = 6.28125
    C2 = 2 * math.pi - C1
    bt = b_pf

    def reduce_and_sin(shift, out_ap, post_sign_col):
        V = lambda f: P.op("vector", f, [bt, Bc], [bt])
        V(lambda e: e.tensor_scalar(out=ang_t, in0=ang_t, scalar1=float(shift), scalar2=None, op0=ALU.add))
        V(lambda e: e.tensor_scalar(out=t4_t, in0=ang_t, scalar1=1.0 / (2 * math.pi), scalar2=0.5, op0=ALU.mult, op1=ALU.add))
        V(lambda e: e.tensor_copy(out=ki_t, in_=t4_t))
        V(lambda e: e.tensor_copy(out=kf_t, in_=ki_t))
        V(lambda e: e.scalar_tensor_tensor(out=t4_t, in0=kf_t, scalar=-C1, in1=ang_t, op0=ALU.mult, op1=ALU.add))
        V(lambda e: e.scalar_tensor_tensor(out=t4_t, in0=kf_t, scalar=-C2, in1=t4_t, op0=ALU.mult, op1=ALU.add))
        V(lambda e: e.tensor_scalar(out=mk_t, in0=t4_t, scalar1=-math.pi, scalar2=2 * math.pi, op0=ALU.is_lt, op1=ALU.mult))
        V(lambda e: e.tensor_tensor(out=t4_t, in0=t4_t, in1=mk_t, op=ALU.add))
        V(lambda e: e.tensor_scalar(out=mk_t, in0=t4_t, scalar1=math.pi, scalar2=-2 * math.pi, op0=ALU.is_gt, op1=ALU.mult))
        V(lambda e: e.tensor_tensor(out=t4_t, in0=t4_t, in1=mk_t, op=ALU.add))
        V(lambda e: e.tensor_scalar(out=t4_t, in0=t4_t, scalar1=-3.1415925, scalar2=3.1415925, op0=ALU.max, op1=ALU.min))
        P.op("scalar", lambda e: e.activation(out=out_ap, in_=t4_t, func=AF.Sin), [bt], [b_tb])
        if post_sign_col is not None:
            P.op("vector", lambda e: e.tensor_scalar(out=out_ap, in0=out_ap, scalar1=post_sign_col, scalar2=None, op0=ALU.mult), [b_tb, Bc], [b_tb])

    for which, tab in ((0, tabR), (1, tabM)):
        for t0 in range(0, S, TW):
            P.dma("sync", pi_t, pos[:, t0:t0 + TW].partition_broadcast(128), writes=[b_pi])
            P.op("vector", lambda e: e.tensor_copy(out=pf_t, in_=pi_t), [b_pi], [bt])
            invc = C("invf")[:, which:which + 1]
            sgc = C("sgn")[:, which:which + 1]
            P.op("vector", lambda e, invc=invc: e.tensor_scalar(out=ang_t, in0=pf_t, scalar1=invc, scalar2=None, op0=ALU.mult), [bt, Bc], [bt])
            reduce_and_sin(math.pi / 2, tb_t[:, 0, :], None)
            P.op("vector", lambda e, invc=invc: e.tensor_scalar(out=ang_t, in0=pf_t, scalar1=invc, scalar2=None, op0=ALU.mult), [bt, Bc], [bt])
            reduce_and_sin(0.0, tb_t[:, 1, :], sgc)
            P.dma("gpsimd", tab[:, :, t0:t0 + TW], tb_t, reads=[b_tb], writes=[Btab])

    P.barrier(scr[:, 0:1])
    A.reset()
    PW = 2048
    ldr = Rot(A, 6, [PW], F32, "wld")
    cvr = Rot(A, 6, [PW], BF16, "wcv")
    cnt = 0
    for l in range(depth):
        for k, (K, N) in WSHAPES.items():
            goff = WGAIN.get(k)
            for kc in range(K // 128):
                for c0 in range(0, N, PW):
                    n = min(PW, N - c0)
                    lt, lb = ldr.next()
                    ct, cbf = cvr.next()
                    P.dma("sync", lt[:, 0:n], Win[k][l, kc * 128:(kc + 1) * 128, c0:c0 + n], writes=[lb])
                    useact = (cnt % 2 == 1) and OPT_P0ACT
                    cnt += 1
                    if goff is None:
                        if useact:
                            P.op("scalar", lambda e, ct=ct, lt=lt, n=n: e.copy(out=ct[:, 0:n], in_=lt[:, 0:n]), [lb], [cbf])
                        else:
                            P.op("vector", lambda e, ct=ct, lt=lt, n=n: e.tensor_copy(out=ct[:, 0:n], in_=lt[:, 0:n]), [lb], [cbf])
                    else:
                        gcol = gsb[:, l, goff + kc:goff + kc + 1]
                        if useact:
                            P.op("scalar", lambda e, ct=ct, lt=lt, n=n, gcol=gcol: e.mul(out=ct[:, 0:n], in_=lt[:, 0:n], mul=gcol), [lb, Bg], [cbf])
                        else:
                            P.op("vector", lambda e, ct=ct, lt=lt, n=n, gcol=gcol: e.tensor_scalar(out=ct[:, 0:n], in0=lt[:, 0:n], scalar1=gcol, scalar2=None, op0=ALU.mult), [lb, Bg], [cbf])
                    P.dma("gpsimd", Wb[k][l, kc * 128:(kc + 1) * 128, c0:c0 + n], ct[:, 0:n], reads=[cbf], writes=[Bw])

    def wview(k, l):
        return Wb[k][l].rearrange("(k p) n -> p k n", p=128)

    def ACT(fn, r, w):
        return P.op("scalar", fn, r, w)

    def DVE(fn, r, w):
        return P.op("vector", fn, r, w)

    def POOL(fn, r, w):
        return P.op("gpsimd", fn, r, w)

    def MM(ps, lhsT, rhs, start, stop, r, w):
        return P.op("tensor", lambda e: e.matmul(ps, lhsT=lhsT, rhs=rhs, start=start, stop=stop), r, [w])

    def rstd_from_ps(ps, pb, n, out_ap, out_b, tmp, tmpb):
        ACT(lambda e: e.activation(out=tmp, in_=ps, func=AF.Ln, bias=EPS, scale=1.0 / n), [pb], [tmpb])
        ACT(lambda e: e.activation(out=out_ap, in_=tmp, func=AF.Exp, scale=-0.5), [tmpb], [out_b])

    xv = xres.rearrange("(k p) s -> p k s", p=128)

    for l in range(depth):
        P.barrier(scr[:, 0:1])
        A.reset()
        cqT, b_cq = A.tile([3, S], BF16, "cqT")
        ckvT, b_ckv = A.tile([2, S], BF16, "ckvT")
        hT, b_h = A.tile([8, S], BF16, "hT")
        Bh = [P.buf(f"h{g}") for g in range(NG)]
        xg_r = Rot(A, 1, [8, 512], F32, "xg")
        sq_r = Rot(A, 3, [512], BF16, "sq")
        f32_r = Rot(A, 6, [512], F32, "f32")
        w_r = Rot(A, 2, [8, 256], BF16, "wblk")
        wv_t, b_wv = A.tile([8, 512], BF16, "wv")
        st_r = Rot(A, 2, [S], BF16, "stage")
        tab_r = Rot(A, 2, [2, 512], F32, "tab")
        vst_r = Rot(A, 2, [4, 512], BF16, "vst")
        pr = PsRot([0, 1, 2, 3, 4, 5, 6, 7])

        def sl(g):
            return slice(g * 512, (g + 1) * 512)

        for g in range(NG):
            xg, bxg = xg_r.next()
            P.dma("sync", xg, xv[:, :, sl(g)], reads=[Bx[g]], writes=[bxg])
            ps, pb = pr.next()
            for k in range(8):
                sq, bsq = sq_r.next()
                ACT(lambda e, sq=sq, xg=xg, k=k: e.activation(out=sq, in_=xg[:, k, :], func=AF.Square), [bxg], [bsq])
                MM(ps, onesb, sq, k == 0, k == 7, [bsq, Bcb], pb)
            tmp, tmpb = f32_r.next()
            rs, rsb = f32_r.next()
            rstd_from_ps(ps, pb, 1024.0, rs, rsb, tmp, tmpb)
            for k in range(8):
                DVE(lambda e, k=k, xg=xg, rs=rs, g=g: e.tensor_tensor(out=hT[:, k, sl(g)], in0=xg[:, k, :], in1=rs, op=ALU.mult), [bxg, rsb], [Bh[g]])

        wa = wview("WA", l)

        def fm_block(c0, M, n_mm=1, c1=None):
            wt, wtb = w_r.next()
            P.dma("sync", wt[:, :, 0:M], wa[:, :, c0:c0 + M], reads=[Bw], writes=[wtb])
            if c1 is not None:
                P.dma("sync", wt[:, :, 128:128 + M], wa[:, :, c1:c1 + M], reads=[Bw], writes=[wtb])
            return wt, wtb

        def fm_mm(wt, wtb, off, M, g):
            ps, pb = pr.next()
            for k in range(8):
                MM(ps[0:M, :], wt[:, k, off:off + M], hT[:, k, sl(g)], k == 0, k == 7, [wtb, Bh[g]], pb)
            return ps, pb

        for name, dst, dstb, nblk in (("cq", cqT, b_cq, 3), ("ckv", ckvT, b_ckv, 2)):
            for c in range(nblk):
                wt, wtb = fm_block(WA_OFF[name] + c * 128, 128)
                for g in range(NG):
                    ps, pb = fm_mm(wt, wtb, 0, 128, g)
                    ACT(lambda e, ps=ps, dst=dst, c=c, g=g: e.copy(out=dst[:, c, sl(g)], in_=ps), [pb], [dstb])
        for name, dd, dbuf, scale in (("sbq", d_sq, Bd["sq"], 0.125), ("sbk", d_sk, Bd["sk"], 1.0)):
            for c in range(4):
                wt, wtb = fm_block(WA_OFF[name] + c * 128, 128)
                st, stb = st_r.next()
                for g in range(NG):
                    ps, pb = fm_mm(wt, wtb, 0, 128, g)
                    ACT(lambda e, ps=ps, st=st, g=g, scale=scale: e.mul(out=st[:, sl(g)], in_=ps, mul=scale), [pb], [stb])
                P.dma("gpsimd", dd[c * 128:(c + 1) * 128, :], st, reads=[stb], writes=[dbuf])
        for name, sname, dd, dbuf, scale, M, tab, nblk in (
                ("rq", "rqs", d_rq, Bd["rq"], 1.0, 128, tabR, 4), ("rk", "rks", d_rk, Bd["rk"], 0.125, 128, tabR, 4),
                ("kpe", "kpes", d_krot, Bd["krot"], 1.0, 32, tabM, 1)):
            for c in range(nblk):
                wt, wtb = fm_block(WA_OFF[name] + c * 128, M, c1=WA_OFF[sname] + c * 128)
                st, stb = st_r.next()
                for g in range(NG):
                    tb, tbb = tab_r.next()
                    P.dma("sync", tb[0:M], tab[0:M, :, sl(g)], reads=[Btab], writes=[tbb])
                    ps, pb = fm_mm(wt, wtb, 0, M, g)
                    ps2, pb2 = fm_mm(wt, wtb, 128, M, g)
                    t1, t1b = f32_r.next()
                    t2, t2b = f32_r.next()
                    DVE(lambda e, ps=ps, tb=tb, t1=t1, M=M, scale=scale: e.scalar_tensor_tensor(out=t1[0:M], in0=ps[0:M, :], scalar=scale, in1=tb[0:M, 0, :], op0=ALU.mult, op1=ALU.mult), [pb, tbb], [t1b])
                    DVE(lambda e, ps2=ps2, tb=tb, t2=t2, M=M, scale=scale: e.scalar_tensor_tensor(out=t2[0:M], in0=ps2[0:M, :], scalar=scale, in1=tb[0:M, 1, :], op0=ALU.mult, op1=ALU.mult), [pb2, tbb], [t2b])
                    POOL(lambda e, st=st, t1=t1, t2=t2, g=g, M=M: e.tensor_tensor(out=st[0:M, sl(g)], in0=t1[0:M], in1=t2[0:M], op=ALU.add), [t1b, t2b], [stb])
                P.dma("gpsimd", dd[c * 128:c * 128 + M, :], st[0:M], reads=[stb], writes=[dbuf])
        wvv = wview("WV", l)
        for vi, (dd, dbuf) in enumerate(((d_sv, Bd["sv"]), (d_rv, Bd["rv"]))):
            P.dma("sync", wv_t, wvv[:, :, vi * 512:(vi + 1) * 512], reads=[Bw], writes=[b_wv])
            ddv = dd.rearrange("(b p) c -> p b c", p=128)
            for g in range(NG):
                vs, vsb = vst_r.next()
                for j in range(4):
                    tbi = g * 4 + j
                    ps, pb = pr.next()
                    for k in range(8):
                        MM(ps, hT[:, k, tbi * 128:(tbi + 1) * 128], wv_t[:, k, :], k == 0, k == 7, [Bh[g], b_wv], pb)
                    if j % 2 == 0:
                        ACT(lambda e, ps=ps, vs=vs, j=j: e.copy(out=vs[:, j, :], in_=ps), [pb], [vsb])
                    else:
                        DVE(lambda e, ps=ps, vs=vs, j=j: e.tensor_copy(out=vs[:, j, :], in_=ps), [pb], [vsb])
                P.dma("gpsimd", ddv[:, g * 4:(g + 1) * 4, :], vs, reads=[vsb], writes=[dbuf])

        P.barrier(scr[:, 0:1])
        A.reset()
        cqT, b_cq = A.tile([3, S], BF16, "cqT")
        ckvT, b_ckv = A.tile([2, S], BF16, "ckvT")
        sq_r = Rot(A, 4, [512], BF16, "sq")
        f32_r = Rot(A, 8, [512], F32, "f32")
        tab_r = Rot(A, 2, [2, 512], F32, "tab")
        wq_t, b_wq = A.tile([3, 1024], BF16, "wq")
        wkv_t, b_wkv = A.tile([2, 1024], BF16, "wkv")
        ost_r = Rot(A, 4, [512], BF16, "ost")
        vaug_r = Rot(A, 2, [4, 8, 65], BF16, "vaug")
        rtok_r = Rot(A, 2, [4], F32, "rtok")
        P.dma("sync", wq_t, wview("WQ", l), reads=[Bw], writes=[b_wq])
        P.dma("sync", wkv_t, wview("WKV", l), reads=[Bw], writes=[b_wkv])
        for it in vaug_r.items:
            DVE(lambda e, t=it[0]: e.memset(t[:, :, :, 64:65], 1.0), [], [it[1]])
        vav = d_va.rearrange("(b p) h e -> p b (h e)", p=128)
        for g in range(NG):
            tbm, tbmb = tab_r.next()
            P.dma("sync", tbm, tabM[:, :, sl(g)], reads=[Btab], writes=[tbmb])
            ps, pb = pr.next()
            for k in range(3):
                sq, bsq = sq_r.next()
                POOL(lambda e, sq=sq, k=k, g=g: e.tensor_tensor(out=sq, in0=cqT[:, k, sl(g)], in1=cqT[:, k, sl(g)], op=ALU.mult), [b_cq], [bsq])
                MM(ps, onesb, sq, k == 0, k == 2, [bsq, Bcb], pb)
            tmp, tmpb = f32_r.next()
            rq_, rqb = f32_r.next()
            rstd_from_ps(ps, pb, 384.0, rq_, rqb, tmp, tmpb)
            ps, pb = pr.next()
            ps_t, pb_t = pr.next()
            sqs = []
            for k in range(2):
                sq, bsq = sq_r.next()
                POOL(lambda e, sq=sq, k=k, g=g: e.tensor_tensor(out=sq, in0=ckvT[:, k, sl(g)], in1=ckvT[:, k, sl(g)], op=ALU.mult), [b_ckv], [bsq])
                MM(ps, onesb, sq, k == 0, k == 1, [bsq, Bcb], pb)
                sqs.append((sq, bsq))
            for j in range(4):
                for k in range(2):
                    MM(ps_t[:, j:j + 1], sqs[k][0][:, j * 128:(j + 1) * 128], onesb[:, 0:1], k == 0, k == 1, [sqs[k][1], Bcb], pb_t)
            tmp2, tmp2b = f32_r.next()
            rkv, rkvb = f32_r.next()
            rstd_from_ps(ps, pb, 256.0, rkv, rkvb, tmp2, tmp2b)
            rt, rtb = rtok_r.next()
            ACT(lambda e, rt=rt, ps_t=ps_t: e.activation(out=rt, in_=ps_t[:, 0:4], func=AF.Ln, bias=EPS, scale=1.0 / 256.0), [pb_t], [rtb])
            ACT(lambda e, rt=rt: e.activation(out=rt, in_=rt, func=AF.Exp, scale=-0.5), [rtb], [rtb])
            for c in range(4):
                ps, pb = pr.next()
                for k in range(3):
                    MM(ps, wq_t[:, k, c * 128:(c + 1) * 128], cqT[:, k, sl(g)], k == 0, k == 2, [b_wq, b_cq], pb)
                o, ob = ost_r.next()
                DVE(lambda e, o=o, ps=ps, rq_=rq_: e.scalar_tensor_tensor(out=o, in0=ps, scalar=QS, in1=rq_, op0=ALU.mult, op1=ALU.mult), [pb, rqb], [ob])
                P.dma("gpsimd", d_qn[c * 128:(c + 1) * 128, sl(g)], o, reads=[ob], writes=[Bd["qn"]])
            for c in range(2):
                ps, pb = pr.next()
                ps2, pb2 = pr.next()
                for k in range(3):
                    MM(ps, wq_t[:, k, 512 + c * 128:512 + (c + 1) * 128], cqT[:, k, sl(g)], k == 0, k == 2, [b_wq, b_cq], pb)
                for k in range(3):
                    MM(ps2, wq_t[:, k, 768 + c * 128:768 + (c + 1) * 128], cqT[:, k, sl(g)], k == 0, k == 2, [b_wq, b_cq], pb2)
                t1, t1b = f32_r.next()
                t2, t2b = f32_r.next()
                DVE(lambda e, ps=ps, tbm=tbm, t1=t1: e.scalar_tensor_tensor(out=t1, in0=ps, scalar=QS, in1=tbm[:, 0, :], op0=ALU.mult, op1=ALU.mult), [pb, tbmb], [t1b])
                DVE(lambda e, ps2=ps2, tbm=tbm, t2=t2: e.scalar_tensor_tensor(out=t2, in0=ps2, scalar=QS, in1=tbm[:, 1, :], op0=ALU.mult, op1=ALU.mult), [pb2, tbmb], [t2b])
                POOL(lambda e, t1=t1, t2=t2: e.tensor_tensor(out=t1, in0=t1, in1=t2, op=ALU.add), [t1b, t2b], [t1b])
                o, ob = ost_r.next()
                POOL(lambda e, o=o, t1=t1, rq_=rq_: e.tensor_tensor(out=o, in0=t1, in1=rq_, op=ALU.mult), [t1b, rqb], [ob])
                P.dma("gpsimd", d_qr[c * 128:(c + 1) * 128, sl(g)], o, reads=[ob], writes=[Bd["qr"]])
            for c in range(4):
                ps, pb = pr.next()
                for k in range(2):
                    MM(ps, wkv_t[:, k, c * 128:(c + 1) * 128], ckvT[:, k, sl(g)], k == 0, k == 1, [b_wkv, b_ckv], pb)
                o, ob = ost_r.next()
                DVE(lambda e, o=o, ps=ps, rkv=rkv: e.tensor_tensor(out=o, in0=ps, in1=rkv, op=ALU.mult), [pb, rkvb], [ob])
                P.dma("gpsimd", d_kn[c * 128:(c + 1) * 128, sl(g)], o, reads=[ob], writes=[Bd["kn"]])
            va, vab = vaug_r.next()
            for j in range(4):
                tbi = g * 4 + j
                ps, pb = pr.next()
                for k in range(2):
                    MM(ps, ckvT[:, k, tbi * 128:(tbi + 1) * 128], wkv_t[:, k, 512:1024], k == 0, k == 1, [b_ckv, b_wkv], pb)
                DVE(lambda e, va=va, ps=ps, rt=rt, j=j: e.tensor_scalar(out=va[:, j, :, 0:64], in0=ps.rearrange("p (h e) -> p h e", e=64), scalar1=rt[:, j:j + 1], scalar2=None, op0=ALU.mult), [pb, rtb], [vab])
            P.dma("gpsimd", vav[:, g * 4:(g + 1) * 4, :], va.rearrange("p j h e -> p j (h e)"), reads=[vab], writes=[Bd["va"]])

        P.barrier(scr[:, 0:1])
        A.reset()
        q_r = Rot(A, 2, [S], BF16, "q")
        k_r = Rot(A, 2, [S], BF16, "k")
        v_r = Rot(A, 2, [NB, 128], BF16, "v")
        p_r = Rot(A, 6, [512], BF16, "p")
        e_r = Rot(A, 4, [512], F32, "e")
        t_r = Rot(A, 4, [512], F32, "t")
        lp_r = Rot(A, 4, [512], BF16, "lp")
        csb_r = Rot(A, 2, [512], F32, "csb")
        ys_r = Rot(A, 2, [S], BF16, "ys")
        rden, rdenb = A.tile([512], F32, "rden")
        bcs, bcsb = A.tile([512], F32, "bcs")
        ycs_r = Rot(A, 4, [512], F32, "ycs")
        st_t, st_b = A.tile([64], F32, "state")
        prevb_t, prevb_b = A.tile([64], BF16, "prevb")
        ktok_r = Rot(A, 3, [128], BF16, "ktok")
        sT_r = Rot(A, 6, [128], BF16, "sT")
        tmpc_r = Rot(A, 4, [128], F32, "tmpc")
        cld_r = Rot(A, 2, [2048], F32, "cld")
        ccv_r = Rot(A, 2, [2048], BF16, "ccv")

        def pipeline(n, stages):
            maxs = max(sk for _, sk in stages)
            for step in range(n + maxs):
                for fn, sk in stages:
                    i = step - sk
                    if 0 <= i < n:
                        fn(i)

        tiles_fwd = [(g, kb) for g in range(NG) for kb in range(4 * (g + 1))]
        tiles_rev = [(g, kb) for g in range(NG) for kb in range(4 * (g + 1) - 1, -1, -1)]
        ps_s = PsRot([0, 1, 2, 3])
        ps_y = PsRot([4, 5])
        ps_c = PsRot([6, 7])
        vav4 = d_va.rearrange("(b p) h e -> p b h e", p=128)
        for h in range(8):
            qt, qb = q_r.next()
            kt, kb_ = k_r.next()
            vt, vb = v_r.next()
            P.dma("sync", qt[0:64], d_qn[h * 64:(h + 1) * 64, :], reads=[Bd["qn"]], writes=[qb])
            P.dma("sync", qt[64:96], d_qr[h * 32:(h + 1) * 32, :], reads=[Bd["qr"]], writes=[qb])
            P.dma("sync", kt[0:64], d_kn[h * 64:(h + 1) * 64, :], reads=[Bd["kn"]], writes=[kb_])
            P.dma("sync", kt[64:96], d_krot, reads=[Bd["krot"]], writes=[kb_])
            P.dma("sync", vt[:, :, 0:65], vav4[:, :, h, :], reads=[Bd["va"]], writes=[vb])
            ys, ysb = ys_r.next()
            stt = {}
            ypsd = {}

            def stA(i, qt=qt, qb=qb, kt=kt, kb_=kb_, stt=stt):
                g, kb = tiles_fwd[i]
                sps, spb = ps_s.next()
                MM(sps, kt[0:96, kb * 128:(kb + 1) * 128], qt[0:96, sl(g)], True, True, [kb_, qb], spb)
                pt, ptb = p_r.next()
                ACT(lambda e: e.activation(out=pt, in_=sps, func=AF.Exp), [spb], [ptb])
                d = kb - 4 * g
                if d >= 0:
                    DVE(lambda e: e.tensor_tensor(out=pt, in0=pt, in1=maskA[:, d, :], op=ALU.mult), [ptb, Bcb], [ptb])
                stt[i] = (pt, ptb)

            def stB(i, vt=vt, vb=vb, stt=stt, ypsd=ypsd, ys=ys, ysb=ysb):
                g, kb = tiles_fwd[i]
                nkb = 4 * (g + 1)
                pt, ptb = stt.pop(i)
                if kb == 0:
                    ypsd[g] = ps_y.next()
                yps, ypb = ypsd[g]
                MM(yps[0:65, :], vt[:, kb, 0:65], pt, kb == 0, kb == nkb - 1, [vb, ptb], ypb)
                if kb == nkb - 1:
                    DVE(lambda e: e.reciprocal(out=rden[64:65, :], in_=yps[64:65, :]), [ypb], [rdenb])
                    bps, bpb = ps_c.next()
                    MM(bps[0:64, :], ones32[64:65, 0:64], rden[64:65, :], True, True, [rdenb, Bcb], bpb)
                    ACT(lambda e: e.copy(out=bcs[0:64, :], in_=bps[0:64, :]), [bpb], [bcsb])
                    DVE(lambda e: e.tensor_tensor(out=ys[0:64, sl(g)], in0=yps[0:64, :], in1=bcs[0:64, :], op=ALU.mult), [ypb, bcsb], [ysb])

            pipeline(len(tiles_fwd), [(stA, 0), (stB, 2 * OPT_MLA)])
            P.dma("gpsimd", d_ya[h * 64:(h + 1) * 64, :], ys[0:64], reads=[ysb], writes=[Bd["ya"]])

        svv = d_sv.rearrange("(b p) c -> p b c", p=128)
        ps_a = PsRot([0, 1, 2, 3])
        ps_c = PsRot([4, 5])
        ps_y = PsRot([6, 7])
        for h in range(8):
            qt, qb = q_r.next()
            kt, kb_ = k_r.next()
            vt, vb = v_r.next()
            P.dma("sync", qt[0:64], d_sq[h * 64:(h + 1) * 64, :], reads=[Bd["sq"]], writes=[qb])
            P.dma("sync", kt[0:64], d_sk[h * 64:(h + 1) * 64, :], reads=[Bd["sk"]], writes=[kb_])
            P.dma("sync", vt[:, :, 0:64], svv[:, :, h * 64:(h + 1) * 64], reads=[Bd["sv"]], writes=[vb])
            ys, ysb = ys_r.next()
            stt = {}
            gst = {}
            n_t = len(tiles_rev)

            def s0(i, qt=qt, qb=qb, kt=kt, kb_=kb_, stt=stt, gst=gst):
                g, kb = tiles_rev[i]
                nkb = 4 * (g + 1)
                if kb == nkb - 1:
                    cs_t, cs_b = csb_r.next()
                    POOL(lambda e: e.memset(cs_t, 0.0), [], [cs_b])
                    gst[g] = dict(csb=(cs_t, cs_b), yps=ps_y.next())
                aps, apb = ps_a.next()
                MM(aps, kt[0:64, kb * 128:(kb + 1) * 128], qt[0:64, sl(g)], True, False, [kb_, qb], apb)
                et, etb = e_r.next()
                ACT(lambda e: e.activation(out=et, in_=aps, func=AF.Exp), [apb], [etb])
                stt[i] = dict(aps=(aps, apb), et=(et, etb))

            def s1(i, stt=stt):
                g, kb = tiles_rev[i]
                d = kb - 4 * g
                et, etb = stt[i]["et"]
                lp, lpb = lp_r.next()
                ACT(lambda e: e.activation(out=lp, in_=et, func=AF.Ln, bias=1.0), [etb], [lpb])
                if d >= 0:
                    DVE(lambda e: e.tensor_tensor(out=lp, in0=lp, in1=maskS[:, d, :], op=ALU.mult), [lpb, Bcb], [lpb])
                stt[i]["lp"] = (lp, lpb)

            def s2(i, stt=stt):
                g, kb = tiles_rev[i]
                aps, apb = stt[i]["aps"]
                lp, lpb = stt[i]["lp"]
                MM(aps, trineg, lp, False, True, [Bcb, lpb], apb)
                if kb > 0:
                    cps, cpb = ps_c.next()
                    MM(cps, onesb, lp, True, True, [Bcb, lpb], cpb)
                    stt[i]["cps"] = (cps, cpb)

            def sU(i, stt=stt, gst=gst):
                g, kb = tiles_rev[i]
                if kb > 0:
                    cps, cpb = stt[i]["cps"]
                    cs_t, cs_b = gst[g]["csb"]
                    DVE(lambda e: e.tensor_tensor(out=cs_t, in0=cps, in1=cs_t, op=ALU.add), [cpb, cs_b], [cs_b])

            def sT_(i, stt=stt, gst=gst):
                g, kb = tiles_rev[i]
                aps, apb = stt[i]["aps"]
                cs_t, cs_b = gst[g]["csb"]
                tt, ttb = t_r.next()
                DVE(lambda e: e.tensor_tensor(out=tt, in0=aps, in1=cs_t, op=ALU.subtract), [apb, cs_b], [ttb])
                stt[i]["tt"] = (tt, ttb)

            def s4(i, stt=stt):
                g, kb = tiles_rev[i]
                d = kb - 4 * g
                tt, ttb = stt[i]["tt"]
                pt, ptb = p_r.next()
                ACT(lambda e: e.activation(out=pt, in_=tt, func=AF.Exp), [ttb], [ptb])
                if d >= 0:
                    DVE(lambda e: e.tensor_tensor(out=pt, in0=pt, in1=maskS[:, d, :], op=ALU.mult), [ptb, Bcb], [ptb])
                stt[i]["pt"] = (pt, ptb)

            def s5(i, vt=vt, vb=vb, stt=stt, gst=gst, ys=ys, ysb=ysb):
                g, kb = tiles_rev[i]
                nkb = 4 * (g + 1)
                pt, ptb = stt.pop(i)["pt"]
                yps, ypb = gst[g]["yps"]
                MM(yps[0:64, :], vt[:, kb, 0:64], pt, kb == nkb - 1, kb == 0, [vb, ptb], ypb)
                if kb == 0:
                    ACT(lambda e: e.copy(out=ys[0:64, sl(g)], in_=yps[0:64, :]), [ypb], [ysb])

            pipeline(n_t, [(s0, 0), (s1, 1), (s2, 2), (sU, 3), (sT_, 2), (s4, 3), (s5, 4)] if OPT_SB else [(s0, 0), (s1, 0), (s2, 0), (sT_, 0), (sU, 0), (s4, 0), (s5, 0)])
            P.dma("gpsimd", d_yb[h * 64:(h + 1) * 64, :], ys[0:64], reads=[ysb], writes=[Bd["yb"]])

        rvv = d_rv.rearrange("(b p) c -> p b c", p=128)
        dtv = C("dt").rearrange("p (h c) -> p h c", c=128)
        ztv = C("zeta").rearrange("p (a c) -> p a c", c=128)
        xiv = C("xi").rearrange("p (h c) -> p h c", c=128)
        ps_a = PsRot([0, 1, 2])
        ps_b = PsRot([3, 4])
        ps_k = PsRot([5, 6])
        ps_t = PsRot([7])
        for pr_i in range(4):
            qt, qb = q_r.next()
            kt, kb_ = k_r.next()
            vt, vb = v_r.next()
            P.dma("sync", qt, d_rq[pr_i * 128:(pr_i + 1) * 128, :], reads=[Bd["rq"]], writes=[qb])
            P.dma("sync", kt, d_rk[pr_i * 128:(pr_i + 1) * 128, :], reads=[Bd["rk"]], writes=[kb_])
            P.dma("sync", vt, rvv[:, :, pr_i * 128:(pr_i + 1) * 128], reads=[Bd["rv"]], writes=[vb])
            POOL(lambda e: e.memset(st_t, 0.0), [], [st_b])
            POOL(lambda e: e.memset(prevb_t, 0.0), [], [prevb_b])
            ycs_cur = [None, None]
            for n in range(NB):
                cs = slice(n * 128, (n + 1) * 128)
                tps, tpb = ps_t.next()
                tpv = tps[:, :].bitcast(BF16)
                P.op("tensor", lambda e, tpv=tpv, kt=kt, cs=cs: e.transpose(tpv[:, 0:128], kt[:, cs], identb), [kb_, Bcb], [tpb])
                ktk, ktkb = ktok_r.next()
                DVE(lambda e, ktk=ktk, tpv=tpv, pr_i=pr_i: e.tensor_tensor(out=ktk, in0=tpv[:, 0:128], in1=ztv[:, pr_i, :], op=ALU.mult), [tpb, Bc], [ktkb])
                for j in range(2):
                    hh = 2 * pr_i + j
                    rs_ = slice(64 * j, 64 * j + 64)
                    if n % 4 == 0:
                        ycs_cur[j] = ycs_r.next()
                    yc_t, yc_b = ycs_cur[j]
                    aps, apb = ps_a.next()
                    MM(aps[:, 0:128], kt[rs_, cs], qt[rs_, cs], True, True, [kb_, qb], apb)
                    sT, sTb = sT_r.next()
                    DVE(lambda e, sT=sT, aps=aps, hh=hh: e.tensor_tensor(out=sT, in0=aps[:, 0:128], in1=dtv[:, hh, :], op=ALU.mult), [apb, Bc], [sTb])
                    yps, ypb = ps_b.next()
                    MM(yps[0:64, 0:128], vt[:, n, rs_], sT, True, True, [vb, sTb], ypb)
                    xps, xpb = ps_b.next()
                    MM(xps[0:64, 0:128], prevb_t[rs_, :], qt[rs_, cs], True, True, [prevb_b, qb], xpb)
                    tc_, tcb = tmpc_r.next()
                    DVE(lambda e, tc_=tc_, xps=xps, hh=hh: e.tensor_tensor(out=tc_[0:64], in0=xps[0:64, 0:128], in1=xiv[0:64, hh, :], op=ALU.mult), [xpb, Bc], [tcb])
                    off = (n % 4) * 128
                    DVE(lambda e, yc_t=yc_t, yps=yps, tc_=tc_, off=off: e.tensor_tensor(out=yc_t[0:64, off:off + 128], in0=yps[0:64, 0:128], in1=tc_[0:64], op=ALU.add), [ypb, tcb], [yc_b])
                    if n % 4 == 3:
                        P.dma("gpsimd", d_yc[hh * 64:(hh + 1) * 64, (n - 3) * 128:(n + 1) * 128], yc_t[0:64], reads=[yc_b], writes=[Bd["yc"]])
                kps, kpb = ps_k.next()
                MM(kps[:, 0:128], ktk, vt[:, n, :], True, True, [ktkb, vb], kpb)
                for j in range(2):
                    hh = 2 * pr_i + j
                    rs_ = slice(64 * j, 64 * j + 64)
                    cdc = C("cdecay")[rs_, hh:hh + 1]
                    DVE(lambda e, kps=kps, rs_=rs_, cdc=cdc: e.scalar_tensor_tensor(out=st_t[rs_, :], in0=st_t[rs_, :], scalar=cdc, in1=kps[rs_, rs_], op0=ALU.mult, op1=ALU.add), [kpb, st_b, Bc], [st_b])
                POOL(lambda e: e.tensor_copy(out=prevb_t, in_=st_t), [st_b], [prevb_b])

        P.barrier(scr[:, 0:1])
        A.reset()
        xg_r = Rot(A, 2, [8, 512], F32, "xg")
        hg, hgb = A.tile([8, 512], BF16, "hg")
        sq_r = Rot(A, 3, [512], BF16, "sq")
        f32_r = Rot(A, 6, [512], F32, "f32")
        ya_r = Rot(A, 2, [4, 512], BF16, "ya")
        yb_r = Rot(A, 2, [4, 512], BF16, "yb")
        yc_r = Rot(A, 1, [4, 512], F32, "yc")
        ycg, ycgb = A.tile([4, 512], BF16, "ycg")
        mT, mTb = A.tile([8, 512], BF16, "mT")
        h2, h2b = A.tile([8, 512], BF16, "h2")
        act, actb = A.tile([32, 512], BF16, "act")
        wg_r = Rot(A, 3, [8, 128], BF16, "wg")
        wb_r = Rot(A, 3, [4, 128], BF16, "wbr")
        wo_r = Rot(A, 2, [8, 128], BF16, "wo")
        wu_r = Rot(A, 2, [8, 512], BF16, "wu")
        wd_r = Rot(A, 2, [32, 128], BF16, "wd")
        macc, maccb = A.tile([512], F32, "macc")
        pr = PsRot([0, 1, 2, 3, 4, 5, 6, 7])
        wgv = wview("WG", l)
        wbv = Wb["WB"][l].rearrange("(n k p) d -> p n k d", p=128, k=4)
        wov = wview("WO", l)
        wuv = wview("WU", l)
        wdv = wview("WD", l)
        yav = d_ya.rearrange("(k p) s -> p k s", p=128)
        ybv = d_yb.rearrange("(k p) s -> p k s", p=128)
        ycv = d_yc.rearrange("(k p) s -> p k s", p=128)
        bdm = C("bd")
        last = (l == depth - 1)
        for g in range(NG):
            xg, bxg = xg_r.next()
            P.dma("sync", xg, xv[:, :, sl(g)], reads=[Bx[g]], writes=[bxg])
            yat, yab = ya_r.next()
            ybt, ybb = yb_r.next()
            yct, ycb = yc_r.next()
            P.dma("sync", yat, yav[:, :, sl(g)], reads=[Bd["ya"]], writes=[yab])
            P.dma("sync", ybt, ybv[:, :, sl(g)], reads=[Bd["yb"]], writes=[ybb])
            P.dma("sync", yct, ycv[:, :, sl(g)], reads=[Bd["yc"]], writes=[ycb])
            ps, pb = pr.next()
            for k in range(8):
                sq, bsq = sq_r.next()
                ACT(lambda e, sq=sq, xg=xg, k=k: e.activation(out=sq, in_=xg[:, k, :], func=AF.Square), [bxg], [bsq])
                MM(ps, onesb, sq, k == 0, k == 7, [bsq, Bcb], pb)
            tmp, tmpb = f32_r.next()
            rs, rsb = f32_r.next()
            rstd_from_ps(ps, pb, 1024.0, rs, rsb, tmp, tmpb)
            for k in range(8):
                DVE(lambda e, k=k, xg=xg, rs=rs: e.tensor_tensor(out=hg[:, k, :], in0=xg[:, k, :], in1=rs, op=ALU.mult), [bxg, rsb], [hgb])
            for c in range(4):
                wt, wtb = wg_r.next()
                P.dma("sync", wt, wgv[:, :, c * 128:(c + 1) * 128], reads=[Bw], writes=[wtb])
                gps, gpb = pr.next()
                for k in range(8):
                    MM(gps, wt[:, k, :], hg[:, k, :], k == 0, k == 7, [wtb, hgb], gpb)
                sil, silb = f32_r.next()
                ACT(lambda e, sil=sil, gps=gps: e.activation(out=sil, in_=gps, func=AF.Silu), [gpb], [silb])
                mps, mpb = pr.next()
                MM(mps, bdm, yct[:, c, :], True, True, [Bc, ycb], mpb)
                cen, cenb = f32_r.next()
                DVE(lambda e, cen=cen, mps=mps, yct=yct, c=c: e.scalar_tensor_tensor(out=cen, in0=mps, scalar=-1.0, in1=yct[:, c, :], op0=ALU.mult, op1=ALU.add), [mpb, ycb], [cenb])
                sq32, sq32b = f32_r.next()
                POOL(lambda e, sq32=sq32, cen=cen: e.tensor_tensor(out=sq32, in0=cen, in1=cen, op=ALU.mult), [cenb], [sq32b])
                vps, vpb = pr.next()
                MM(vps, bdm, sq32, True, True, [Bc, sq32b], vpb)
                tmp, tmpb = f32_r.next()
                rv_, rvb = f32_r.next()
                rstd_from_ps(vps, vpb, 1.0, rv_, rvb, tmp, tmpb)
                gcol = gsb[:, l, 21 + c:22 + c]
                DVE(lambda e, cen=cen, rv_=rv_, gcol=gcol: e.scalar_tensor_tensor(out=cen, in0=cen, scalar=gcol, in1=rv_, op0=ALU.mult, op1=ALU.mult), [cenb, rvb, Bg], [cenb])
                POOL(lambda e, cen=cen, sil=sil, c=c: e.tensor_tensor(out=ycg[:, c, :], in0=cen, in1=sil, op=ALU.mult), [cenb, silb], [ycgb])
            for db in range(8):
                for n in range(3):
                    ysrc, ysb_ = ((yat, yab), (ybt, ybb), (ycg, ycgb))[n]
                    wb_t, wb_b = wb_r.next()
                    P.dma("sync", wb_t, wbv[:, n, :, db * 128:(db + 1) * 128], reads=[Bw], writes=[wb_b])
                    wt, wtb = wg_r.next()
                    P.dma("sync", wt, wgv[:, :, 512 + n * 1024 + db * 128:512 + n * 1024 + (db + 1) * 128], reads=[Bw], writes=[wtb])
                    ups, upb = pr.next()
                    for k in range(4):
                        MM(ups, wb_t[:, k, :], ysrc[:, k, :], k == 0, k == 3, [wb_b, ysb_], upb)
                    gps, gpb = pr.next()
                    for k in range(8):
                        MM(gps, wt[:, k, :], hg[:, k, :], k == 0, k == 7, [wtb, hgb], gpb)
                    sg, sgb = f32_r.next()
                    ACT(lambda e, sg=sg, gps=gps: e.activation(out=sg, in_=gps, func=AF.Sigmoid), [gpb], [sgb])
                    if n == 0:
                        DVE(lambda e, ups=ups, sg=sg: e.tensor_tensor(out=macc, in0=ups, in1=sg, op=ALU.mult), [upb, sgb], [maccb])
                    else:
                        DVE(lambda e, ups=ups, sg=sg: e.tensor_tensor(out=sg, in0=ups, in1=sg, op=ALU.mult), [upb, sgb], [sgb])
                        if n == 1:
                            POOL(lambda e, sg=sg: e.tensor_tensor(out=macc, in0=macc, in1=sg, op=ALU.add), [maccb, sgb], [maccb])
                        else:
                            POOL(lambda e, sg=sg, db=db: e.tensor_tensor(out=mT[:, db, :], in0=macc, in1=sg, op=ALU.add), [maccb, sgb], [mTb])
            for ob_ in range(8):
                wt, wtb = wo_r.next()
                P.dma("sync", wt, wov[:, :, ob_ * 128:(ob_ + 1) * 128], reads=[Bw], writes=[wtb])
                ops_, opb = pr.next()
                for k in range(8):
                    MM(ops_, wt[:, k, :], mT[:, k, :], k == 0, k == 7, [wtb, mTb], opb)
                DVE(lambda e, xg=xg, ops_=ops_, ob_=ob_: e.tensor_tensor(out=xg[:, ob_, :], in0=ops_, in1=xg[:, ob_, :], op=ALU.add), [opb, bxg], [bxg])
            ps, pb = pr.next()
            for k in range(8):
                sq, bsq = sq_r.next()
                ACT(lambda e, sq=sq, xg=xg, k=k: e.activation(out=sq, in_=xg[:, k, :], func=AF.Square), [bxg], [bsq])
                MM(ps, onesb, sq, k == 0, k == 7, [bsq, Bcb], pb)
            tmp, tmpb = f32_r.next()
            rs, rsb = f32_r.next()
            rstd_from_ps(ps, pb, 1024.0, rs, rsb, tmp, tmpb)
            for k in range(8):
                DVE(lambda e, k=k, xg=xg, rs=rs: e.tensor_tensor(out=h2[:, k, :], in0=xg[:, k, :], in1=rs, op=ALU.mult), [bxg, rsb], [h2b])
            for fq in range(8):
                wt, wtb = wu_r.next()
                P.dma("sync", wt, wuv[:, :, fq * 512:(fq + 1) * 512], reads=[Bw], writes=[wtb])
                for fi in range(4):
                    f = fq * 4 + fi
                    ups, upb = pr.next()
                    for k in range(8):
                        MM(ups, wt[:, k, fi * 128:(fi + 1) * 128], h2[:, k, :], k == 0, k == 7, [wtb, h2b], upb)
                    rl, rlb = f32_r.next()
                    ACT(lambda e, rl=rl, ups=ups: e.activation(out=rl, in_=ups, func=AF.Relu), [upb], [rlb])
                    eng = DVE if f % 2 == 0 else POOL
                    eng(lambda e, rl=rl, f=f: e.tensor_tensor(out=act[:, f, :], in0=rl, in1=rl, op=ALU.mult), [rlb], [actb])
            for ob_ in range(8):
                wt, wtb = wd_r.next()
                P.dma("sync", wt, wdv[:, :, ob_ * 128:(ob_ + 1) * 128], reads=[Bw], writes=[wtb])
                ops_, opb = pr.next()
                for f in range(32):
                    MM(ops_, wt[:, f, :], act[:, f, :], f == 0, f == 31, [wtb, actb], opb)
                DVE(lambda e, xg=xg, ops_=ops_, ob_=ob_: e.tensor_tensor(out=xg[:, ob_, :], in0=ops_, in1=xg[:, ob_, :], op=ALU.add), [opb, bxg], [bxg])
            if not last:
                P.dma("gpsimd", xv[:, :, sl(g)], xg, reads=[bxg], writes=[Bx[g]])
            else:
                ps, pb = pr.next()
                for k in range(8):
                    sq, bsq = sq_r.next()
                    ACT(lambda e, sq=sq, xg=xg, k=k: e.activation(out=sq, in_=xg[:, k, :], func=AF.Square), [bxg], [bsq])
                    MM(ps, onesb, sq, k == 0, k == 7, [bsq, Bcb], pb)
                tmp, tmpb = f32_r.next()
                rs, rsb = f32_r.next()
                rstd_from_ps(ps, pb, 1024.0, rs, rsb, tmp, tmpb)
                for k in range(8):
                    gcol = gsb[:, 0, 25 + k:26 + k]
                    DVE(lambda e, k=k, xg=xg, rs=rs, gcol=gcol: e.scalar_tensor_tensor(out=xg[:, k, :], in0=xg[:, k, :], scalar=gcol, in1=rs, op0=ALU.mult, op1=ALU.mult), [bxg, rsb, Bg], [bxg])
                fin.append(P.dma("gpsimd", outT.rearrange("(k p) s -> p k s", p=128)[:, :, sl(g)], xg, reads=[bxg], writes=[Bout]))

    return nc, P


fin = []
Bout = Buf("out")


def build_and_emit(S=SEQ, depth=DEPTH, debug=()):
    global fin, Bout
    fin = []
    Bout = Buf("out")
    nc, P = build(S, depth, debug)
    if not fin:
        raise RuntimeError("no output")
    stats = P.emit(final_wait_ops=fin)
    return nc, stats


def kernel(**inputs):
    x = np.asarray(inputs["x"], np.float32)
    B, S, _ = x.shape
    positions = np.asarray(inputs["positions"]).astype(np.int32)
    com = host_layout(inputs)
    consts = build_consts()
    masks = build_masks()
    depth = com["WA"].shape[0]
    nc, _ = build_and_emit(S, depth)
    in_maps = []
    for b in range(B):
        m = dict(com)
        m["xin"] = np.ascontiguousarray(x[b].T)
        m["pos"] = np.ascontiguousarray(positions[b][None, :])
        m["consts"] = consts
        m["cmask"] = masks
        in_maps.append(m)
    res = run_bass_kernel_spmd(nc, in_maps, core_ids=list(range(B)))
    out = np.stack([np.ascontiguousarray(res.results[b]["outT"].T) for b in range(B)], 0)
    return out.astype(np.float32)
```

```python
import math
import os
import numpy as np
import concourse.bass as bass
import concourse.mybir as mybir
from concourse.bass_utils import run_bass_kernel_spmd

F32 = mybir.dt.float32
BF16 = mybir.dt.bfloat16
I32 = mybir.dt.int32
U8 = mybir.dt.uint8
ALU = mybir.AluOpType
AF = mybir.ActivationFunctionType

D = 1024
EPS = 1e-6
NCORES = 8
DEPTH = 4
SEQ = 4096
QS = 96 ** -0.5
OPT_P0ACT = int(os.environ.get('KV_P0ACT', '1'))
OPT_MLA = int(os.environ.get('KV_MLA', '1'))
OPT_SB = int(os.environ.get('KV_SB', '1'))
OPT_RET = int(os.environ.get('KV_RET', '1'))
OPT_CONVOVL = int(os.environ.get('KV_CONVOVL', '1'))


class Buf:
    __slots__ = ("name", "last_w", "readers")

    def __init__(self, name, reg=None):
        self.name = name
        self.last_w = None
        self.readers = []
        if reg is not None:
            reg.append(self)


class Prog:
    ENGS = ("tensor", "vector", "scalar", "gpsimd", "sync")

    def __init__(self, nc, n_dma_sems=32):
        self.nc = nc
        self.ops = []
        self.n_dma_sems = n_dma_sems
        self.allbufs = []
        self.last_barrier = None

    def buf(self, name="b"):
        b = Buf(name, self.allbufs)
        b.last_w = self.last_barrier
        return b

    def op(self, eng, fn, reads=(), writes=(), dma=False):
        idx = len(self.ops)
        deps = set()
        for b in reads:
            if b.last_w is not None:
                deps.add(b.last_w)
        for b in writes:
            if b.last_w is not None:
                deps.add(b.last_w)
            deps.update(b.readers)
        deps.discard(idx)
        self.ops.append(dict(eng=eng, fn=fn, deps=deps, dma=dma, signal=False))
        for b in reads:
            b.readers.append(idx)
        for b in writes:
            b.last_w = idx
            b.readers = []
        return idx

    def dma(self, eng, out, in_, reads=(), writes=()):
        return self.op(eng, lambda e: e.dma_start(out=out, in_=in_), reads, writes, dma=True)

    def barrier(self, scratch_ap):
        bs = list(self.allbufs)
        i = self.op("vector", lambda e: e.memset(scratch_ap, 0.0), bs, bs)
        self.last_barrier = i
        return i

    def emit(self, final_wait_ops=()):
        nc = self.nc
        ops = self.ops
        for i, o in enumerate(ops):
            nd = set()
            for d in o["deps"]:
                p = ops[d]
                if (not p["dma"]) and (not o["dma"]) and p["eng"] == o["eng"] and o["eng"] == "tensor":
                    continue
                nd.add(d)
            o["deps"] = nd
            for d in nd:
                ops[d]["signal"] = True
        for d in final_wait_ops:
            ops[d]["signal"] = True
        eng_sem = {e: nc.alloc_semaphore(name=f"s_{e}") for e in self.ENGS}
        dma_sems = [nc.alloc_semaphore(name=f"d_{i}") for i in range(self.n_dma_sems)]
        cnt = {e: 0 for e in self.ENGS}
        dma_cnt = [0] * self.n_dma_sems
        dma_rr = 0
        for o in ops:
            if o["dma"]:
                s = dma_rr % self.n_dma_sems
                dma_rr += 1
                o["prev_val"] = dma_cnt[s]
                dma_cnt[s] += 16
                o["sem"] = ("d", s)
                o["val"] = dma_cnt[s]
            elif o["signal"]:
                cnt[o["eng"]] += 1
                o["sem"] = ("e", o["eng"])
                o["val"] = cnt[o["eng"]]
        per_eng = {e: [] for e in self.ENGS}
        for i, o in enumerate(ops):
            per_eng[o["eng"]].append(i)

        def semof(key):
            return dma_sems[key[1]] if key[0] == "d" else eng_sem[key[1]]

        def run_engine(ename, eng, final=False):
            waited = {}
            for i in per_eng[ename]:
                o = ops[i]
                need = {}
                for d in o["deps"]:
                    p = ops[d]
                    k = p["sem"]
                    if p["val"] > need.get(k, 0):
                        need[k] = p["val"]
                if o["dma"] and o["prev_val"] > 0:
                    k = o["sem"]
                    need[k] = max(need.get(k, 0), o["prev_val"])
                for k, v in need.items():
                    if waited.get(k, 0) < v:
                        eng.wait_ge(semof(k), v)
                        waited[k] = v
                ins = o["fn"](eng)
                if o["dma"]:
                    ins.then_inc(semof(o["sem"]), 16)
                elif o["signal"]:
                    ins.then_inc(semof(o["sem"]), 1)
            if final:
                need = {}
                for d in final_wait_ops:
                    p = ops[d]
                    need[p["sem"]] = max(need.get(p["sem"], 0), p["val"])
                for k, v in need.items():
                    if waited.get(k, 0) < v:
                        eng.wait_ge(semof(k), v)

        with nc.Block() as block:
            @block.tensor
            def _(e):
                run_engine("tensor", e)

            @block.vector
            def _(e):
                run_engine("vector", e)

            @block.scalar
            def _(e):
                run_engine("scalar", e)

            @block.gpsimd
            def _(e):
                run_engine("gpsimd", e)

            @block.sync
            def _(e):
                run_engine("sync", e, final=True)
        return {e: len(per_eng[e]) for e in self.ENGS}


class Arena:
    def __init__(self, ap_u8, P):
        self.ap = ap_u8
        self.size = ap_u8.shape[1]
        self.off = 0
        self.P = P

    def reset(self):
        self.off = 0

    def tile(self, free_shape, dtype, name="t"):
        esz = 2 if dtype == BF16 else 4
        n = 1
        for s in free_shape:
            n *= s
        nbytes = (n * esz + 31) // 32 * 32
        assert self.off + nbytes <= self.size, (name, self.off, nbytes, self.size)
        v = self.ap[:, self.off:self.off + nbytes]
        self.off += nbytes
        v = v[:, 0:n * esz].bitcast(dtype)
        if len(free_shape) == 2:
            v = v.rearrange("p (a b) -> p a b", b=free_shape[1])
        elif len(free_shape) == 3:
            v = v.rearrange("p (a b c) -> p a b c", b=free_shape[1], c=free_shape[2])
        return v, self.P.buf(name)


class Rot:
    def __init__(self, arena, n, free_shape, dtype, name="r"):
        self.items = [arena.tile(free_shape, dtype, f"{name}{i}") for i in range(n)]
        self.i = 0

    def next(self):
        it = self.items[self.i % len(self.items)]
        self.i += 1
        return it


CONST_LAYOUT = {}


def build_consts():
    cols = []

    def add(name, arr):
        arr = np.asarray(arr, np.float64).reshape(128, -1)
        off = sum(c.shape[1] for c in cols)
        CONST_LAYOUT[name] = (off, arr.shape[1])
        cols.append(arr)

    bd = np.zeros((128, 128))
    bd[0:64, 0:64] = 1.0 / 64
    bd[64:128, 64:128] = 1.0 / 64
    add("bd", bd)
    h = np.arange(8)
    lg = np.log1p(-np.exp2(-5.0 - h).astype(np.float32)).astype(np.float32).astype(np.float64)
    m = np.arange(128)[:, None]
    c = np.arange(128)[None, :]
    dt = np.stack([np.where(c >= m, np.exp(np.maximum(c - m, 0) * lg[hh]), 0.0) for hh in range(8)], 1)
    add("dt", dt)
    z = np.zeros((128, 4, 128))
    for pr in range(4):
        for col in range(128):
            hh = 2 * pr + col // 64
            z[:, pr, col] = np.exp((127 - np.arange(128)) * lg[hh])
    add("zeta", z)
    xi = np.zeros((128, 8, 128))
    for hh in range(8):
        xi[:, hh, :] = np.exp((np.arange(128) + 1.0) * lg[hh])[None, :]
    add("xi", xi)
    r = np.arange(128)
    invf_r = (10000.0 ** (-(2.0 * (r % 32)) / 64)).astype(np.float32)
    invf_m = (10000.0 ** (-(2.0 * (r % 16)) / 32)).astype(np.float32)
    add("invf", np.stack([invf_r, invf_m], 1))
    sg_r = np.where((r % 64) < 32, -1.0, 1.0)
    sg_m = np.where((r % 32) < 16, -1.0, 1.0)
    add("sgn", np.stack([sg_r, sg_m], 1))
    cd = np.exp(128.0 * lg)
    add("cdecay", np.tile(cd[None, :], (128, 1)))
    return np.concatenate(cols, 1).astype(np.float32)


IN_OFFS = dict(cq=(0, 384), ckv=(384, 640), kpe=(640, 672), sbq=(672, 1184), sbk=(1184, 1696), sbv=(1696, 2208),
               rq=(2208, 2720), rk=(2720, 3232), rv=(3232, 3744), rg=(3744, 4256), gate=(4256, 7328))
WA_COLS = 3840
WA_OFF = dict(cq=0, ckv=384, sbq=640, sbk=1152, rq=1664, rqs=2176, rk=2688, rks=3200, kpe=3712, kpes=3744)


def build_masks():
    p = np.arange(128)[:, None]
    j = np.arange(512)[None, :]
    mA = np.stack([(j >= p + 128 * d) for d in range(4)], 1).astype(np.float32).reshape(128, -1)
    mS = np.stack([(j > p + 128 * d) for d in range(4)], 1).astype(np.float32).reshape(128, -1)
    jj = np.arange(128)[:, None]
    ss = np.arange(128)[None, :]
    tri = -(jj >= ss).astype(np.float32)
    return np.concatenate([mA, mS, tri, np.eye(128, dtype=np.float32)], 1)


def host_layout(inputs):
    w_in = np.asarray(inputs["w_in"])
    depth = w_in.shape[0]

    def sl(name):
        a, b = IN_OFFS[name]
        return w_in[:, :, a:b]

    def swap_heads(w, hd):
        L, K, N = w.shape
        w4 = w.reshape(L, K, N // hd, hd)
        return np.concatenate([w4[..., hd // 2:], w4[..., :hd // 2]], -1).reshape(L, K, N)

    WA = np.concatenate([sl("cq"), sl("ckv"), sl("sbq"), sl("sbk"), sl("rq"), swap_heads(sl("rq"), 64),
                         sl("rk"), swap_heads(sl("rk"), 64), sl("kpe"), swap_heads(sl("kpe"), 32)], -1)
    WV = np.concatenate([sl("sbv"), sl("rv")], -1)
    WG = np.concatenate([sl("rg"), sl("gate")], -1)
    wuq = np.asarray(inputs["mla_w_uq"]).reshape(depth, 384, 8, 96)
    qn = wuq[..., :64].reshape(depth, 384, 512)
    qr = wuq[..., 64:].reshape(depth, 384, 256)
    WQ = np.concatenate([qn, qr, swap_heads(qr, 32)], -1)
    wukv = np.asarray(inputs["mla_w_ukv"]).reshape(depth, 256, 8, 128)
    WKV = np.concatenate([wukv[..., :64].reshape(depth, 256, 512), wukv[..., 64:].reshape(depth, 256, 512)], -1)
    WB = np.asarray(inputs["w_branch"]).reshape(depth, 1536, 1024)

    def g128(v):
        L, N = v.shape
        return v.reshape(L, N // 128, 128).transpose(0, 2, 1)

    gains = np.concatenate([g128(np.asarray(inputs["norm_mix_g"])), g128(np.asarray(inputs["mla_q_norm_g"])),
                            g128(np.asarray(inputs["mla_kv_norm_g"])), g128(np.asarray(inputs["norm_mlp_g"])),
                            g128(np.asarray(inputs["ret_norm_g"])),
                            np.broadcast_to(g128(np.asarray(inputs["final_norm_g"])[None]), (depth, 128, 8))], -1)
    com = dict(WA=WA, WV=WV, WG=WG, WQ=WQ, WKV=WKV, WB=WB, WO=np.asarray(inputs["w_out"]),
               WU=np.asarray(inputs["w_up"]), WD=np.asarray(inputs["w_down"]), gains=gains)
    return {k: np.ascontiguousarray(v, dtype=np.float32) for k, v in com.items()}


WSHAPES = dict(WA=(1024, 3776), WV=(1024, 1024), WG=(1024, 3584), WQ=(384, 1024), WKV=(256, 1024),
               WB=(1536, 1024), WO=(1024, 1024), WU=(1024, 4096), WD=(4096, 1024))
WGAIN = dict(WA=0, WV=0, WG=0, WQ=8, WKV=11, WU=13)


def build(S=SEQ, depth=DEPTH, debug=()):
    NG = S // 512
    NB = S // 128
    nc = bass.Bass("TRN2", target_bir_lowering=False)
    P = Prog(nc)
    consts_np = build_consts()
    NCONST = consts_np.shape[1]

    def dram(name, shape, dt, kind=None):
        if kind is None:
            kind = "ExternalOutput" if name in debug else "Internal"
        return nc.dram_tensor(name, list(shape), dt, kind=kind).ap()

    xin = dram("xin", [D, S], F32, "ExternalInput")
    pos = dram("pos", [1, S], I32, "ExternalInput")
    cin = dram("consts", [128, NCONST], F32, "ExternalInput")
    cmin = dram("cmask", [128, 4352], F32, "ExternalInput")
    gin = dram("gains", [depth, 128, 33], F32, "ExternalInput")
    Win = {k: dram(k, [depth] + list(s), F32, "ExternalInput") for k, s in WSHAPES.items()}
    outT = dram("outT", [D, S], F32, "ExternalOutput")
    Wb = {k: dram("b" + k, [depth] + list(s), BF16) for k, s in WSHAPES.items()}
    xres = dram("xres", [D, S], F32)
    tabR = dram("tabR", [128, 2, S], F32)
    tabM = dram("tabM", [128, 2, S], F32)
    d_sq = dram("d_sq", [512, S], BF16)
    d_sk = dram("d_sk", [512, S], BF16)
    d_sv = dram("d_sv", [S, 512], BF16)
    d_rq = dram("d_rq", [512, S], BF16)
    d_rk = dram("d_rk", [512, S], BF16)
    d_rv = dram("d_rv", [S, 512], BF16)
    d_krot = dram("d_krot", [32, S], BF16)
    d_qn = dram("d_qn", [512, S], BF16)
    d_qr = dram("d_qr", [256, S], BF16)
    d_kn = dram("d_kn", [512, S], BF16)
    d_va = dram("d_va", [S, 8, 65], BF16)
    d_ya = dram("d_ya", [512, S], BF16)
    d_yb = dram("d_yb", [512, S], BF16)
    d_yc = dram("d_yc", [512, S], F32)
    Bx = [P.buf(f"x{g}") for g in range(NG)]
    Bwl = [P.buf(f"wb16_{l}") for l in range(depth)]
    Btab = P.buf("tab")
    Bd = {n: P.buf(n) for n in "sq sk sv rq rk rv krot qn qr kn va ya yb yc".split()}

    cst = nc.alloc_sbuf_tensor("cst", [128, NCONST], F32)
    Bc = P.buf("cst")
    gsb = nc.alloc_sbuf_tensor("gsb", [128, depth, 33], F32)
    Bg = P.buf("gsb")
    PERS_BF = 4 * 512 * 2 + 128 * 4
    cb = nc.alloc_sbuf_tensor("cb", [128, 4 * 512 * 2 + 128 * 3], BF16)
    Bcb = P.buf("cb")
    ones32 = nc.alloc_sbuf_tensor("ones32", [128, 128], F32)
    scr = nc.alloc_sbuf_tensor("scr", [128, 8], F32)
    ARENA_BYTES = 186 * 1024
    arena_t = nc.alloc_sbuf_tensor("arena", [128, ARENA_BYTES], U8)
    A = Arena(arena_t[:, :], P)
    psb = [nc.alloc_psum_tensor(f"ps{i}", [128, 512], F32)[:, :] for i in range(8)]
    PB = [P.buf(f"ps{i}") for i in range(8)]

    class PsRot:
        def __init__(self, idxs):
            self.idxs = idxs
            self.i = 0

        def next(self):
            k = self.idxs[self.i % len(self.idxs)]
            self.i += 1
            return psb[k], PB[k]

    def C(name):
        o, n = CONST_LAYOUT[name]
        return cst[:, o:o + n]

    maskA = cb[:, 0:2048].rearrange("p (d j) -> p d j", j=512)
    maskS = cb[:, 2048:4096].rearrange("p (d j) -> p d j", j=512)
    trineg = cb[:, 4096:4224]
    identb = cb[:, 4224:4352]
    onesb = cb[:, 4352:4480]

    P.dma("sync", cst[:], cin, writes=[Bc])
    P.dma("sync", gsb[:], gin.rearrange("l p c -> p l c"), writes=[Bg])
    A.reset()
    cm_t, cm_b = A.tile([4352], F32, "cmask")
    P.dma("sync", cm_t, cmin, writes=[cm_b])
    P.op("vector", lambda e: e.tensor_copy(out=cb[:, 0:4352], in_=cm_t), [cm_b], [Bcb])
    P.barrier(scr[:, 0:1])
    P.op("vector", lambda e: e.memset(onesb, 1.0), [], [Bcb])
    P.op("vector", lambda e: e.memset(ones32[:], 1.0), [], [Bcb])
    P.dma("sync", xres, xin, writes=Bx)

    A.reset()
    TW = min(S, 2048)
    pi_t, b_pi = A.tile([TW], I32, "pi")
    pf_t, b_pf = A.tile([TW], F32, "pf")
    ang_t, _ = A.tile([TW], F32, "ang")
    t4_t, _ = A.tile([TW], F32, "t4")
    ki_t, _ = A.tile([TW], I32, "ki")
    kf_t, _ = A.tile([TW], F32, "kf")
    mk_t, _ = A.tile([TW], F32, "mk")
    tb_t, b_tb = A.tile([2, TW], F32, "tabst")
    C1 = 6.28125
    C2 = 2 * math.pi - C1
    bt = b_pf

    def reduce_and_sin(shift, out_ap, post_sign_col):
        V = lambda f: P.op("vector", f, [bt, Bc], [bt])
        V(lambda e: e.tensor_scalar(out=ang_t, in0=ang_t, scalar1=float(shift), scalar2=None, op0=ALU.add))
        V(lambda e: e.tensor_scalar(out=t4_t, in0=ang_t, scalar1=1.0 / (2 * math.pi), scalar2=0.5, op0=ALU.mult, op1=ALU.add))
        V(lambda e: e.tensor_copy(out=ki_t, in_=t4_t))
        V(lambda e: e.tensor_copy(out=kf_t, in_=ki_t))
        V(lambda e: e.scalar_tensor_tensor(out=t4_t, in0=kf_t, scalar=-C1, in1=ang_t, op0=ALU.mult, op1=ALU.add))
        V(lambda e: e.scalar_tensor_tensor(out=t4_t, in0=kf_t, scalar=-C2, in1=t4_t, op0=ALU.mult, op1=ALU.add))
        V(lambda e: e.tensor_scalar(out=mk_t, in0=t4_t, scalar1=-math.pi, scalar2=2 * math.pi, op0=ALU.is_lt, op1=ALU.mult))
        V(lambda e: e.tensor_tensor(out=t4_t, in0=t4_t, in1=mk_t, op=ALU.add))
        V(lambda e: e.tensor_scalar(out=mk_t, in0=t4_t, scalar1=math.pi, scalar2=-2 * math.pi, op0=ALU.is_gt, op1=ALU.mult))
        V(lambda e: e.tensor_tensor(out=t4_t, in0=t4_t, in1=mk_t, op=ALU.add))
        V(lambda e: e.tensor_scalar(out=t4_t, in0=t4_t, scalar1=-3.1415925, scalar2=3.1415925, op0=ALU.max, op1=ALU.min))
        P.op("scalar", lambda e: e.activation(out=out_ap, in_=t4_t, func=AF.Sin), [bt], [b_tb])
        if post_sign_col is not None:
            P.op("vector", lambda e: e.tensor_scalar(out=out_ap, in0=out_ap, scalar1=post_sign_col, scalar2=None, op0=ALU.mult), [b_tb, Bc], [b_tb])

    for which, tab in ((0, tabR), (1, tabM)):
        for t0 in range(0, S, TW):
            P.dma("sync", pi_t, pos[:, t0:t0 + TW].partition_broadcast(128), writes=[b_pi])
            P.op("vector", lambda e: e.tensor_copy(out=pf_t, in_=pi_t), [b_pi], [bt])
            invc = C("invf")[:, which:which + 1]
            sgc = C("sgn")[:, which:which + 1]
            P.op("vector", lambda e, invc=invc: e.tensor_scalar(out=ang_t, in0=pf_t, scalar1=invc, scalar2=None, op0=ALU.mult), [bt, Bc], [bt])
            reduce_and_sin(math.pi / 2, tb_t[:, 0, :], None)
            P.op("vector", lambda e, invc=invc: e.tensor_scalar(out=ang_t, in0=pf_t, scalar1=invc, scalar2=None, op0=ALU.mult), [bt, Bc], [bt])
            reduce_and_sin(0.0, tb_t[:, 1, :], sgc)
            P.dma("gpsimd", tab[:, :, t0:t0 + TW], tb_t, reads=[b_tb], writes=[Btab])

    P.barrier(scr[:, 0:1])
    A.reset()
    PW = 2048
    ldr0 = Rot(A, 6, [PW], F32, "wld")
    cvr0 = Rot(A, 6, [PW], BF16, "wcv")
    conv_cnt = [0]

    def conv_piece_list(l):
        out = []
        for k, (K, N) in WSHAPES.items():
            for kc in range(K // 128):
                for c0 in range(0, N, PW):
                    out.append((l, k, kc, c0, min(PW, N - c0)))
        return out

    def emit_conv(piece, ldr, cvr, allow_act):
        l, k, kc, c0, n = piece
        goff = WGAIN.get(k)
        lt, lb = ldr.next()
        ct, cbf = cvr.next()
        P.dma("sync", lt[:, 0:n], Win[k][l, kc * 128:(kc + 1) * 128, c0:c0 + n], writes=[lb])
        useact = allow_act and (conv_cnt[0] % 2 == 1) and OPT_P0ACT
        conv_cnt[0] += 1
        if goff is None:
            if useact:
                P.op("scalar", lambda e: e.copy(out=ct[:, 0:n], in_=lt[:, 0:n]), [lb], [cbf])
            else:
                P.op("vector", lambda e: e.tensor_copy(out=ct[:, 0:n], in_=lt[:, 0:n]), [lb], [cbf])
        else:
            gcol = gsb[:, l, goff + kc:goff + kc + 1]
            if useact:
                P.op("scalar", lambda e: e.mul(out=ct[:, 0:n], in_=lt[:, 0:n], mul=gcol), [lb, Bg], [cbf])
            else:
                P.op("vector", lambda e: e.tensor_scalar(out=ct[:, 0:n], in0=lt[:, 0:n], scalar1=gcol, scalar2=None, op0=ALU.mult), [lb, Bg], [cbf])
        P.dma("gpsimd", Wb[k][l, kc * 128:(kc + 1) * 128, c0:c0 + n], ct[:, 0:n], reads=[cbf], writes=[Bwl[l]])

    n_up = depth if not OPT_CONVOVL else 1
    for l in range(n_up):
        for piece in conv_piece_list(l):
            emit_conv(piece, ldr0, cvr0, True)

    def wview(k, l):
        return Wb[k][l].rearrange("(k p) n -> p k n", p=128)

    def ACT(fn, r, w):
        return P.op("scalar", fn, r, w)

    def DVE(fn, r, w):
        return P.op("vector", fn, r, w)

    def POOL(fn, r, w):
        return P.op("gpsimd", fn, r, w)

    def MM(ps, lhsT, rhs, start, stop, r, w):
        return P.op("tensor", lambda e: e.matmul(ps, lhsT=lhsT, rhs=rhs, start=start, stop=stop), r, [w])

    def rstd_from_ps(ps, pb, n, out_ap, out_b, tmp, tmpb):
        ACT(lambda e: e.activation(out=tmp, in_=ps, func=AF.Ln, bias=EPS, scale=1.0 / n), [pb], [tmpb])
        ACT(lambda e: e.activation(out=out_ap, in_=tmp, func=AF.Exp, scale=-0.5), [tmpb], [out_b])

    xv = xres.rearrange("(k p) s -> p k s", p=128)

    for l in range(depth):
        P.barrier(scr[:, 0:1])
        A.reset()
        cqT, b_cq = A.tile([3, S], BF16, "cqT")
        ckvT, b_ckv = A.tile([2, S], BF16, "ckvT")
        hT, b_h = A.tile([8, S], BF16, "hT")
        Bh = [P.buf(f"h{g}") for g in range(NG)]
        xg_r = Rot(A, 1, [8, 512], F32, "xg")
        sq_r = Rot(A, 3, [512], BF16, "sq")
        f32_r = Rot(A, 6, [512], F32, "f32")
        w_r = Rot(A, 2, [8, 256], BF16, "wblk")
        wv_t, b_wv = A.tile([8, 512], BF16, "wv")
        st_r = Rot(A, 2, [S], BF16, "stage")
        tab_r = Rot(A, 2, [2, 512], F32, "tab")
        vst_r = Rot(A, 2, [4, 512], BF16, "vst")
        pr = PsRot([0, 1, 2, 3, 4, 5, 6, 7])

        def sl(g):
            return slice(g * 512, (g + 1) * 512)

        for g in range(NG):
            xg, bxg = xg_r.next()
            P.dma("sync", xg, xv[:, :, sl(g)], reads=[Bx[g]], writes=[bxg])
            ps, pb = pr.next()
            for k in range(8):
                sq, bsq = sq_r.next()
                ACT(lambda e, sq=sq, xg=xg, k=k: e.activation(out=sq, in_=xg[:, k, :], func=AF.Square), [bxg], [bsq])
                MM(ps, onesb, sq, k == 0, k == 7, [bsq, Bcb], pb)
            tmp, tmpb = f32_r.next()
            rs, rsb = f32_r.next()
            rstd_from_ps(ps, pb, 1024.0, rs, rsb, tmp, tmpb)
            for k in range(8):
                DVE(lambda e, k=k, xg=xg, rs=rs, g=g: e.tensor_tensor(out=hT[:, k, sl(g)], in0=xg[:, k, :], in1=rs, op=ALU.mult), [bxg, rsb], [Bh[g]])

        wa = wview("WA", l)

        def fm_block(c0, M, n_mm=1, c1=None):
            wt, wtb = w_r.next()
            P.dma("sync", wt[:, :, 0:M], wa[:, :, c0:c0 + M], reads=[Bwl[l]], writes=[wtb])
            if c1 is not None:
                P.dma("sync", wt[:, :, 128:128 + M], wa[:, :, c1:c1 + M], reads=[Bwl[l]], writes=[wtb])
            return wt, wtb

        def fm_mm(wt, wtb, off, M, g):
            ps, pb = pr.next()
            for k in range(8):
                MM(ps[0:M, :], wt[:, k, off:off + M], hT[:, k, sl(g)], k == 0, k == 7, [wtb, Bh[g]], pb)
            return ps, pb

        for name, dst, dstb, nblk in (("cq", cqT, b_cq, 3), ("ckv", ckvT, b_ckv, 2)):
            for c in range(nblk):
                wt, wtb = fm_block(WA_OFF[name] + c * 128, 128)
                for g in range(NG):
                    ps, pb = fm_mm(wt, wtb, 0, 128, g)
                    ACT(lambda e, ps=ps, dst=dst, c=c, g=g: e.copy(out=dst[:, c, sl(g)], in_=ps), [pb], [dstb])
        for name, dd, dbuf, scale in (("sbq", d_sq, Bd["sq"], 0.125), ("sbk", d_sk, Bd["sk"], 1.0)):
            for c in range(4):
                wt, wtb = fm_block(WA_OFF[name] + c * 128, 128)
                st, stb = st_r.next()
                for g in range(NG):
                    ps, pb = fm_mm(wt, wtb, 0, 128, g)
                    ACT(lambda e, ps=ps, st=st, g=g, scale=scale: e.mul(out=st[:, sl(g)], in_=ps, mul=scale), [pb], [stb])
                P.dma("gpsimd", dd[c * 128:(c + 1) * 128, :], st, reads=[stb], writes=[dbuf])
        for name, sname, dd, dbuf, scale, M, tab, nblk in (
                ("rq", "rqs", d_rq, Bd["rq"], 1.0, 128, tabR, 4), ("rk", "rks", d_rk, Bd["rk"], 0.125, 128, tabR, 4),
                ("kpe", "kpes", d_krot, Bd["krot"], 1.0, 32, tabM, 1)):
            for c in range(nblk):
                wt, wtb = fm_block(WA_OFF[name] + c * 128, M, c1=WA_OFF[sname] + c * 128)
                st, stb = st_r.next()
                for g in range(NG):
                    tb, tbb = tab_r.next()
                    P.dma("sync", tb[0:M], tab[0:M, :, sl(g)], reads=[Btab], writes=[tbb])
                    ps, pb = fm_mm(wt, wtb, 0, M, g)
                    ps2, pb2 = fm_mm(wt, wtb, 128, M, g)
                    t1, t1b = f32_r.next()
                    t2, t2b = f32_r.next()
                    DVE(lambda e, ps=ps, tb=tb, t1=t1, M=M, scale=scale: e.scalar_tensor_tensor(out=t1[0:M], in0=ps[0:M, :], scalar=scale, in1=tb[0:M, 0, :], op0=ALU.mult, op1=ALU.mult), [pb, tbb], [t1b])
                    DVE(lambda e, ps2=ps2, tb=tb, t2=t2, M=M, scale=scale: e.scalar_tensor_tensor(out=t2[0:M], in0=ps2[0:M, :], scalar=scale, in1=tb[0:M, 1, :], op0=ALU.mult, op1=ALU.mult), [pb2, tbb], [t2b])
                    POOL(lambda e, st=st, t1=t1, t2=t2, g=g, M=M: e.tensor_tensor(out=st[0:M, sl(g)], in0=t1[0:M], in1=t2[0:M], op=ALU.add), [t1b, t2b], [stb])
                P.dma("gpsimd", dd[c * 128:c * 128 + M, :], st[0:M], reads=[stb], writes=[dbuf])
        wvv = wview("WV", l)
        for vi, (dd, dbuf) in enumerate(((d_sv, Bd["sv"]), (d_rv, Bd["rv"]))):
            P.dma("sync", wv_t, wvv[:, :, vi * 512:(vi + 1) * 512], reads=[Bwl[l]], writes=[b_wv])
            ddv = dd.rearrange("(b p) c -> p b c", p=128)
            for g in range(NG):
                vs, vsb = vst_r.next()
                for j in range(4):
                    tbi = g * 4 + j
                    ps, pb = pr.next()
                    for k in range(8):
                        MM(ps, hT[:, k, tbi * 128:(tbi + 1) * 128], wv_t[:, k, :], k == 0, k == 7, [Bh[g], b_wv], pb)
                    if j % 2 == 0:
                        ACT(lambda e, ps=ps, vs=vs, j=j: e.copy(out=vs[:, j, :], in_=ps), [pb], [vsb])
                    else:
                        DVE(lambda e, ps=ps, vs=vs, j=j: e.tensor_copy(out=vs[:, j, :], in_=ps), [pb], [vsb])
                P.dma("gpsimd", ddv[:, g * 4:(g + 1) * 4, :], vs, reads=[vsb], writes=[dbuf])

        P.barrier(scr[:, 0:1])
        A.reset()
        cqT, b_cq = A.tile([3, S], BF16, "cqT")
        ckvT, b_ckv = A.tile([2, S], BF16, "ckvT")
        sq_r = Rot(A, 4, [512], BF16, "sq")
        f32_r = Rot(A, 8, [512], F32, "f32")
        tab_r = Rot(A, 2, [2, 512], F32, "tab")
        wq_t, b_wq = A.tile([3, 1024], BF16, "wq")
        wkv_t, b_wkv = A.tile([2, 1024], BF16, "wkv")
        ost_r = Rot(A, 4, [512], BF16, "ost")
        vaug_r = Rot(A, 2, [4, 8, 65], BF16, "vaug")
        rtok_r = Rot(A, 2, [4], F32, "rtok")
        P.dma("sync", wq_t, wview("WQ", l), reads=[Bwl[l]], writes=[b_wq])
        P.dma("sync", wkv_t, wview("WKV", l), reads=[Bwl[l]], writes=[b_wkv])
        for it in vaug_r.items:
            DVE(lambda e, t=it[0]: e.memset(t[:, :, :, 64:65], 1.0), [], [it[1]])
        vav = d_va.rearrange("(b p) h e -> p b (h e)", p=128)
        for g in range(NG):
            tbm, tbmb = tab_r.next()
            P.dma("sync", tbm, tabM[:, :, sl(g)], reads=[Btab], writes=[tbmb])
            ps, pb = pr.next()
            for k in range(3):
                sq, bsq = sq_r.next()
                POOL(lambda e, sq=sq, k=k, g=g: e.tensor_tensor(out=sq, in0=cqT[:, k, sl(g)], in1=cqT[:, k, sl(g)], op=ALU.mult), [b_cq], [bsq])
                MM(ps, onesb, sq, k == 0, k == 2, [bsq, Bcb], pb)
            tmp, tmpb = f32_r.next()
            rq_, rqb = f32_r.next()
            rstd_from_ps(ps, pb, 384.0, rq_, rqb, tmp, tmpb)
            ps, pb = pr.next()
            ps_t, pb_t = pr.next()
            sqs = []
            for k in range(2):
                sq, bsq = sq_r.next()
                POOL(lambda e, sq=sq, k=k, g=g: e.tensor_tensor(out=sq, in0=ckvT[:, k, sl(g)], in1=ckvT[:, k, sl(g)], op=ALU.mult), [b_ckv], [bsq])
                MM(ps, onesb, sq, k == 0, k == 1, [bsq, Bcb], pb)
                sqs.append((sq, bsq))
            for j in range(4):
                for k in range(2):
                    MM(ps_t[:, j:j + 1], sqs[k][0][:, j * 128:(j + 1) * 128], onesb[:, 0:1], k == 0, k == 1, [sqs[k][1], Bcb], pb_t)
            tmp2, tmp2b = f32_r.next()
            rkv, rkvb = f32_r.next()
            rstd_from_ps(ps, pb, 256.0, rkv, rkvb, tmp2, tmp2b)
            rt, rtb = rtok_r.next()
            ACT(lambda e, rt=rt, ps_t=ps_t: e.activation(out=rt, in_=ps_t[:, 0:4], func=AF.Ln, bias=EPS, scale=1.0 / 256.0), [pb_t], [rtb])
            ACT(lambda e, rt=rt: e.activation(out=rt, in_=rt, func=AF.Exp, scale=-0.5), [rtb], [rtb])
            for c in range(4):
                ps, pb = pr.next()
                for k in range(3):
                    MM(ps, wq_t[:, k, c * 128:(c + 1) * 128], cqT[:, k, sl(g)], k == 0, k == 2, [b_wq, b_cq], pb)
                o, ob = ost_r.next()
                DVE(lambda e, o=o, ps=ps, rq_=rq_: e.scalar_tensor_tensor(out=o, in0=ps, scalar=QS, in1=rq_, op0=ALU.mult, op1=ALU.mult), [pb, rqb], [ob])
                P.dma("gpsimd", d_qn[c * 128:(c + 1) * 128, sl(g)], o, reads=[ob], writes=[Bd["qn"]])
            for c in range(2):
                ps, pb = pr.next()
                ps2, pb2 = pr.next()
                for k in range(3):
                    MM(ps, wq_t[:, k, 512 + c * 128:512 + (c + 1) * 128], cqT[:, k, sl(g)], k == 0, k == 2, [b_wq, b_cq], pb)
                for k in range(3):
                    MM(ps2, wq_t[:, k, 768 + c * 128:768 + (c + 1) * 128], cqT[:, k, sl(g)], k == 0, k == 2, [b_wq, b_cq], pb2)
                t1, t1b = f32_r.next()
                t2, t2b = f32_r.next()
                DVE(lambda e, ps=ps, tbm=tbm, t1=t1: e.scalar_tensor_tensor(out=t1, in0=ps, scalar=QS, in1=tbm[:, 0, :], op0=ALU.mult, op1=ALU.mult), [pb, tbmb], [t1b])
                DVE(lambda e, ps2=ps2, tbm=tbm, t2=t2: e.scalar_tensor_tensor(out=t2, in0=ps2, scalar=QS, in1=tbm[:, 1, :], op0=ALU.mult, op1=ALU.mult), [pb2, tbmb], [t2b])
                POOL(lambda e, t1=t1, t2=t2: e.tensor_tensor(out=t1, in0=t1, in1=t2, op=ALU.add), [t1b, t2b], [t1b])
                o, ob = ost_r.next()
                POOL(lambda e, o=o, t1=t1, rq_=rq_: e.tensor_tensor(out=o, in0=t1, in1=rq_, op=ALU.mult), [t1b, rqb], [ob])
                P.dma("gpsimd", d_qr[c * 128:(c + 1) * 128, sl(g)], o, reads=[ob], writes=[Bd["qr"]])
            for c in range(4):
                ps, pb = pr.next()
                for k in range(2):
                    MM(ps, wkv_t[:, k, c * 128:(c + 1) * 128], ckvT[:, k, sl(g)], k == 0, k == 1, [b_wkv, b_ckv], pb)
                o, ob = ost_r.next()
                DVE(lambda e, o=o, ps=ps, rkv=rkv: e.tensor_tensor(out=o, in0=ps, in1=rkv, op=ALU.mult), [pb, rkvb], [ob])
                P.dma("gpsimd", d_kn[c * 128:(c + 1) * 128, sl(g)], o, reads=[ob], writes=[Bd["kn"]])
            va, vab = vaug_r.next()
            for j in range(4):
                tbi = g * 4 + j
                ps, pb = pr.next()
                for k in range(2):
                    MM(ps, ckvT[:, k, tbi * 128:(tbi + 1) * 128], wkv_t[:, k, 512:1024], k == 0, k == 1, [b_ckv, b_wkv], pb)
                DVE(lambda e, va=va, ps=ps, rt=rt, j=j: e.tensor_scalar(out=va[:, j, :, 0:64], in0=ps.rearrange("p (h e) -> p h e", e=64), scalar1=rt[:, j:j + 1], scalar2=None, op0=ALU.mult), [pb, rtb], [vab])
            P.dma("gpsimd", vav[:, g * 4:(g + 1) * 4, :], va.rearrange("p j h e -> p j (h e)"), reads=[vab], writes=[Bd["va"]])

        P.barrier(scr[:, 0:1])
        A.reset()
        q_r = Rot(A, 2, [S], BF16, "q")
        k_r = Rot(A, 2, [S], BF16, "k")
        v_r = Rot(A, 2, [NB, 128], BF16, "v")
        p_r = Rot(A, 6, [512], BF16, "p")
        e_r = Rot(A, 4, [512], F32, "e")
        t_r = Rot(A, 4, [512], F32, "t")
        lp_r = Rot(A, 4, [512], BF16, "lp")
        csb_r = Rot(A, 2, [512], F32, "csb")
        ys_r = Rot(A, 2, [S], BF16, "ys")
        rden, rdenb = A.tile([512], F32, "rden")
        bcs, bcsb = A.tile([512], F32, "bcs")
        ycs_r = Rot(A, 4, [512], F32, "ycs")
        st_t, st_b = A.tile([64], F32, "state")
        prevb_t, prevb_b = A.tile([64], BF16, "prevb")
        ktok_r = Rot(A, 3, [128], BF16, "ktok")
        sT_r = Rot(A, 6, [128], BF16, "sT")
        tmpc_r = Rot(A, 4, [128], F32, "tmpc")
        cld_r = Rot(A, 3, [2048], F32, "cld")
        ccv_r = Rot(A, 3, [2048], BF16, "ccv")
        pend = conv_piece_list(l + 1) if (OPT_CONVOVL and l + 1 < depth) else []

        def pipeline(n, stages):
            maxs = max(sk for _, sk in stages)
            for step in range(n + maxs):
                for fn, sk in stages:
                    i = step - sk
                    if 0 <= i < n:
                        fn(i)

        tiles_fwd = [(g, kb) for g in range(NG) for kb in range(4 * (g + 1))]
        tiles_rev = [(g, kb) for g in range(NG) for kb in range(4 * (g + 1) - 1, -1, -1)]
        ps_s = PsRot([0, 1, 2, 3])
        ps_y = PsRot([4, 5])
        ps_c = PsRot([6, 7])
        vav4 = d_va.rearrange("(b p) h e -> p b h e", p=128)
        for h in range(8):
            qt, qb = q_r.next()
            kt, kb_ = k_r.next()
            vt, vb = v_r.next()
            P.dma("sync", qt[0:64], d_qn[h * 64:(h + 1) * 64, :], reads=[Bd["qn"]], writes=[qb])
            P.dma("sync", qt[64:96], d_qr[h * 32:(h + 1) * 32, :], reads=[Bd["qr"]], writes=[qb])
            P.dma("sync", kt[0:64], d_kn[h * 64:(h + 1) * 64, :], reads=[Bd["kn"]], writes=[kb_])
            P.dma("sync", kt[64:96], d_krot, reads=[Bd["krot"]], writes=[kb_])
            P.dma("sync", vt[:, :, 0:65], vav4[:, :, h, :], reads=[Bd["va"]], writes=[vb])
            ys, ysb = ys_r.next()
            stt = {}
            ypsd = {}

            def stA(i, qt=qt, qb=qb, kt=kt, kb_=kb_, stt=stt):
                g, kb = tiles_fwd[i]
                if pend and i % 10 == 5:
                    emit_conv(pend.pop(0), cld_r, ccv_r, False)
                sps, spb = ps_s.next()
                MM(sps, kt[0:96, kb * 128:(kb + 1) * 128], qt[0:96, sl(g)], True, True, [kb_, qb], spb)
                pt, ptb = p_r.next()
                ACT(lambda e: e.activation(out=pt, in_=sps, func=AF.Exp), [spb], [ptb])
                d = kb - 4 * g
                if d >= 0:
                    DVE(lambda e: e.tensor_tensor(out=pt, in0=pt, in1=maskA[:, d, :], op=ALU.mult), [ptb, Bcb], [ptb])
                stt[i] = (pt, ptb)

            def stB(i, vt=vt, vb=vb, stt=stt, ypsd=ypsd, ys=ys, ysb=ysb):
                g, kb = tiles_fwd[i]
                nkb = 4 * (g + 1)
                pt, ptb = stt.pop(i)
                if kb == 0:
                    ypsd[g] = ps_y.next()
                yps, ypb = ypsd[g]
                MM(yps[0:65, :], vt[:, kb, 0:65], pt, kb == 0, kb == nkb - 1, [vb, ptb], ypb)
                if kb == nkb - 1:
                    DVE(lambda e: e.reciprocal(out=rden[64:65, :], in_=yps[64:65, :]), [ypb], [rdenb])
                    bps, bpb = ps_c.next()
                    MM(bps[0:64, :], ones32[64:65, 0:64], rden[64:65, :], True, True, [rdenb, Bcb], bpb)
                    ACT(lambda e: e.copy(out=bcs[0:64, :], in_=bps[0:64, :]), [bpb], [bcsb])
                    DVE(lambda e: e.tensor_tensor(out=ys[0:64, sl(g)], in0=yps[0:64, :], in1=bcs[0:64, :], op=ALU.mult), [ypb, bcsb], [ysb])

            pipeline(len(tiles_fwd), [(stA, 0), (stB, 2 * OPT_MLA)])
            P.dma("gpsimd", d_ya[h * 64:(h + 1) * 64, :], ys[0:64], reads=[ysb], writes=[Bd["ya"]])
        while pend:
            emit_conv(pend.pop(0), cld_r, ccv_r, False)

        svv = d_sv.rearrange("(b p) c -> p b c", p=128)
        ps_a = PsRot([0, 1, 2, 3])
        ps_c = PsRot([4, 5])
        ps_y = PsRot([6, 7])
        for h in range(8):
            qt, qb = q_r.next()
            kt, kb_ = k_r.next()
            vt, vb = v_r.next()
            P.dma("sync", qt[0:64], d_sq[h * 64:(h + 1) * 64, :], reads=[Bd["sq"]], writes=[qb])
            P.dma("sync", kt[0:64], d_sk[h * 64:(h + 1) * 64, :], reads=[Bd["sk"]], writes=[kb_])
            P.dma("sync", vt[:, :, 0:64], svv[:, :, h * 64:(h + 1) * 64], reads=[Bd["sv"]], writes=[vb])
            ys, ysb = ys_r.next()
            stt = {}
            gst = {}
            n_t = len(tiles_rev)

            def s0(i, qt=qt, qb=qb, kt=kt, kb_=kb_, stt=stt, gst=gst):
                g, kb = tiles_rev[i]
                nkb = 4 * (g + 1)
                if kb == nkb - 1:
                    cs_t, cs_b = csb_r.next()
                    POOL(lambda e: e.memset(cs_t, 0.0), [], [cs_b])
                    gst[g] = dict(csb=(cs_t, cs_b), yps=ps_y.next())
                aps, apb = ps_a.next()
                MM(aps, kt[0:64, kb * 128:(kb + 1) * 128], qt[0:64, sl(g)], True, False, [kb_, qb], apb)
                et, etb = e_r.next()
                ACT(lambda e: e.activation(out=et, in_=aps, func=AF.Exp), [apb], [etb])
                stt[i] = dict(aps=(aps, apb), et=(et, etb))

            def s1(i, stt=stt):
                g, kb = tiles_rev[i]
                d = kb - 4 * g
                et, etb = stt[i]["et"]
                lp, lpb = lp_r.next()
                ACT(lambda e: e.activation(out=lp, in_=et, func=AF.Ln, bias=1.0), [etb], [lpb])
                if d >= 0:
                    DVE(lambda e: e.tensor_tensor(out=lp, in0=lp, in1=maskS[:, d, :], op=ALU.mult), [lpb, Bcb], [lpb])
                stt[i]["lp"] = (lp, lpb)

            def s2(i, stt=stt):
                g, kb = tiles_rev[i]
                aps, apb = stt[i]["aps"]
                lp, lpb = stt[i]["lp"]
                MM(aps, trineg, lp, False, True, [Bcb, lpb], apb)
                if kb > 0:
                    cps, cpb = ps_c.next()
                    MM(cps, onesb, lp, True, True, [Bcb, lpb], cpb)
                    stt[i]["cps"] = (cps, cpb)

            def sU(i, stt=stt, gst=gst):
                g, kb = tiles_rev[i]
                if kb > 0:
                    cps, cpb = stt[i]["cps"]
                    cs_t, cs_b = gst[g]["csb"]
                    DVE(lambda e: e.tensor_tensor(out=cs_t, in0=cps, in1=cs_t, op=ALU.add), [cpb, cs_b], [cs_b])

            def sT_(i, stt=stt, gst=gst):
                g, kb = tiles_rev[i]
                aps, apb = stt[i]["aps"]
                cs_t, cs_b = gst[g]["csb"]
                tt, ttb = t_r.next()
                DVE(lambda e: e.tensor_tensor(out=tt, in0=aps, in1=cs_t, op=ALU.subtract), [apb, cs_b], [ttb])
                stt[i]["tt"] = (tt, ttb)

            def s4(i, stt=stt):
                g, kb = tiles_rev[i]
                d = kb - 4 * g
                tt, ttb = stt[i]["tt"]
                pt, ptb = p_r.next()
                ACT(lambda e: e.activation(out=pt, in_=tt, func=AF.Exp), [ttb], [ptb])
                if d >= 0:
                    DVE(lambda e: e.tensor_tensor(out=pt, in0=pt, in1=maskS[:, d, :], op=ALU.mult), [ptb, Bcb], [ptb])
                stt[i]["pt"] = (pt, ptb)

            def s5(i, vt=vt, vb=vb, stt=stt, gst=gst, ys=ys, ysb=ysb):
                g, kb = tiles_rev[i]
                nkb = 4 * (g + 1)
                pt, ptb = stt.pop(i)["pt"]
                yps, ypb = gst[g]["yps"]
                MM(yps[0:64, :], vt[:, kb, 0:64], pt, kb == nkb - 1, kb == 0, [vb, ptb], ypb)
                if kb == 0:
                    ACT(lambda e: e.copy(out=ys[0:64, sl(g)], in_=yps[0:64, :]), [ypb], [ysb])

            pipeline(n_t, [(s0, 0), (s1, 1), (s2, 2), (sU, 3), (sT_, 2), (s4, 3), (s5, 4)] if OPT_SB else [(s0, 0), (s1, 0), (s2, 0), (sT_, 0), (sU, 0), (s4, 0), (s5, 0)])
            P.dma("gpsimd", d_yb[h * 64:(h + 1) * 64, :], ys[0:64], reads=[ysb], writes=[Bd["yb"]])

        rvv = d_rv.rearrange("(b p) c -> p b c", p=128)
        dtv = C("dt").rearrange("p (h c) -> p h c", c=128)
        ztv = C("zeta").rearrange("p (a c) -> p a c", c=128)
        xiv = C("xi").rearrange("p (h c) -> p h c", c=128)
        ps_a = PsRot([0, 1, 2])
        ps_b = PsRot([3, 4])
        ps_k = PsRot([5, 6])
        ps_t = PsRot([7])
        for pr_i in range(4):
            qt, qb = q_r.next()
            kt, kb_ = k_r.next()
            vt, vb = v_r.next()
            P.dma("sync", qt, d_rq[pr_i * 128:(pr_i + 1) * 128, :], reads=[Bd["rq"]], writes=[qb])
            P.dma("sync", kt, d_rk[pr_i * 128:(pr_i + 1) * 128, :], reads=[Bd["rk"]], writes=[kb_])
            P.dma("sync", vt, rvv[:, :, pr_i * 128:(pr_i + 1) * 128], reads=[Bd["rv"]], writes=[vb])
            POOL(lambda e: e.memset(st_t, 0.0), [], [st_b])
            POOL(lambda e: e.memset(prevb_t, 0.0), [], [prevb_b])
            ycs_cur = [None, None]
            for n in range(NB):
                cs = slice(n * 128, (n + 1) * 128)
                tps, tpb = ps_t.next()
                tpv = tps[:, :].bitcast(BF16)
                P.op("tensor", lambda e, tpv=tpv, kt=kt, cs=cs: e.transpose(tpv[:, 0:128], kt[:, cs], identb), [kb_, Bcb], [tpb])
                ktk, ktkb = ktok_r.next()
                DVE(lambda e, ktk=ktk, tpv=tpv, pr_i=pr_i: e.tensor_tensor(out=ktk, in0=tpv[:, 0:128], in1=ztv[:, pr_i, :], op=ALU.mult), [tpb, Bc], [ktkb])
                for j in range(2):
                    hh = 2 * pr_i + j
                    rs_ = slice(64 * j, 64 * j + 64)
                    if n % 4 == 0:
                        ycs_cur[j] = ycs_r.next()
                    yc_t, yc_b = ycs_cur[j]
                    aps, apb = ps_a.next()
                    MM(aps[:, 0:128], kt[rs_, cs], qt[rs_, cs], True, True, [kb_, qb], apb)
                    sT, sTb = sT_r.next()
                    DVE(lambda e, sT=sT, aps=aps, hh=hh: e.tensor_tensor(out=sT, in0=aps[:, 0:128], in1=dtv[:, hh, :], op=ALU.mult), [apb, Bc], [sTb])
                    yps, ypb = ps_b.next()
                    MM(yps[0:64, 0:128], vt[:, n, rs_], sT, True, True, [vb, sTb], ypb)
                    xps, xpb = ps_b.next()
                    MM(xps[0:64, 0:128], prevb_t[rs_, :], qt[rs_, cs], True, True, [prevb_b, qb], xpb)
                    tc_, tcb = tmpc_r.next()
                    DVE(lambda e, tc_=tc_, xps=xps, hh=hh: e.tensor_tensor(out=tc_[0:64], in0=xps[0:64, 0:128], in1=xiv[0:64, hh, :], op=ALU.mult), [xpb, Bc], [tcb])
                    off = (n % 4) * 128
                    DVE(lambda e, yc_t=yc_t, yps=yps, tc_=tc_, off=off: e.tensor_tensor(out=yc_t[0:64, off:off + 128], in0=yps[0:64, 0:128], in1=tc_[0:64], op=ALU.add), [ypb, tcb], [yc_b])
                    if n % 4 == 3:
                        P.dma("gpsimd", d_yc[hh * 64:(hh + 1) * 64, (n - 3) * 128:(n + 1) * 128], yc_t[0:64], reads=[yc_b], writes=[Bd["yc"]])
                kps, kpb = ps_k.next()
                MM(kps[:, 0:128], ktk, vt[:, n, :], True, True, [ktkb, vb], kpb)
                for j in range(2):
                    hh = 2 * pr_i + j
                    rs_ = slice(64 * j, 64 * j + 64)
                    cdc = C("cdecay")[rs_, hh:hh + 1]
                    DVE(lambda e, kps=kps, rs_=rs_, cdc=cdc: e.scalar_tensor_tensor(out=st_t[rs_, :], in0=st_t[rs_, :], scalar=cdc, in1=kps[rs_, rs_], op0=ALU.mult, op1=ALU.add), [kpb, st_b, Bc], [st_b])
                POOL(lambda e: e.tensor_copy(out=prevb_t, in_=st_t), [st_b], [prevb_b])

        P.barrier(scr[:, 0:1])
        A.reset()
        xg_r = Rot(A, 2, [8, 512], F32, "xg")
        hg, hgb = A.tile([8, 512], BF16, "hg")
        sq_r = Rot(A, 3, [512], BF16, "sq")
        f32_r = Rot(A, 6, [512], F32, "f32")
        ya_r = Rot(A, 2, [4, 512], BF16, "ya")
        yb_r = Rot(A, 2, [4, 512], BF16, "yb")
        yc_r = Rot(A, 1, [4, 512], F32, "yc")
        ycg, ycgb = A.tile([4, 512], BF16, "ycg")
        mT, mTb = A.tile([8, 512], BF16, "mT")
        h2, h2b = A.tile([8, 512], BF16, "h2")
        act, actb = A.tile([32, 512], BF16, "act")
        wg_r = Rot(A, 3, [8, 128], BF16, "wg")
        wb_r = Rot(A, 3, [4, 128], BF16, "wbr")
        wo_r = Rot(A, 2, [8, 128], BF16, "wo")
        wu_r = Rot(A, 2, [8, 512], BF16, "wu")
        wd_r = Rot(A, 2, [32, 128], BF16, "wd")
        macc, maccb = A.tile([512], F32, "macc")
        pr = PsRot([0, 1, 2, 3, 4, 5, 6, 7])
        wgv = wview("WG", l)
        wbv = Wb["WB"][l].rearrange("(n k p) d -> p n k d", p=128, k=4)
        wov = wview("WO", l)
        wuv = wview("WU", l)
        wdv = wview("WD", l)
        yav = d_ya.rearrange("(k p) s -> p k s", p=128)
        ybv = d_yb.rearrange("(k p) s -> p k s", p=128)
        ycv = d_yc.rearrange("(k p) s -> p k s", p=128)
        bdm = C("bd")
        last = (l == depth - 1)
        for g in range(NG):
            xg, bxg = xg_r.next()
            P.dma("sync", xg, xv[:, :, sl(g)], reads=[Bx[g]], writes=[bxg])
            yat, yab = ya_r.next()
            ybt, ybb = yb_r.next()
            yct, ycb = yc_r.next()
            P.dma("sync", yat, yav[:, :, sl(g)], reads=[Bd["ya"]], writes=[yab])
            P.dma("sync", ybt, ybv[:, :, sl(g)], reads=[Bd["yb"]], writes=[ybb])
            P.dma("sync", yct, ycv[:, :, sl(g)], reads=[Bd["yc"]], writes=[ycb])
            ps, pb = pr.next()
            for k in range(8):
                sq, bsq = sq_r.next()
                ACT(lambda e, sq=sq, xg=xg, k=k: e.activation(out=sq, in_=xg[:, k, :], func=AF.Square), [bxg], [bsq])
                MM(ps, onesb, sq, k == 0, k == 7, [bsq, Bcb], pb)
            tmp, tmpb = f32_r.next()
            rs, rsb = f32_r.next()
            rstd_from_ps(ps, pb, 1024.0, rs, rsb, tmp, tmpb)
            for k in range(8):
                DVE(lambda e, k=k, xg=xg, rs=rs: e.tensor_tensor(out=hg[:, k, :], in0=xg[:, k, :], in1=rs, op=ALU.mult), [bxg, rsb], [hgb])
            for c in range(4):
                wt, wtb = wg_r.next()
                P.dma("sync", wt, wgv[:, :, c * 128:(c + 1) * 128], reads=[Bwl[l]], writes=[wtb])
                gps, gpb = pr.next()
                for k in range(8):
                    MM(gps, wt[:, k, :], hg[:, k, :], k == 0, k == 7, [wtb, hgb], gpb)
                sil, silb = f32_r.next()
                ACT(lambda e, sil=sil, gps=gps: e.activation(out=sil, in_=gps, func=AF.Silu), [gpb], [silb])
                mps, mpb = pr.next()
                MM(mps, bdm, yct[:, c, :], True, True, [Bc, ycb], mpb)
                cen, cenb = f32_r.next()
                DVE(lambda e, cen=cen, mps=mps, yct=yct, c=c: e.scalar_tensor_tensor(out=cen, in0=mps, scalar=-1.0, in1=yct[:, c, :], op0=ALU.mult, op1=ALU.add), [mpb, ycb], [cenb])
                sq32, sq32b = f32_r.next()
                POOL(lambda e, sq32=sq32, cen=cen: e.tensor_tensor(out=sq32, in0=cen, in1=cen, op=ALU.mult), [cenb], [sq32b])
                vps, vpb = pr.next()
                MM(vps, bdm, sq32, True, True, [Bc, sq32b], vpb)
                tmp, tmpb = f32_r.next()
                rv_, rvb = f32_r.next()
                rstd_from_ps(vps, vpb, 1.0, rv_, rvb, tmp, tmpb)
                gcol = gsb[:, l, 21 + c:22 + c]
                DVE(lambda e, cen=cen, rv_=rv_, gcol=gcol: e.scalar_tensor_tensor(out=cen, in0=cen, scalar=gcol, in1=rv_, op0=ALU.mult, op1=ALU.mult), [cenb, rvb, Bg], [cenb])
                POOL(lambda e, cen=cen, sil=sil, c=c: e.tensor_tensor(out=ycg[:, c, :], in0=cen, in1=sil, op=ALU.mult), [cenb, silb], [ycgb])
            for db in range(8):
                for n in range(3):
                    ysrc, ysb_ = ((yat, yab), (ybt, ybb), (ycg, ycgb))[n]
                    wb_t, wb_b = wb_r.next()
                    P.dma("sync", wb_t, wbv[:, n, :, db * 128:(db + 1) * 128], reads=[Bwl[l]], writes=[wb_b])
                    wt, wtb = wg_r.next()
                    P.dma("sync", wt, wgv[:, :, 512 + n * 1024 + db * 128:512 + n * 1024 + (db + 1) * 128], reads=[Bwl[l]], writes=[wtb])
                    ups, upb = pr.next()
                    for k in range(4):
                        MM(ups, wb_t[:, k, :], ysrc[:, k, :], k == 0, k == 3, [wb_b, ysb_], upb)
                    gps, gpb = pr.next()
                    for k in range(8):
                        MM(gps, wt[:, k, :], hg[:, k, :], k == 0, k == 7, [wtb, hgb], gpb)
                    sg, sgb = f32_r.next()
                    ACT(lambda e, sg=sg, gps=gps: e.activation(out=sg, in_=gps, func=AF.Sigmoid), [gpb], [sgb])
                    if n == 0:
                        DVE(lambda e, ups=ups, sg=sg: e.tensor_tensor(out=macc, in0=ups, in1=sg, op=ALU.mult), [upb, sgb], [maccb])
                    else:
                        DVE(lambda e, ups=ups, sg=sg: e.tensor_tensor(out=sg, in0=ups, in1=sg, op=ALU.mult), [upb, sgb], [sgb])
                        if n == 1:
                            POOL(lambda e, sg=sg: e.tensor_tensor(out=macc, in0=macc, in1=sg, op=ALU.add), [maccb, sgb], [maccb])
                        else:
                            POOL(lambda e, sg=sg, db=db: e.tensor_tensor(out=mT[:, db, :], in0=macc, in1=sg, op=ALU.add), [maccb, sgb], [mTb])
            for ob_ in range(8):
                wt, wtb = wo_r.next()
                P.dma("sync", wt, wov[:, :, ob_ * 128:(ob_ + 1) * 128], reads=[Bwl[l]], writes=[wtb])
                ops_, opb = pr.next()
                for k in range(8):
                    MM(ops_, wt[:, k, :], mT[:, k, :], k == 0, k == 7, [wtb, mTb], opb)
                DVE(lambda e, xg=xg, ops_=ops_, ob_=ob_: e.tensor_tensor(out=xg[:, ob_, :], in0=ops_, in1=xg[:, ob_, :], op=ALU.add), [opb, bxg], [bxg])
            ps, pb = pr.next()
            for k in range(8):
                sq, bsq = sq_r.next()
                ACT(lambda e, sq=sq, xg=xg, k=k: e.activation(out=sq, in_=xg[:, k, :], func=AF.Square), [bxg], [bsq])
                MM(ps, onesb, sq, k == 0, k == 7, [bsq, Bcb], pb)
            tmp, tmpb = f32_r.next()
            rs, rsb = f32_r.next()
            rstd_from_ps(ps, pb, 1024.0, rs, rsb, tmp, tmpb)
            for k in range(8):
                DVE(lambda e, k=k, xg=xg, rs=rs: e.tensor_tensor(out=h2[:, k, :], in0=xg[:, k, :], in1=rs, op=ALU.mult), [bxg, rsb], [h2b])
            for fq in range(8):
                wt, wtb = wu_r.next()
                P.dma("sync", wt, wuv[:, :, fq * 512:(fq + 1) * 512], reads=[Bwl[l]], writes=[wtb])
                for fi in range(4):
                    f = fq * 4 + fi
                    ups, upb = pr.next()
                    for k in range(8):
                        MM(ups, wt[:, k, fi * 128:(fi + 1) * 128], h2[:, k, :], k == 0, k == 7, [wtb, h2b], upb)
                    rl, rlb = f32_r.next()
                    ACT(lambda e, rl=rl, ups=ups: e.activation(out=rl, in_=ups, func=AF.Relu), [upb], [rlb])
                    eng = DVE if f % 2 == 0 else POOL
                    eng(lambda e, rl=rl, f=f: e.tensor_tensor(out=act[:, f, :], in0=rl, in1=rl, op=ALU.mult), [rlb], [actb])
            for ob_ in range(8):
                wt, wtb = wd_r.next()
                P.dma("sync", wt, wdv[:, :, ob_ * 128:(ob_ + 1) * 128], reads=[Bwl[l]], writes=[wtb])
                ops_, opb = pr.next()
                for f in range(32):
                    MM(ops_, wt[:, f, :], act[:, f, :], f == 0, f == 31, [wtb, actb], opb)
                DVE(lambda e, xg=xg, ops_=ops_, ob_=ob_: e.tensor_tensor(out=xg[:, ob_, :], in0=ops_, in1=xg[:, ob_, :], op=ALU.add), [opb, bxg], [bxg])
            if not last:
                P.dma("gpsimd", xv[:, :, sl(g)], xg, reads=[bxg], writes=[Bx[g]])
            else:
                ps, pb = pr.next()
                for k in range(8):
                    sq, bsq = sq_r.next()
                    ACT(lambda e, sq=sq, xg=xg, k=k: e.activation(out=sq, in_=xg[:, k, :], func=AF.Square), [bxg], [bsq])
                    MM(ps, onesb, sq, k == 0, k == 7, [bsq, Bcb], pb)
                tmp, tmpb = f32_r.next()
                rs, rsb = f32_r.next()
                rstd_from_ps(ps, pb, 1024.0, rs, rsb, tmp, tmpb)
                for k in range(8):
                    gcol = gsb[:, 0, 25 + k:26 + k]
                    DVE(lambda e, k=k, xg=xg, rs=rs, gcol=gcol: e.scalar_tensor_tensor(out=xg[:, k, :], in0=xg[:, k, :], scalar=gcol, in1=rs, op0=ALU.mult, op1=ALU.mult), [bxg, rsb, Bg], [bxg])
                fin.append(P.dma("gpsimd", outT.rearrange("(k p) s -> p k s", p=128)[:, :, sl(g)], xg, reads=[bxg], writes=[Bout]))

    return nc, P


fin = []
Bout = Buf("out")


def build_and_emit(S=SEQ, depth=DEPTH, debug=()):
    global fin, Bout
    fin = []
    Bout = Buf("out")
    nc, P = build(S, depth, debug)
    if not fin:
        raise RuntimeError("no output")
    stats = P.emit(final_wait_ops=fin)
    return nc, stats


def kernel(**inputs):
    x = np.asarray(inputs["x"], np.float32)
    B, S, _ = x.shape
    positions = np.asarray(inputs["positions"]).astype(np.int32)
    com = host_layout(inputs)
    consts = build_consts()
    masks = build_masks()
    depth = com["WA"].shape[0]
    nc, _ = build_and_emit(S, depth)
    in_maps = []
    for b in range(B):
        m = dict(com)
        m["xin"] = np.ascontiguousarray(x[b].T)
        m["pos"] = np.ascontiguousarray(positions[b][None, :])
        m["consts"] = consts
        m["cmask"] = masks
        in_maps.append(m)
    res = run_bass_kernel_spmd(nc, in_maps, core_ids=list(range(B)))
    out = np.stack([np.ascontiguousarray(res.results[b]["outT"].T) for b in range(B)], 0)
    return out.astype(np.float32)
```

```python
import math
import os
import numpy as np
import concourse.bass as bass
import concourse.mybir as mybir
from concourse.bass_utils import run_bass_kernel_spmd

F32 = mybir.dt.float32
BF16 = mybir.dt.bfloat16
I32 = mybir.dt.int32
U8 = mybir.dt.uint8
ALU = mybir.AluOpType
AF = mybir.ActivationFunctionType

D = 1024
EPS = 1e-6
NCORES = 8
DEPTH = 4
SEQ = 4096
QS = 96 ** -0.5
OPT_P0ACT = int(os.environ.get('KV_P0ACT', '1'))
OPT_MLA = int(os.environ.get('KV_MLA', '1'))
OPT_SB = int(os.environ.get('KV_SB', '1'))
OPT_RET = int(os.environ.get('KV_RET', '1'))
OPT_CONVOVL = int(os.environ.get('KV_CONVOVL', '1'))


class Buf:
    __slots__ = ("name", "last_w", "readers")

    def __init__(self, name, reg=None):
        self.name = name
        self.last_w = None
        self.readers = []
        if reg is not None:
            reg.append(self)


class Prog:
    ENGS = ("tensor", "vector", "scalar", "gpsimd", "sync")

    def __init__(self, nc, n_dma_sems=32):
        self.nc = nc
        self.ops = []
        self.n_dma_sems = n_dma_sems
        self.allbufs = []
        self.last_barrier = None

    def buf(self, name="b"):
        b = Buf(name, self.allbufs)
        b.last_w = self.last_barrier
        return b

    def op(self, eng, fn, reads=(), writes=(), dma=False):
        idx = len(self.ops)
        deps = set()
        for b in reads:
            if b.last_w is not None:
                deps.add(b.last_w)
        for b in writes:
            if b.last_w is not None:
                deps.add(b.last_w)
            deps.update(b.readers)
        deps.discard(idx)
        self.ops.append(dict(eng=eng, fn=fn, deps=deps, dma=dma, signal=False))
        for b in reads:
            b.readers.append(idx)
        for b in writes:
            b.last_w = idx
            b.readers = []
        return idx

    def dma(self, eng, out, in_, reads=(), writes=()):
        return self.op(eng, lambda e: e.dma_start(out=out, in_=in_), reads, writes, dma=True)

    def barrier(self, scratch_ap):
        bs = list(self.allbufs)
        i = self.op("vector", lambda e: e.memset(scratch_ap, 0.0), bs, bs)
        self.last_barrier = i
        return i

    def emit(self, final_wait_ops=()):
        nc = self.nc
        ops = self.ops
        for i, o in enumerate(ops):
            nd = set()
            for d in o["deps"]:
                p = ops[d]
                if (not p["dma"]) and (not o["dma"]) and p["eng"] == o["eng"] and o["eng"] == "tensor":
                    continue
                nd.add(d)
            o["deps"] = nd
            for d in nd:
                ops[d]["signal"] = True
        for d in final_wait_ops:
            ops[d]["signal"] = True
        eng_sem = {e: nc.alloc_semaphore(name=f"s_{e}") for e in self.ENGS}
        dma_sems = [nc.alloc_semaphore(name=f"d_{i}") for i in range(self.n_dma_sems)]
        cnt = {e: 0 for e in self.ENGS}
        dma_cnt = [0] * self.n_dma_sems
        dma_rr = 0
        for o in ops:
            if o["dma"]:
                s = dma_rr % self.n_dma_sems
                dma_rr += 1
                o["prev_val"] = dma_cnt[s]
                dma_cnt[s] += 16
                o["sem"] = ("d", s)
                o["val"] = dma_cnt[s]
            elif o["signal"]:
                cnt[o["eng"]] += 1
                o["sem"] = ("e", o["eng"])
                o["val"] = cnt[o["eng"]]
        per_eng = {e: [] for e in self.ENGS}
        for i, o in enumerate(ops):
            per_eng[o["eng"]].append(i)

        def semof(key):
            return dma_sems[key[1]] if key[0] == "d" else eng_sem[key[1]]

        def run_engine(ename, eng, final=False):
            waited = {}
            for i in per_eng[ename]:
                o = ops[i]
                need = {}
                for d in o["deps"]:
                    p = ops[d]
                    k = p["sem"]
                    if p["val"] > need.get(k, 0):
                        need[k] = p["val"]
                if o["dma"] and o["prev_val"] > 0:
                    k = o["sem"]
                    need[k] = max(need.get(k, 0), o["prev_val"])
                for k, v in need.items():
                    if waited.get(k, 0) < v:
                        eng.wait_ge(semof(k), v)
                        waited[k] = v
                ins = o["fn"](eng)
                if o["dma"]:
                    ins.then_inc(semof(o["sem"]), 16)
                elif o["signal"]:
                    ins.then_inc(semof(o["sem"]), 1)
            if final:
                need = {}
                for d in final_wait_ops:
                    p = ops[d]
                    need[p["sem"]] = max(need.get(p["sem"], 0), p["val"])
                for k, v in need.items():
                    if waited.get(k, 0) < v:
                        eng.wait_ge(semof(k), v)

        with nc.Block() as block:
            @block.tensor
            def _(e):
                run_engine("tensor", e)

            @block.vector
            def _(e):
                run_engine("vector", e)

            @block.scalar
            def _(e):
                run_engine("scalar", e)

            @block.gpsimd
            def _(e):
                run_engine("gpsimd", e)

            @block.sync
            def _(e):
                run_engine("sync", e, final=True)
        return {e: len(per_eng[e]) for e in self.ENGS}


class Arena:
    def __init__(self, ap_u8, P):
        self.ap = ap_u8
        self.size = ap_u8.shape[1]
        self.off = 0
        self.P = P

    def reset(self):
        self.off = 0

    def tile(self, free_shape, dtype, name="t"):
        esz = 2 if dtype == BF16 else 4
        n = 1
        for s in free_shape:
            n *= s
        nbytes = (n * esz + 31) // 32 * 32
        assert self.off + nbytes <= self.size, (name, self.off, nbytes, self.size)
        v = self.ap[:, self.off:self.off + nbytes]
        self.off += nbytes
        v = v[:, 0:n * esz].bitcast(dtype)
        if len(free_shape) == 2:
            v = v.rearrange("p (a b) -> p a b", b=free_shape[1])
        elif len(free_shape) == 3:
            v = v.rearrange("p (a b c) -> p a b c", b=free_shape[1], c=free_shape[2])
        return v, self.P.buf(name)


class Rot:
    def __init__(self, arena, n, free_shape, dtype, name="r"):
        self.items = [arena.tile(free_shape, dtype, f"{name}{i}") for i in range(n)]
        self.i = 0

    def next(self):
        it = self.items[self.i % len(self.items)]
        self.i += 1
        return it


CONST_LAYOUT = {}


def build_consts():
    cols = []

    def add(name, arr):
        arr = np.asarray(arr, np.float64).reshape(128, -1)
        off = sum(c.shape[1] for c in cols)
        CONST_LAYOUT[name] = (off, arr.shape[1])
        cols.append(arr)

    bd = np.zeros((128, 128))
    bd[0:64, 0:64] = 1.0 / 64
    bd[64:128, 64:128] = 1.0 / 64
    add("bd", bd)
    h = np.arange(8)
    lg = np.log1p(-np.exp2(-5.0 - h).astype(np.float32)).astype(np.float32).astype(np.float64)
    m = np.arange(128)[:, None]
    c = np.arange(128)[None, :]
    dt = np.stack([np.where(c >= m, np.exp(np.maximum(c - m, 0) * lg[hh]), 0.0) for hh in range(8)], 1)
    add("dt", dt)
    z = np.zeros((128, 4, 128))
    for pr in range(4):
        for col in range(128):
            hh = 2 * pr + col // 64
            z[:, pr, col] = np.exp((127 - np.arange(128)) * lg[hh])
    add("zeta", z)
    xi = np.zeros((128, 8, 128))
    for hh in range(8):
        xi[:, hh, :] = np.exp((np.arange(128) + 1.0) * lg[hh])[None, :]
    add("xi", xi)
    r = np.arange(128)
    invf_r = (10000.0 ** (-(2.0 * (r % 32)) / 64)).astype(np.float32)
    invf_m = (10000.0 ** (-(2.0 * (r % 16)) / 32)).astype(np.float32)
    add("invf", np.stack([invf_r, invf_m], 1))
    sg_r = np.where((r % 64) < 32, -1.0, 1.0)
    sg_m = np.where((r % 32) < 16, -1.0, 1.0)
    add("sgn", np.stack([sg_r, sg_m], 1))
    cd = np.exp(128.0 * lg)
    add("cdecay", np.tile(cd[None, :], (128, 1)))
    return np.concatenate(cols, 1).astype(np.float32)


IN_OFFS = dict(cq=(0, 384), ckv=(384, 640), kpe=(640, 672), sbq=(672, 1184), sbk=(1184, 1696), sbv=(1696, 2208),
               rq=(2208, 2720), rk=(2720, 3232), rv=(3232, 3744), rg=(3744, 4256), gate=(4256, 7328))
WA_COLS = 3840
WA_OFF = dict(cq=0, ckv=384, sbq=640, sbk=1152, rq=1664, rqs=2176, rk=2688, rks=3200, kpe=3712, kpes=3744)


def build_masks():
    p = np.arange(128)[:, None]
    j = np.arange(512)[None, :]
    mA = np.stack([(j >= p + 128 * d) for d in range(4)], 1).astype(np.float32).reshape(128, -1)
    mS = np.stack([(j > p + 128 * d) for d in range(4)], 1).astype(np.float32).reshape(128, -1)
    jj = np.arange(128)[:, None]
    ss = np.arange(128)[None, :]
    tri = -(jj >= ss).astype(np.float32)
    return np.concatenate([mA, mS, tri, np.eye(128, dtype=np.float32)], 1)


def host_layout(inputs):
    w_in = np.asarray(inputs["w_in"])
    depth = w_in.shape[0]

    def sl(name):
        a, b = IN_OFFS[name]
        return w_in[:, :, a:b]

    def swap_heads(w, hd):
        L, K, N = w.shape
        w4 = w.reshape(L, K, N // hd, hd)
        return np.concatenate([w4[..., hd // 2:], w4[..., :hd // 2]], -1).reshape(L, K, N)

    WA = np.concatenate([sl("cq"), sl("ckv"), sl("sbq"), sl("sbk"), sl("rq"), swap_heads(sl("rq"), 64),
                         sl("rk"), swap_heads(sl("rk"), 64), sl("kpe"), swap_heads(sl("kpe"), 32)], -1)
    WV = np.concatenate([sl("sbv"), sl("rv")], -1)
    WG = np.concatenate([sl("rg"), sl("gate")], -1)
    wuq = np.asarray(inputs["mla_w_uq"]).reshape(depth, 384, 8, 96)
    qn = wuq[..., :64].reshape(depth, 384, 512)
    qr = wuq[..., 64:].reshape(depth, 384, 256)
    WQ = np.concatenate([qn, qr, swap_heads(qr, 32)], -1)
    wukv = np.asarray(inputs["mla_w_ukv"]).reshape(depth, 256, 8, 128)
    WKV = np.concatenate([wukv[..., :64].reshape(depth, 256, 512), wukv[..., 64:].reshape(depth, 256, 512)], -1)
    WB = np.asarray(inputs["w_branch"]).reshape(depth, 1536, 1024)

    def g128(v):
        L, N = v.shape
        return v.reshape(L, N // 128, 128).transpose(0, 2, 1)

    gains = np.concatenate([g128(np.asarray(inputs["norm_mix_g"])), g128(np.asarray(inputs["mla_q_norm_g"])),
                            g128(np.asarray(inputs["mla_kv_norm_g"])), g128(np.asarray(inputs["norm_mlp_g"])),
                            g128(np.asarray(inputs["ret_norm_g"])),
                            np.broadcast_to(g128(np.asarray(inputs["final_norm_g"])[None]), (depth, 128, 8))], -1)
    com = dict(WA=WA, WV=WV, WG=WG, WQ=WQ, WKV=WKV, WB=WB, WO=np.asarray(inputs["w_out"]),
               WU=np.asarray(inputs["w_up"]), WD=np.asarray(inputs["w_down"]), gains=gains)
    return {k: np.ascontiguousarray(v, dtype=np.float32) for k, v in com.items()}


WSHAPES = dict(WA=(1024, 3776), WV=(1024, 1024), WG=(1024, 3584), WQ=(384, 1024), WKV=(256, 1024),
               WB=(1536, 1024), WO=(1024, 1024), WU=(1024, 4096), WD=(4096, 1024))
WGAIN = dict(WA=0, WV=0, WG=0, WQ=8, WKV=11, WU=13)


def build(S=SEQ, depth=DEPTH, debug=()):
    NG = S // 512
    NB = S // 128
    nc = bass.Bass("TRN2", target_bir_lowering=False)
    P = Prog(nc)
    consts_np = build_consts()
    NCONST = consts_np.shape[1]

    def dram(name, shape, dt, kind=None):
        if kind is None:
            kind = "ExternalOutput" if name in debug else "Internal"
        return nc.dram_tensor(name, list(shape), dt, kind=kind).ap()

    xin = dram("xin", [D, S], F32, "ExternalInput")
    pos = dram("pos", [1, S], I32, "ExternalInput")
    cin = dram("consts", [128, NCONST], F32, "ExternalInput")
    cmin = dram("cmask", [128, 4352], F32, "ExternalInput")
    gin = dram("gains", [depth, 128, 33], F32, "ExternalInput")
    Win = {k: dram(k, [depth] + list(s), F32, "ExternalInput") for k, s in WSHAPES.items()}
    outT = dram("outT", [D, S], F32, "ExternalOutput")
    Wb = {k: dram("b" + k, [depth] + list(s), BF16) for k, s in WSHAPES.items()}
    xres = dram("xres", [D, S], F32)
    tabR = dram("tabR", [128, 2, S], F32)
    tabM = dram("tabM", [128, 2, S], F32)
    d_sq = dram("d_sq", [512, S], BF16)
    d_sk = dram("d_sk", [512, S], BF16)
    d_sv = dram("d_sv", [S, 512], BF16)
    d_rq = dram("d_rq", [512, S], BF16)
    d_rk = dram("d_rk", [512, S], BF16)
    d_rv = dram("d_rv", [S, 512], BF16)
    d_krot = dram("d_krot", [32, S], BF16)
    d_qn = dram("d_qn", [512, S], BF16)
    d_qr = dram("d_qr", [256, S], BF16)
    d_kn = dram("d_kn", [512, S], BF16)
    d_va = dram("d_va", [S, 8, 65], BF16)
    d_ya = dram("d_ya", [512, S], BF16)
    d_yb = dram("d_yb", [512, S], BF16)
    d_yc = dram("d_yc", [512, S], F32)
    Bx = [P.buf(f"x{g}") for g in range(NG)]
    Bwl = [P.buf(f"wb16_{l}") for l in range(depth)]
    Btab = P.buf("tab")
    Bd = {n: P.buf(n) for n in "sq sk sv rq rk rv krot qn qr kn va ya yb yc".split()}

    cst = nc.alloc_sbuf_tensor("cst", [128, NCONST], F32)
    Bc = P.buf("cst")
    gsb = nc.alloc_sbuf_tensor("gsb", [128, depth, 33], F32)
    Bg = P.buf("gsb")
    PERS_BF = 4 * 512 * 2 + 128 * 4
    cb = nc.alloc_sbuf_tensor("cb", [128, 4 * 512 * 2 + 128 * 3], BF16)
    Bcb = P.buf("cb")
    ones32 = nc.alloc_sbuf_tensor("ones32", [128, 128], F32)
    scr = nc.alloc_sbuf_tensor("scr", [128, 8], F32)
    ARENA_BYTES = 186 * 1024
    arena_t = nc.alloc_sbuf_tensor("arena", [128, ARENA_BYTES], U8)
    A = Arena(arena_t[:, :], P)
    psb = [nc.alloc_psum_tensor(f"ps{i}", [128, 512], F32)[:, :] for i in range(8)]
    PB = [P.buf(f"ps{i}") for i in range(8)]

    class PsRot:
        def __init__(self, idxs):
            self.idxs = idxs
            self.i = 0

        def next(self):
            k = self.idxs[self.i % len(self.idxs)]
            self.i += 1
            return psb[k], PB[k]

    def C(name):
        o, n = CONST_LAYOUT[name]
        return cst[:, o:o + n]

    maskA = cb[:, 0:2048].rearrange("p (d j) -> p d j", j=512)
    maskS = cb[:, 2048:4096].rearrange("p (d j) -> p d j", j=512)
    trineg = cb[:, 4096:4224]
    identb = cb[:, 4224:4352]
    onesb = cb[:, 4352:4480]

    P.dma("sync", cst[:], cin, writes=[Bc])
    P.dma("sync", gsb[:], gin.rearrange("l p c -> p l c"), writes=[Bg])
    A.reset()
    cm_t, cm_b = A.tile([4352], F32, "cmask")
    P.dma("sync", cm_t, cmin, writes=[cm_b])
    P.op("vector", lambda e: e.tensor_copy(out=cb[:, 0:4352], in_=cm_t), [cm_b], [Bcb])
    P.barrier(scr[:, 0:1])
    P.op("vector", lambda e: e.memset(onesb, 1.0), [], [Bcb])
    P.op("vector", lambda e: e.memset(ones32[:], 1.0), [], [Bcb])
    P.dma("sync", xres, xin, writes=Bx)

    A.reset()
    TW = min(S, 2048)
    pi_t, b_pi = A.tile([TW], I32, "pi")
    pf_t, b_pf = A.tile([TW], F32, "pf")
    ang_t, _ = A.tile([TW], F32, "ang")
    t4_t, _ = A.tile([TW], F32, "t4")
    ki_t, _ = A.tile([TW], I32, "ki")
    kf_t, _ = A.tile([TW], F32, "kf")
    mk_t, _ = A.tile([TW], F32, "mk")
    tb_t, b_tb = A.tile([2, TW], F32, "tabst")
    C1 = 6.28125
    C2 = 2 * math.pi - C1
    bt = b_pf

    def reduce_and_sin(shift, out_ap, post_sign_col):
        V = lambda f: P.op("vector", f, [bt, Bc], [bt])
        V(lambda e: e.tensor_scalar(out=ang_t, in0=ang_t, scalar1=float(shift), scalar2=None, op0=ALU.add))
        V(lambda e: e.tensor_scalar(out=t4_t, in0=ang_t, scalar1=1.0 / (2 * math.pi), scalar2=0.5, op0=ALU.mult, op1=ALU.add))
        V(lambda e: e.tensor_copy(out=ki_t, in_=t4_t))
        V(lambda e: e.tensor_copy(out=kf_t, in_=ki_t))
        V(lambda e: e.scalar_tensor_tensor(out=t4_t, in0=kf_t, scalar=-C1, in1=ang_t, op0=ALU.mult, op1=ALU.add))
        V(lambda e: e.scalar_tensor_tensor(out=t4_t, in0=kf_t, scalar=-C2, in1=t4_t, op0=ALU.mult, op1=ALU.add))
        V(lambda e: e.tensor_scalar(out=mk_t, in0=t4_t, scalar1=-math.pi, scalar2=2 * math.pi, op0=ALU.is_lt, op1=ALU.mult))
        V(lambda e: e.tensor_tensor(out=t4_t, in0=t4_t, in1=mk_t, op=ALU.add))
        V(lambda e: e.tensor_scalar(out=mk_t, in0=t4_t, scalar1=math.pi, scalar2=-2 * math.pi, op0=ALU.is_gt, op1=ALU.mult))
        V(lambda e: e.tensor_tensor(out=t4_t, in0=t4_t, in1=mk_t, op=ALU.add))
        V(lambda e: e.tensor_scalar(out=t4_t, in0=t4_t, scalar1=-3.1415925, scalar2=3.1415925, op0=ALU.max, op1=ALU.min))
        P.op("scalar", lambda e: e.activation(out=out_ap, in_=t4_t, func=AF.Sin), [bt], [b_tb])
        if post_sign_col is not None:
            P.op("vector", lambda e: e.tensor_scalar(out=out_ap, in0=out_ap, scalar1=post_sign_col, scalar2=None, op0=ALU.mult), [b_tb, Bc], [b_tb])

    for which, tab in ((0, tabR), (1, tabM)):
        for t0 in range(0, S, TW):
            P.dma("sync", pi_t, pos[:, t0:t0 + TW].partition_broadcast(128), writes=[b_pi])
            P.op("vector", lambda e: e.tensor_copy(out=pf_t, in_=pi_t), [b_pi], [bt])
            invc = C("invf")[:, which:which + 1]
            sgc = C("sgn")[:, which:which + 1]
            P.op("vector", lambda e, invc=invc: e.tensor_scalar(out=ang_t, in0=pf_t, scalar1=invc, scalar2=None, op0=ALU.mult), [bt, Bc], [bt])
            reduce_and_sin(math.pi / 2, tb_t[:, 0, :], None)
            P.op("vector", lambda e, invc=invc: e.tensor_scalar(out=ang_t, in0=pf_t, scalar1=invc, scalar2=None, op0=ALU.mult), [bt, Bc], [bt])
            reduce_and_sin(0.0, tb_t[:, 1, :], sgc)
            P.dma("gpsimd", tab[:, :, t0:t0 + TW], tb_t, reads=[b_tb], writes=[Btab])

    P.barrier(scr[:, 0:1])
    A.reset()
    PW = 2048
    ldr0 = Rot(A, 6, [PW], F32, "wld")
    cvr0 = Rot(A, 6, [PW], BF16, "wcv")
    conv_cnt = [0]

    def conv_piece_list(l):
        out = []
        for k, (K, N) in WSHAPES.items():
            for kc in range(K // 128):
                for c0 in range(0, N, PW):
                    out.append((l, k, kc, c0, min(PW, N - c0)))
        return out

    def emit_conv(piece, ldr, cvr, allow_act):
        l, k, kc, c0, n = piece
        goff = WGAIN.get(k)
        lt, lb = ldr.next()
        ct, cbf = cvr.next()
        P.dma("sync", lt[:, 0:n], Win[k][l, kc * 128:(kc + 1) * 128, c0:c0 + n], writes=[lb])
        useact = allow_act and (conv_cnt[0] % 2 == 1) and OPT_P0ACT
        conv_cnt[0] += 1
        if goff is None:
            if useact:
                P.op("scalar", lambda e: e.copy(out=ct[:, 0:n], in_=lt[:, 0:n]), [lb], [cbf])
            else:
                P.op("vector", lambda e: e.tensor_copy(out=ct[:, 0:n], in_=lt[:, 0:n]), [lb], [cbf])
        else:
            gcol = gsb[:, l, goff + kc:goff + kc + 1]
            if useact:
                P.op("scalar", lambda e: e.mul(out=ct[:, 0:n], in_=lt[:, 0:n], mul=gcol), [lb, Bg], [cbf])
            else:
                P.op("vector", lambda e: e.tensor_scalar(out=ct[:, 0:n], in0=lt[:, 0:n], scalar1=gcol, scalar2=None, op0=ALU.mult), [lb, Bg], [cbf])
        P.dma("gpsimd", Wb[k][l, kc * 128:(kc + 1) * 128, c0:c0 + n], ct[:, 0:n], reads=[cbf], writes=[Bwl[l]])

    n_up = depth if not OPT_CONVOVL else 1
    for l in range(n_up):
        for piece in conv_piece_list(l):
            emit_conv(piece, ldr0, cvr0, True)

    def wview(k, l):
        return Wb[k][l].rearrange("(k p) n -> p k n", p=128)

    def ACT(fn, r, w):
        return P.op("scalar", fn, r, w)

    def DVE(fn, r, w):
        return P.op("vector", fn, r, w)

    def POOL(fn, r, w):
        return P.op("gpsimd", fn, r, w)

    def MM(ps, lhsT, rhs, start, stop, r, w):
        return P.op("tensor", lambda e: e.matmul(ps, lhsT=lhsT, rhs=rhs, start=start, stop=stop), r, [w])

    def rstd_from_ps(ps, pb, n, out_ap, out_b, tmp, tmpb):
        ACT(lambda e: e.activation(out=tmp, in_=ps, func=AF.Ln, bias=EPS, scale=1.0 / n), [pb], [tmpb])
        ACT(lambda e: e.activation(out=out_ap, in_=tmp, func=AF.Exp, scale=-0.5), [tmpb], [out_b])

    xv = xres.rearrange("(k p) s -> p k s", p=128)

    for l in range(depth):
        P.barrier(scr[:, 0:1])
        A.reset()
        cqT, b_cq = A.tile([3, S], BF16, "cqT")
        ckvT, b_ckv = A.tile([2, S], BF16, "ckvT")
        hT, b_h = A.tile([8, S], BF16, "hT")
        Bh = [P.buf(f"h{g}") for g in range(NG)]
        xg_r = Rot(A, 1, [8, 512], F32, "xg")
        sq_r = Rot(A, 3, [512], BF16, "sq")
        f32_r = Rot(A, 6, [512], F32, "f32")
        w_r = Rot(A, 2, [8, 256], BF16, "wblk")
        wv_t, b_wv = A.tile([8, 512], BF16, "wv")
        st_r = Rot(A, 2, [S], BF16, "stage")
        tab_r = Rot(A, 2, [2, 512], F32, "tab")
        vst_r = Rot(A, 2, [4, 512], BF16, "vst")
        pr = PsRot([0, 1, 2, 3, 4, 5, 6, 7])

        def sl(g):
            return slice(g * 512, (g + 1) * 512)

        for g in range(NG):
            xg, bxg = xg_r.next()
            P.dma("sync", xg, xv[:, :, sl(g)], reads=[Bx[g]], writes=[bxg])
            ps, pb = pr.next()
            for k in range(8):
                sq, bsq = sq_r.next()
                ACT(lambda e, sq=sq, xg=xg, k=k: e.activation(out=sq, in_=xg[:, k, :], func=AF.Square), [bxg], [bsq])
                MM(ps, onesb, sq, k == 0, k == 7, [bsq, Bcb], pb)
            tmp, tmpb = f32_r.next()
            rs, rsb = f32_r.next()
            rstd_from_ps(ps, pb, 1024.0, rs, rsb, tmp, tmpb)
            for k in range(8):
                DVE(lambda e, k=k, xg=xg, rs=rs, g=g: e.tensor_tensor(out=hT[:, k, sl(g)], in0=xg[:, k, :], in1=rs, op=ALU.mult), [bxg, rsb], [Bh[g]])

        wa = wview("WA", l)

        def fm_block(c0, M, n_mm=1, c1=None):
            wt, wtb = w_r.next()
            P.dma("sync", wt[:, :, 0:M], wa[:, :, c0:c0 + M], reads=[Bwl[l]], writes=[wtb])
            if c1 is not None:
                P.dma("sync", wt[:, :, 128:128 + M], wa[:, :, c1:c1 + M], reads=[Bwl[l]], writes=[wtb])
            return wt, wtb

        def fm_mm(wt, wtb, off, M, g):
            ps, pb = pr.next()
            for k in range(8):
                MM(ps[0:M, :], wt[:, k, off:off + M], hT[:, k, sl(g)], k == 0, k == 7, [wtb, Bh[g]], pb)
            return ps, pb

        for name, dst, dstb, nblk in (("cq", cqT, b_cq, 3), ("ckv", ckvT, b_ckv, 2)):
            for c in range(nblk):
                wt, wtb = fm_block(WA_OFF[name] + c * 128, 128)
                for g in range(NG):
                    ps, pb = fm_mm(wt, wtb, 0, 128, g)
                    ACT(lambda e, ps=ps, dst=dst, c=c, g=g: e.copy(out=dst[:, c, sl(g)], in_=ps), [pb], [dstb])
        for name, dd, dbuf, scale in (("sbq", d_sq, Bd["sq"], 0.125), ("sbk", d_sk, Bd["sk"], 1.0)):
            for c in range(4):
                wt, wtb = fm_block(WA_OFF[name] + c * 128, 128)
                st, stb = st_r.next()
                for g in range(NG):
                    ps, pb = fm_mm(wt, wtb, 0, 128, g)
                    ACT(lambda e, ps=ps, st=st, g=g, scale=scale: e.mul(out=st[:, sl(g)], in_=ps, mul=scale), [pb], [stb])
                P.dma("gpsimd", dd[c * 128:(c + 1) * 128, :], st, reads=[stb], writes=[dbuf])
        for name, sname, dd, dbuf, scale, M, tab, nblk in (
                ("rq", "rqs", d_rq, Bd["rq"], 1.0, 128, tabR, 4), ("rk", "rks", d_rk, Bd["rk"], 0.125, 128, tabR, 4),
                ("kpe", "kpes", d_krot, Bd["krot"], 1.0, 32, tabM, 1)):
            for c in range(nblk):
                wt, wtb = fm_block(WA_OFF[name] + c * 128, M, c1=WA_OFF[sname] + c * 128)
                st, stb = st_r.next()
                for g in range(NG):
                    tb, tbb = tab_r.next()
                    P.dma("sync", tb[0:M], tab[0:M, :, sl(g)], reads=[Btab], writes=[tbb])
                    ps, pb = fm_mm(wt, wtb, 0, M, g)
                    ps2, pb2 = fm_mm(wt, wtb, 128, M, g)
                    t1, t1b = f32_r.next()
                    t2, t2b = f32_r.next()
                    DVE(lambda e, ps=ps, tb=tb, t1=t1, M=M, scale=scale: e.scalar_tensor_tensor(out=t1[0:M], in0=ps[0:M, :], scalar=scale, in1=tb[0:M, 0, :], op0=ALU.mult, op1=ALU.mult), [pb, tbb], [t1b])
                    DVE(lambda e, ps2=ps2, tb=tb, t2=t2, M=M, scale=scale: e.scalar_tensor_tensor(out=t2[0:M], in0=ps2[0:M, :], scalar=scale, in1=tb[0:M, 1, :], op0=ALU.mult, op1=ALU.mult), [pb2, tbb], [t2b])
                    POOL(lambda e, st=st, t1=t1, t2=t2, g=g, M=M: e.tensor_tensor(out=st[0:M, sl(g)], in0=t1[0:M], in1=t2[0:M], op=ALU.add), [t1b, t2b], [stb])
                P.dma("gpsimd", dd[c * 128:c * 128 + M, :], st[0:M], reads=[stb], writes=[dbuf])
        wvv = wview("WV", l)
        for vi, (dd, dbuf) in enumerate(((d_sv, Bd["sv"]), (d_rv, Bd["rv"]))):
            P.dma("sync", wv_t, wvv[:, :, vi * 512:(vi + 1) * 512], reads=[Bwl[l]], writes=[b_wv])
            ddv = dd.rearrange("(b p) c -> p b c", p=128)
            for g in range(NG):
                vs, vsb = vst_r.next()
                for j in range(4):
                    tbi = g * 4 + j
                    ps, pb = pr.next()
                    for k in range(8):
                        MM(ps, hT[:, k, tbi * 128:(tbi + 1) * 128], wv_t[:, k, :], k == 0, k == 7, [Bh[g], b_wv], pb)
                    if j % 2 == 0:
                        ACT(lambda e, ps=ps, vs=vs, j=j: e.copy(out=vs[:, j, :], in_=ps), [pb], [vsb])
                    else:
                        DVE(lambda e, ps=ps, vs=vs, j=j: e.tensor_copy(out=vs[:, j, :], in_=ps), [pb], [vsb])
                P.dma("gpsimd", ddv[:, g * 4:(g + 1) * 4, :], vs, reads=[vsb], writes=[dbuf])

        P.barrier(scr[:, 0:1])
        A.reset()
        cqT, b_cq = A.tile([3, S], BF16, "cqT")
        ckvT, b_ckv = A.tile([2, S], BF16, "ckvT")
        sq_r = Rot(A, 4, [512], BF16, "sq")
        f32_r = Rot(A, 8, [512], F32, "f32")
        tab_r = Rot(A, 2, [2, 512], F32, "tab")
        wq_t, b_wq = A.tile([3, 1024], BF16, "wq")
        wkv_t, b_wkv = A.tile([2, 1024], BF16, "wkv")
        ost_r = Rot(A, 4, [512], BF16, "ost")
        vaug_r = Rot(A, 2, [4, 8, 65], BF16, "vaug")
        rtok_r = Rot(A, 2, [4], F32, "rtok")
        P.dma("sync", wq_t, wview("WQ", l), reads=[Bwl[l]], writes=[b_wq])
        P.dma("sync", wkv_t, wview("WKV", l), reads=[Bwl[l]], writes=[b_wkv])
        for it in vaug_r.items:
            DVE(lambda e, t=it[0]: e.memset(t[:, :, :, 64:65], 1.0), [], [it[1]])
        vav = d_va.rearrange("(b p) h e -> p b (h e)", p=128)
        for g in range(NG):
            tbm, tbmb = tab_r.next()
            P.dma("sync", tbm, tabM[:, :, sl(g)], reads=[Btab], writes=[tbmb])
            ps, pb = pr.next()
            for k in range(3):
                sq, bsq = sq_r.next()
                POOL(lambda e, sq=sq, k=k, g=g: e.tensor_tensor(out=sq, in0=cqT[:, k, sl(g)], in1=cqT[:, k, sl(g)], op=ALU.mult), [b_cq], [bsq])
                MM(ps, onesb, sq, k == 0, k == 2, [bsq, Bcb], pb)
            tmp, tmpb = f32_r.next()
            rq_, rqb = f32_r.next()
            rstd_from_ps(ps, pb, 384.0, rq_, rqb, tmp, tmpb)
            ps, pb = pr.next()
            ps_t, pb_t = pr.next()
            sqs = []
            for k in range(2):
                sq, bsq = sq_r.next()
                POOL(lambda e, sq=sq, k=k, g=g: e.tensor_tensor(out=sq, in0=ckvT[:, k, sl(g)], in1=ckvT[:, k, sl(g)], op=ALU.mult), [b_ckv], [bsq])
                MM(ps, onesb, sq, k == 0, k == 1, [bsq, Bcb], pb)
                sqs.append((sq, bsq))
            for j in range(4):
                for k in range(2):
                    MM(ps_t[:, j:j + 1], sqs[k][0][:, j * 128:(j + 1) * 128], onesb[:, 0:1], k == 0, k == 1, [sqs[k][1], Bcb], pb_t)
            tmp2, tmp2b = f32_r.next()
            rkv, rkvb = f32_r.next()
            rstd_from_ps(ps, pb, 256.0, rkv, rkvb, tmp2, tmp2b)
            rt, rtb = rtok_r.next()
            ACT(lambda e, rt=rt, ps_t=ps_t: e.activation(out=rt, in_=ps_t[:, 0:4], func=AF.Ln, bias=EPS, scale=1.0 / 256.0), [pb_t], [rtb])
            ACT(lambda e, rt=rt: e.activation(out=rt, in_=rt, func=AF.Exp, scale=-0.5), [rtb], [rtb])
            for c in range(4):
                ps, pb = pr.next()
                for k in range(3):
                    MM(ps, wq_t[:, k, c * 128:(c + 1) * 128], cqT[:, k, sl(g)], k == 0, k == 2, [b_wq, b_cq], pb)
                o, ob = ost_r.next()
                DVE(lambda e, o=o, ps=ps, rq_=rq_: e.scalar_tensor_tensor(out=o, in0=ps, scalar=QS, in1=rq_, op0=ALU.mult, op1=ALU.mult), [pb, rqb], [ob])
                P.dma("gpsimd", d_qn[c * 128:(c + 1) * 128, sl(g)], o, reads=[ob], writes=[Bd["qn"]])
            for c in range(2):
                ps, pb = pr.next()
                ps2, pb2 = pr.next()
                for k in range(3):
                    MM(ps, wq_t[:, k, 512 + c * 128:512 + (c + 1) * 128], cqT[:, k, sl(g)], k == 0, k == 2, [b_wq, b_cq], pb)
                for k in range(3):
                    MM(ps2, wq_t[:, k, 768 + c * 128:768 + (c + 1) * 128], cqT[:, k, sl(g)], k == 0, k == 2, [b_wq, b_cq], pb2)
                t1, t1b = f32_r.next()
                t2, t2b = f32_r.next()
                DVE(lambda e, ps=ps, tbm=tbm, t1=t1: e.scalar_tensor_tensor(out=t1, in0=ps, scalar=QS, in1=tbm[:, 0, :], op0=ALU.mult, op1=ALU.mult), [pb, tbmb], [t1b])
                DVE(lambda e, ps2=ps2, tbm=tbm, t2=t2: e.scalar_tensor_tensor(out=t2, in0=ps2, scalar=QS, in1=tbm[:, 1, :], op0=ALU.mult, op1=ALU.mult), [pb2, tbmb], [t2b])
                POOL(lambda e, t1=t1, t2=t2: e.tensor_tensor(out=t1, in0=t1, in1=t2, op=ALU.add), [t1b, t2b], [t1b])
                o, ob = ost_r.next()
                POOL(lambda e, o=o, t1=t1, rq_=rq_: e.tensor_tensor(out=o, in0=t1, in1=rq_, op=ALU.mult), [t1b, rqb], [ob])
                P.dma("gpsimd", d_qr[c * 128:(c + 1) * 128, sl(g)], o, reads=[ob], writes=[Bd["qr"]])
            for c in range(4):
                ps, pb = pr.next()
                for k in range(2):
                    MM(ps, wkv_t[:, k, c * 128:(c + 1) * 128], ckvT[:, k, sl(g)], k == 0, k == 1, [b_wkv, b_ckv], pb)
                o, ob = ost_r.next()
                DVE(lambda e, o=o, ps=ps, rkv=rkv: e.tensor_tensor(out=o, in0=ps, in1=rkv, op=ALU.mult), [pb, rkvb], [ob])
                P.dma("gpsimd", d_kn[c * 128:(c + 1) * 128, sl(g)], o, reads=[ob], writes=[Bd["kn"]])
            va, vab = vaug_r.next()
            for j in range(4):
                tbi = g * 4 + j
                ps, pb = pr.next()
                for k in range(2):
                    MM(ps, ckvT[:, k, tbi * 128:(tbi + 1) * 128], wkv_t[:, k, 512:1024], k == 0, k == 1, [b_ckv, b_wkv], pb)
                DVE(lambda e, va=va, ps=ps, rt=rt, j=j: e.tensor_scalar(out=va[:, j, :, 0:64], in0=ps.rearrange("p (h e) -> p h e", e=64), scalar1=rt[:, j:j + 1], scalar2=None, op0=ALU.mult), [pb, rtb], [vab])
            P.dma("gpsimd", vav[:, g * 4:(g + 1) * 4, :], va.rearrange("p j h e -> p j (h e)"), reads=[vab], writes=[Bd["va"]])

        P.barrier(scr[:, 0:1])
        A.reset()
        q_r = Rot(A, 2, [S], BF16, "q")
        k_r = Rot(A, 2, [S], BF16, "k")
        v_r = Rot(A, 2, [NB, 128], BF16, "v")
        p_r = Rot(A, 6, [512], BF16, "p")
        e_r = Rot(A, 4, [512], F32, "e")
        t_r = Rot(A, 4, [512], F32, "t")
        lp_r = Rot(A, 4, [512], BF16, "lp")
        csb_r = Rot(A, 2, [512], F32, "csb")
        ys_r = Rot(A, 2, [S], BF16, "ys")
        rden, rdenb = A.tile([512], F32, "rden")
        bcs, bcsb = A.tile([512], F32, "bcs")
        ycs_r = Rot(A, 4, [512], F32, "ycs")
        st_t, st_b = A.tile([64], F32, "state")
        prevb_t, prevb_b = A.tile([64], BF16, "prevb")
        ktok_r = Rot(A, 3, [128], BF16, "ktok")
        sT_r = Rot(A, 6, [128], BF16, "sT")
        tmpc_r = Rot(A, 4, [128], F32, "tmpc")
        cld_r = Rot(A, 3, [2048], F32, "cld")
        ccv_r = Rot(A, 3, [2048], BF16, "ccv")
        pend = conv_piece_list(l + 1) if (OPT_CONVOVL and l + 1 < depth) else []

        def pipeline(n, stages):
            maxs = max(sk for _, sk in stages)
            for step in range(n + maxs):
                for fn, sk in stages:
                    i = step - sk
                    if 0 <= i < n:
                        fn(i)

        tiles_fwd = [(g, kb) for g in range(NG) for kb in range(4 * (g + 1))]
        tiles_rev = [(g, kb) for g in range(NG) for kb in range(4 * (g + 1) - 1, -1, -1)]
        ps_s = PsRot([0, 1, 2, 3])
        ps_y = PsRot([4, 5])
        ps_c = PsRot([6, 7])
        vav4 = d_va.rearrange("(b p) h e -> p b h e", p=128)
        for h in range(8):
            qt, qb = q_r.next()
            kt, kb_ = k_r.next()
            vt, vb = v_r.next()
            P.dma("sync", qt[0:64], d_qn[h * 64:(h + 1) * 64, :], reads=[Bd["qn"]], writes=[qb])
            P.dma("sync", qt[64:96], d_qr[h * 32:(h + 1) * 32, :], reads=[Bd["qr"]], writes=[qb])
            P.dma("sync", kt[0:64], d_kn[h * 64:(h + 1) * 64, :], reads=[Bd["kn"]], writes=[kb_])
            P.dma("sync", kt[64:96], d_krot, reads=[Bd["krot"]], writes=[kb_])
            P.dma("sync", vt[:, :, 0:65], vav4[:, :, h, :], reads=[Bd["va"]], writes=[vb])
            ys, ysb = ys_r.next()
            stt = {}
            ypsd = {}

            def stA(i, qt=qt, qb=qb, kt=kt, kb_=kb_, stt=stt):
                g, kb = tiles_fwd[i]
                if pend and i % 10 == 5:
                    emit_conv(pend.pop(0), cld_r, ccv_r, False)
                sps, spb = ps_s.next()
                MM(sps, kt[0:96, kb * 128:(kb + 1) * 128], qt[0:96, sl(g)], True, True, [kb_, qb], spb)
                pt, ptb = p_r.next()
                ACT(lambda e: e.activation(out=pt, in_=sps, func=AF.Exp), [spb], [ptb])
                d = kb - 4 * g
                if d >= 0:
                    DVE(lambda e: e.tensor_tensor(out=pt, in0=pt, in1=maskA[:, d, :], op=ALU.mult), [ptb, Bcb], [ptb])
                stt[i] = (pt, ptb)

            def stB(i, vt=vt, vb=vb, stt=stt, ypsd=ypsd, ys=ys, ysb=ysb):
                g, kb = tiles_fwd[i]
                nkb = 4 * (g + 1)
                pt, ptb = stt.pop(i)
                if kb == 0:
                    ypsd[g] = ps_y.next()
                yps, ypb = ypsd[g]
                MM(yps[0:65, :], vt[:, kb, 0:65], pt, kb == 0, kb == nkb - 1, [vb, ptb], ypb)
                if kb == nkb - 1:
                    DVE(lambda e: e.reciprocal(out=rden[64:65, :], in_=yps[64:65, :]), [ypb], [rdenb])
                    bps, bpb = ps_c.next()
                    MM(bps[0:64, :], ones32[64:65, 0:64], rden[64:65, :], True, True, [rdenb, Bcb], bpb)
                    ACT(lambda e: e.copy(out=bcs[0:64, :], in_=bps[0:64, :]), [bpb], [bcsb])
                    DVE(lambda e: e.tensor_tensor(out=ys[0:64, sl(g)], in0=yps[0:64, :], in1=bcs[0:64, :], op=ALU.mult), [ypb, bcsb], [ysb])

            pipeline(len(tiles_fwd), [(stA, 0), (stB, 2 * OPT_MLA)])
            P.dma("gpsimd", d_ya[h * 64:(h + 1) * 64, :], ys[0:64], reads=[ysb], writes=[Bd["ya"]])
        while pend:
            emit_conv(pend.pop(0), cld_r, ccv_r, False)

        svv = d_sv.rearrange("(b p) c -> p b c", p=128)
        ps_a = PsRot([0, 1, 2, 3])
        ps_c = PsRot([4, 5])
        ps_y = PsRot([6, 7])
        for h in range(8):
            qt, qb = q_r.next()
            kt, kb_ = k_r.next()
            vt, vb = v_r.next()
            P.dma("sync", qt[0:64], d_sq[h * 64:(h + 1) * 64, :], reads=[Bd["sq"]], writes=[qb])
            P.dma("sync", kt[0:64], d_sk[h * 64:(h + 1) * 64, :], reads=[Bd["sk"]], writes=[kb_])
            P.dma("sync", vt[:, :, 0:64], svv[:, :, h * 64:(h + 1) * 64], reads=[Bd["sv"]], writes=[vb])
            ys, ysb = ys_r.next()
            stt = {}
            gst = {}
            n_t = len(tiles_rev)

            def s0(i, qt=qt, qb=qb, kt=kt, kb_=kb_, stt=stt, gst=gst):
                g, kb = tiles_rev[i]
                nkb = 4 * (g + 1)
                if kb == nkb - 1:
                    cs_t, cs_b = csb_r.next()
                    POOL(lambda e: e.memset(cs_t, 0.0), [], [cs_b])
                    gst[g] = dict(csb=(cs_t, cs_b), yps=ps_y.next())
                aps, apb = ps_a.next()
                MM(aps, kt[0:64, kb * 128:(kb + 1) * 128], qt[0:64, sl(g)], True, False, [kb_, qb], apb)
                et, etb = e_r.next()
                ACT(lambda e: e.activation(out=et, in_=aps, func=AF.Exp), [apb], [etb])
                stt[i] = dict(aps=(aps, apb), et=(et, etb))

            def s1(i, stt=stt):
                g, kb = tiles_rev[i]
                d = kb - 4 * g
                et, etb = stt[i]["et"]
                lp, lpb = lp_r.next()
                ACT(lambda e: e.activation(out=lp, in_=et, func=AF.Ln, bias=1.0), [etb], [lpb])
                if d >= 0:
                    DVE(lambda e: e.tensor_tensor(out=lp, in0=lp, in1=maskS[:, d, :], op=ALU.mult), [lpb, Bcb], [lpb])
                stt[i]["lp"] = (lp, lpb)

            def s2(i, stt=stt):
                g, kb = tiles_rev[i]
                aps, apb = stt[i]["aps"]
                lp, lpb = stt[i]["lp"]
                MM(aps, trineg, lp, False, True, [Bcb, lpb], apb)
                if kb > 0:
                    cps, cpb = ps_c.next()
                    MM(cps, onesb, lp, True, True, [Bcb, lpb], cpb)
                    stt[i]["cps"] = (cps, cpb)

            def sU(i, stt=stt, gst=gst):
                g, kb = tiles_rev[i]
                if kb > 0:
                    cps, cpb = stt[i]["cps"]
                    cs_t, cs_b = gst[g]["csb"]
                    DVE(lambda e: e.tensor_tensor(out=cs_t, in0=cps, in1=cs_t, op=ALU.add), [cpb, cs_b], [cs_b])

            def sT_(i, stt=stt, gst=gst):
                g, kb = tiles_rev[i]
                aps, apb = stt[i]["aps"]
                cs_t, cs_b = gst[g]["csb"]
                tt, ttb = t_r.next()
                DVE(lambda e: e.tensor_tensor(out=tt, in0=aps, in1=cs_t, op=ALU.subtract), [apb, cs_b], [ttb])
                stt[i]["tt"] = (tt, ttb)

            def s4(i, stt=stt):
                g, kb = tiles_rev[i]
                d = kb - 4 * g
                tt, ttb = stt[i]["tt"]
                pt, ptb = p_r.next()
                ACT(lambda e: e.activation(out=pt, in_=tt, func=AF.Exp), [ttb], [ptb])
                if d >= 0:
                    DVE(lambda e: e.tensor_tensor(out=pt, in0=pt, in1=maskS[:, d, :], op=ALU.mult), [ptb, Bcb], [ptb])
                stt[i]["pt"] = (pt, ptb)

            def s5(i, vt=vt, vb=vb, stt=stt, gst=gst, ys=ys, ysb=ysb):
                g, kb = tiles_rev[i]
                nkb = 4 * (g + 1)
                pt, ptb = stt.pop(i)["pt"]
                yps, ypb = gst[g]["yps"]
                MM(yps[0:64, :], vt[:, kb, 0:64], pt, kb == nkb - 1, kb == 0, [vb, ptb], ypb)
                if kb == 0:
                    ACT(lambda e: e.copy(out=ys[0:64, sl(g)], in_=yps[0:64, :]), [ypb], [ysb])

            pipeline(n_t, [(s0, 0), (s1, 1), (s2, 2), (sU, 3), (sT_, 2), (s4, 3), (s5, 4)] if OPT_SB else [(s0, 0), (s1, 0), (s2, 0), (sT_, 0), (sU, 0), (s4, 0), (s5, 0)])
            P.dma("gpsimd", d_yb[h * 64:(h + 1) * 64, :], ys[0:64], reads=[ysb], writes=[Bd["yb"]])

        rvv = d_rv.rearrange("(b p) c -> p b c", p=128)
        dtv = C("dt").rearrange("p (h c) -> p h c", c=128)
        ztv = C("zeta").rearrange("p (a c) -> p a c", c=128)
        xiv = C("xi").rearrange("p (h c) -> p h c", c=128)
        for pr_i in range(4):
            qt, qb = q_r.next()
            kt, kb_ = k_r.next()
            vt, vb = v_r.next()
            P.dma("sync", qt, d_rq[pr_i * 128:(pr_i + 1) * 128, :], reads=[Bd["rq"]], writes=[qb])
            P.dma("sync", kt, d_rk[pr_i * 128:(pr_i + 1) * 128, :], reads=[Bd["rk"]], writes=[kb_])
            P.dma("sync", vt, rvv[:, :, pr_i * 128:(pr_i + 1) * 128], reads=[Bd["rv"]], writes=[vb])
            POOL(lambda e: e.memset(st_t, 0.0), [], [st_b])
            POOL(lambda e: e.memset(prevb_t, 0.0), [], [prevb_b])
            ycs_cur = [None, None]
            for n in range(NB):
                cs = slice(n * 128, (n + 1) * 128)
                par = n % 2
                tps, tpb = psb[7], PB[7]
                kps, kpb = psb[6], PB[6]
                abk = [(psb[2 * par + j], PB[2 * par + j]) for j in range(2)]
                xbk = [(psb[4 + j], PB[4 + j]) for j in range(2)]
                tpv = tps[:, :].bitcast(BF16)
                P.op("tensor", lambda e, tpv=tpv, kt=kt, cs=cs: e.transpose(tpv[:, 0:128], kt[:, cs], identb), [kb_, Bcb], [tpb])
                ktk, ktkb = ktok_r.next()
                DVE(lambda e, ktk=ktk, tpv=tpv, pr_i=pr_i: e.tensor_tensor(out=ktk, in0=tpv[:, 0:128], in1=ztv[:, pr_i, :], op=ALU.mult), [tpb, Bc], [ktkb])
                for j in range(2):
                    rs_ = slice(64 * j, 64 * j + 64)
                    aps, apb = abk[j]
                    MM(aps[:, 0:128], kt[rs_, cs], qt[rs_, cs], True, True, [kb_, qb], apb)
                MM(kps[:, 0:128], ktk, vt[:, n, :], True, True, [ktkb, vb], kpb)
                for j in range(2):
                    rs_ = slice(64 * j, 64 * j + 64)
                    xps, xpb = xbk[j]
                    MM(xps[0:64, 0:128], prevb_t[rs_, :], qt[rs_, cs], True, True, [prevb_b, qb], xpb)
                sTs = []
                for j in range(2):
                    hh = 2 * pr_i + j
                    aps, apb = abk[j]
                    sT, sTb = sT_r.next()
                    DVE(lambda e, sT=sT, aps=aps, hh=hh: e.tensor_tensor(out=sT, in0=aps[:, 0:128], in1=dtv[:, hh, :], op=ALU.mult), [apb, Bc], [sTb])
                    sTs.append((sT, sTb))
                for j in range(2):
                    hh = 2 * pr_i + j
                    rs_ = slice(64 * j, 64 * j + 64)
                    cdc = C("cdecay")[rs_, hh:hh + 1]
                    DVE(lambda e, kps=kps, rs_=rs_, cdc=cdc: e.scalar_tensor_tensor(out=st_t[rs_, :], in0=st_t[rs_, :], scalar=cdc, in1=kps[rs_, rs_], op0=ALU.mult, op1=ALU.add), [kpb, st_b, Bc], [st_b])
                ACT(lambda e: e.copy(out=prevb_t, in_=st_t), [st_b], [prevb_b])
                for j in range(2):
                    rs_ = slice(64 * j, 64 * j + 64)
                    aps, apb = abk[j]
                    sT, sTb = sTs[j]
                    MM(aps[0:64, 0:128], vt[:, n, rs_], sT, True, True, [vb, sTb], apb)
                for j in range(2):
                    hh = 2 * pr_i + j
                    aps, apb = abk[j]
                    xps, xpb = xbk[j]
                    if n % 4 == 0:
                        ycs_cur[j] = ycs_r.next()
                    yc_t, yc_b = ycs_cur[j]
                    tc_, tcb = tmpc_r.next()
                    DVE(lambda e, tc_=tc_, xps=xps, hh=hh: e.tensor_tensor(out=tc_[0:64], in0=xps[0:64, 0:128], in1=xiv[0:64, hh, :], op=ALU.mult), [xpb, Bc], [tcb])
                    off = (n % 4) * 128
                    DVE(lambda e, yc_t=yc_t, aps=aps, tc_=tc_, off=off: e.tensor_tensor(out=yc_t[0:64, off:off + 128], in0=aps[0:64, 0:128], in1=tc_[0:64], op=ALU.add), [apb, tcb], [yc_b])
                    if n % 4 == 3:
                        P.dma("gpsimd", d_yc[hh * 64:(hh + 1) * 64, (n - 3) * 128:(n + 1) * 128], yc_t[0:64], reads=[yc_b], writes=[Bd["yc"]])

        P.barrier(scr[:, 0:1])
        A.reset()
        xg_r = Rot(A, 2, [8, 512], F32, "xg")
        hg, hgb = A.tile([8, 512], BF16, "hg")
        sq_r = Rot(A, 3, [512], BF16, "sq")
        f32_r = Rot(A, 6, [512], F32, "f32")
        ya_r = Rot(A, 2, [4, 512], BF16, "ya")
        yb_r = Rot(A, 2, [4, 512], BF16, "yb")
        yc_r = Rot(A, 1, [4, 512], F32, "yc")
        ycg, ycgb = A.tile([4, 512], BF16, "ycg")
        mT, mTb = A.tile([8, 512], BF16, "mT")
        h2, h2b = A.tile([8, 512], BF16, "h2")
        act, actb = A.tile([32, 512], BF16, "act")
        wg_r = Rot(A, 3, [8, 128], BF16, "wg")
        wb_r = Rot(A, 3, [4, 128], BF16, "wbr")
        wo_r = Rot(A, 2, [8, 128], BF16, "wo")
        wu_r = Rot(A, 2, [8, 512], BF16, "wu")
        wd_r = Rot(A, 2, [32, 128], BF16, "wd")
        macc, maccb = A.tile([512], F32, "macc")
        pr = PsRot([0, 1, 2, 3, 4, 5, 6, 7])
        wgv = wview("WG", l)
        wbv = Wb["WB"][l].rearrange("(n k p) d -> p n k d", p=128, k=4)
        wov = wview("WO", l)
        wuv = wview("WU", l)
        wdv = wview("WD", l)
        yav = d_ya.rearrange("(k p) s -> p k s", p=128)
        ybv = d_yb.rearrange("(k p) s -> p k s", p=128)
        ycv = d_yc.rearrange("(k p) s -> p k s", p=128)
        bdm = C("bd")
        last = (l == depth - 1)
        for g in range(NG):
            xg, bxg = xg_r.next()
            P.dma("sync", xg, xv[:, :, sl(g)], reads=[Bx[g]], writes=[bxg])
            yat, yab = ya_r.next()
            ybt, ybb = yb_r.next()
            yct, ycb = yc_r.next()
            P.dma("sync", yat, yav[:, :, sl(g)], reads=[Bd["ya"]], writes=[yab])
            P.dma("sync", ybt, ybv[:, :, sl(g)], reads=[Bd["yb"]], writes=[ybb])
            P.dma("sync", yct, ycv[:, :, sl(g)], reads=[Bd["yc"]], writes=[ycb])
            ps, pb = pr.next()
            for k in range(8):
                sq, bsq = sq_r.next()
                ACT(lambda e, sq=sq, xg=xg, k=k: e.activation(out=sq, in_=xg[:, k, :], func=AF.Square), [bxg], [bsq])
                MM(ps, onesb, sq, k == 0, k == 7, [bsq, Bcb], pb)
            tmp, tmpb = f32_r.next()
            rs, rsb = f32_r.next()
            rstd_from_ps(ps, pb, 1024.0, rs, rsb, tmp, tmpb)
            for k in range(8):
                DVE(lambda e, k=k, xg=xg, rs=rs: e.tensor_tensor(out=hg[:, k, :], in0=xg[:, k, :], in1=rs, op=ALU.mult), [bxg, rsb], [hgb])
            for c in range(4):
                wt, wtb = wg_r.next()
                P.dma("sync", wt, wgv[:, :, c * 128:(c + 1) * 128], reads=[Bwl[l]], writes=[wtb])
                gps, gpb = pr.next()
                for k in range(8):
                    MM(gps, wt[:, k, :], hg[:, k, :], k == 0, k == 7, [wtb, hgb], gpb)
                sil, silb = f32_r.next()
                ACT(lambda e, sil=sil, gps=gps: e.activation(out=sil, in_=gps, func=AF.Silu), [gpb], [silb])
                mps, mpb = pr.next()
                MM(mps, bdm, yct[:, c, :], True, True, [Bc, ycb], mpb)
                cen, cenb = f32_r.next()
                DVE(lambda e, cen=cen, mps=mps, yct=yct, c=c: e.scalar_tensor_tensor(out=cen, in0=mps, scalar=-1.0, in1=yct[:, c, :], op0=ALU.mult, op1=ALU.add), [mpb, ycb], [cenb])
                sq32, sq32b = f32_r.next()
                POOL(lambda e, sq32=sq32, cen=cen: e.tensor_tensor(out=sq32, in0=cen, in1=cen, op=ALU.mult), [cenb], [sq32b])
                vps, vpb = pr.next()
                MM(vps, bdm, sq32, True, True, [Bc, sq32b], vpb)
                tmp, tmpb = f32_r.next()
                rv_, rvb = f32_r.next()
                rstd_from_ps(vps, vpb, 1.0, rv_, rvb, tmp, tmpb)
                gcol = gsb[:, l, 21 + c:22 + c]
                DVE(lambda e, cen=cen, rv_=rv_, gcol=gcol: e.scalar_tensor_tensor(out=cen, in0=cen, scalar=gcol, in1=rv_, op0=ALU.mult, op1=ALU.mult), [cenb, rvb, Bg], [cenb])
                POOL(lambda e, cen=cen, sil=sil, c=c: e.tensor_tensor(out=ycg[:, c, :], in0=cen, in1=sil, op=ALU.mult), [cenb, silb], [ycgb])
            for db in range(8):
                for n in range(3):
                    ysrc, ysb_ = ((yat, yab), (ybt, ybb), (ycg, ycgb))[n]
                    wb_t, wb_b = wb_r.next()
                    P.dma("sync", wb_t, wbv[:, n, :, db * 128:(db + 1) * 128], reads=[Bwl[l]], writes=[wb_b])
                    wt, wtb = wg_r.next()
                    P.dma("sync", wt, wgv[:, :, 512 + n * 1024 + db * 128:512 + n * 1024 + (db + 1) * 128], reads=[Bwl[l]], writes=[wtb])
                    ups, upb = pr.next()
                    for k in range(4):
                        MM(ups, wb_t[:, k, :], ysrc[:, k, :], k == 0, k == 3, [wb_b, ysb_], upb)
                    gps, gpb = pr.next()
                    for k in range(8):
                        MM(gps, wt[:, k, :], hg[:, k, :], k == 0, k == 7, [wtb, hgb], gpb)
                    sg, sgb = f32_r.next()
                    ACT(lambda e, sg=sg, gps=gps: e.activation(out=sg, in_=gps, func=AF.Sigmoid), [gpb], [sgb])
                    if n == 0:
                        DVE(lambda e, ups=ups, sg=sg: e.tensor_tensor(out=macc, in0=ups, in1=sg, op=ALU.mult), [upb, sgb], [maccb])
                    else:
                        DVE(lambda e, ups=ups, sg=sg: e.tensor_tensor(out=sg, in0=ups, in1=sg, op=ALU.mult), [upb, sgb], [sgb])
                        if n == 1:
                            POOL(lambda e, sg=sg: e.tensor_tensor(out=macc, in0=macc, in1=sg, op=ALU.add), [maccb, sgb], [maccb])
                        else:
                            POOL(lambda e, sg=sg, db=db: e.tensor_tensor(out=mT[:, db, :], in0=macc, in1=sg, op=ALU.add), [maccb, sgb], [mTb])
            for ob_ in range(8):
                wt, wtb = wo_r.next()
                P.dma("sync", wt, wov[:, :, ob_ * 128:(ob_ + 1) * 128], reads=[Bwl[l]], writes=[wtb])
                ops_, opb = pr.next()
                for k in range(8):
                    MM(ops_, wt[:, k, :], mT[:, k, :], k == 0, k == 7, [wtb, mTb], opb)
                DVE(lambda e, xg=xg, ops_=ops_, ob_=ob_: e.tensor_tensor(out=xg[:, ob_, :], in0=ops_, in1=xg[:, ob_, :], op=ALU.add), [opb, bxg], [bxg])
            ps, pb = pr.next()
            for k in range(8):
                sq, bsq = sq_r.next()
                ACT(lambda e, sq=sq, xg=xg, k=k: e.activation(out=sq, in_=xg[:, k, :], func=AF.Square), [bxg], [bsq])
                MM(ps, onesb, sq, k == 0, k == 7, [bsq, Bcb], pb)
            tmp, tmpb = f32_r.next()
            rs, rsb = f32_r.next()
            rstd_from_ps(ps, pb, 1024.0, rs, rsb, tmp, tmpb)
            for k in range(8):
                DVE(lambda e, k=k, xg=xg, rs=rs: e.tensor_tensor(out=h2[:, k, :], in0=xg[:, k, :], in1=rs, op=ALU.mult), [bxg, rsb], [h2b])
            for fq in range(8):
                wt, wtb = wu_r.next()
                P.dma("sync", wt, wuv[:, :, fq * 512:(fq + 1) * 512], reads=[Bwl[l]], writes=[wtb])
                for fi in range(4):
                    f = fq * 4 + fi
                    ups, upb = pr.next()
                    for k in range(8):
                        MM(ups, wt[:, k, fi * 128:(fi + 1) * 128], h2[:, k, :], k == 0, k == 7, [wtb, h2b], upb)
                    rl, rlb = f32_r.next()
                    ACT(lambda e, rl=rl, ups=ups: e.activation(out=rl, in_=ups, func=AF.Relu), [upb], [rlb])
                    eng = DVE if f % 2 == 0 else POOL
                    eng(lambda e, rl=rl, f=f: e.tensor_tensor(out=act[:, f, :], in0=rl, in1=rl, op=ALU.mult), [rlb], [actb])
            for ob_ in range(8):
                wt, wtb = wd_r.next()
                P.dma("sync", wt, wdv[:, :, ob_ * 128:(ob_ + 1) * 128], reads=[Bwl[l]], writes=[wtb])
                ops_, opb = pr.next()
                for f in range(32):
                    MM(ops_, wt[:, f, :], act[:, f, :], f == 0, f == 31, [wtb, actb], opb)
                DVE(lambda e, xg=xg, ops_=ops_, ob_=ob_: e.tensor_tensor(out=xg[:, ob_, :], in0=ops_, in1=xg[:, ob_, :], op=ALU.add), [opb, bxg], [bxg])
            if not last:
                P.dma("gpsimd", xv[:, :, sl(g)], xg, reads=[bxg], writes=[Bx[g]])
            else:
                ps, pb = pr.next()
                for k in range(8):
                    sq, bsq = sq_r.next()
                    ACT(lambda e, sq=sq, xg=xg, k=k: e.activation(out=sq, in_=xg[:, k, :], func=AF.Square), [bxg], [bsq])
                    MM(ps, onesb, sq, k == 0, k == 7, [bsq, Bcb], pb)
                tmp, tmpb = f32_r.next()
                rs, rsb = f32_r.next()
                rstd_from_ps(ps, pb, 1024.0, rs, rsb, tmp, tmpb)
                for k in range(8):
                    gcol = gsb[:, 0, 25 + k:26 + k]
                    DVE(lambda e, k=k, xg=xg, rs=rs, gcol=gcol: e.scalar_tensor_tensor(out=xg[:, k, :], in0=xg[:, k, :], scalar=gcol, in1=rs, op0=ALU.mult, op1=ALU.mult), [bxg, rsb, Bg], [bxg])
                fin.append(P.dma("gpsimd", outT.rearrange("(k p) s -> p k s", p=128)[:, :, sl(g)], xg, reads=[bxg], writes=[Bout]))

    return nc, P


fin = []
Bout = Buf("out")


def build_and_emit(S=SEQ, depth=DEPTH, debug=()):
    global fin, Bout
    fin = []
    Bout = Buf("out")
    nc, P = build(S, depth, debug)
    if not fin:
        raise RuntimeError("no output")
    stats = P.emit(final_wait_ops=fin)
    return nc, stats


def kernel(**inputs):
    x = np.asarray(inputs["x"], np.float32)
    B, S, _ = x.shape
    positions = np.asarray(inputs["positions"]).astype(np.int32)
    com = host_layout(inputs)
    consts = build_consts()
    masks = build_masks()
    depth = com["WA"].shape[0]
    nc, _ = build_and_emit(S, depth)
    in_maps = []
    for b in range(B):
        m = dict(com)
        m["xin"] = np.ascontiguousarray(x[b].T)
        m["pos"] = np.ascontiguousarray(positions[b][None, :])
        m["consts"] = consts
        m["cmask"] = masks
        in_maps.append(m)
    res = run_bass_kernel_spmd(nc, in_maps, core_ids=list(range(B)))
    out = np.stack([np.ascontiguousarray(res.results[b]["outT"].T) for b in range(B)], 0)
    return out.astype(np.float32)
```
